# Optimizing a Trainium2 kernel written in Bass

```python
import math
import jax
import jax.numpy as jnp
from jax import lax
import numpy as np

D_MODEL = 1024
BATCH = 4
SEQ = 4096
DEPTH = 2

GRID_W = 64
CTX_LEN = 256
N_BRANCH = 4
W_BR = 256
N_MOD = 6
NORM_EPS = 1e-6
F32 = jnp.float32

HY_SHORT = 3
HY_BANDS = 8
HY_EMB = 2 * HY_BANDS + 1
HY_HID = 64
HY_TARGET = 1e-2
HY_FAST_PCT = 0.3
HY_SLOW_PCT = 1.5

S5_GROUP = 16
S5_GROUPS = W_BR // S5_GROUP
S5_STATE = 64

RW_HEAD = 64
RW_HEADS = W_BR // RW_HEAD
RW_W_RANK = 32
RW_A_RANK = 32
RW_G_RANK = 64
RW_GN_EPS = 64e-5

M2_HEADDIM = 64
M2_HEADS = W_BR // M2_HEADDIM
M2_GROUPS = 2
M2_STATE = 128
M2_CONV = 3

MOE_GROUPS = 4
MOE_PER_GROUP = 4
MOE_EXPERTS = MOE_GROUPS * MOE_PER_GROUP
MOE_TOPK = 2
MOE_FF = 512

HY_COLS = 3 * W_BR
S5_COLS = W_BR
RW_COLS = 3 * W_BR + 2 * RW_W_RANK + 2 * RW_A_RANK + RW_G_RANK
M2_XBC = W_BR + 2 * M2_GROUPS * M2_STATE
M2_COLS = W_BR + M2_XBC + 2 * M2_HEADS
GATE_COLS = N_BRANCH * D_MODEL
IN_COLS = HY_COLS + S5_COLS + RW_COLS + M2_COLS + GATE_COLS

kernel_name = 'hybrid_parallel_mixers_hmoe_dit'


def _split(x, sizes):
    idx = [int(i) for i in np.cumsum(sizes)[:-1]]
    return jnp.split(x, idx, axis=-1)


def rmsnorm(x, g):
    xf = x.astype(F32)
    xf = xf * lax.rsqrt(jnp.mean(xf * xf, axis=-1, keepdims=True) + NORM_EPS)
    return xf.astype(x.dtype) * g


def modulate(x, g, shift, scale):
    return rmsnorm(x, g) * (1 + scale) + shift


def centred_conv(x, w, b):
    k = w.shape[0]
    pad = k // 2
    L = x.shape[1]
    xp = jnp.pad(x, ((0, 0), (pad, pad), (0, 0)))
    y = b
    for j in range(k):
        y = y + xp[:, j:j + L] * w[j]
    return y


def centred_shift(x):
    xp = jnp.pad(x, ((0, 0), (1, 1), (0, 0)))
    return 0.5 * (xp[:, :-2] + xp[:, 2:])


def hyena_filters(L, w1, b1, w2, b2, w3, freq):
    t = jnp.linspace(0.0, 1.0, L, dtype=F32)[:, None]
    w = 2.0 * math.pi * jnp.arange(L, dtype=F32)[:, None] / L
    f = jnp.linspace(1e-4, HY_BANDS - 1, HY_BANDS, dtype=F32)[None, :]
    feats = jnp.concatenate([t, jnp.cos(f * w), -jnp.sin(f * w)], axis=-1)
    fr = freq.astype(F32)
    h = jnp.sin(fr * (feats @ w1.astype(F32) + b1.astype(F32)))
    h = jnp.sin(fr * (h @ w2.astype(F32) + b2.astype(F32)))
    h = h @ w3.astype(F32)
    deltas = jnp.abs(jnp.linspace(math.log(HY_TARGET) / HY_SLOW_PCT,
                                  math.log(HY_TARGET) / HY_FAST_PCT, W_BR, dtype=F32))
    decay = jnp.exp(-t * deltas)
    return h * jnp.concatenate([decay, decay], axis=-1)


def hyena_mixer(p, conv_w, conv_b, f_w1, f_b1, f_w2, f_b2, f_w3, f_freq, h_bias):
    L = p.shape[1]
    x0, x1, v = jnp.split(centred_conv(p, conv_w, conv_b), 3, axis=-1)
    filt = hyena_filters(L, f_w1, f_b1, f_w2, f_b2, f_w3, f_freq)
    h_f, h_b = filt[:, :W_BR], filt[:, W_BR:]
    filt_full = jnp.concatenate([h_f, jnp.zeros((1, W_BR), F32), h_b[:0:-1]], axis=0)
    u = (x1 * v).astype(F32)
    n = 2 * L
    y = jnp.fft.irfft(jnp.fft.rfft(u, n=n, axis=1) * jnp.fft.rfft(filt_full, axis=0)[None],
                      n=n, axis=1)[:, :L]
    y = y + h_bias.astype(F32) * u
    return x0 * y.astype(p.dtype)


def _lin_rec(e1, e2):
    a1, b1 = e1
    a2, b2 = e2
    return a1 * a2, a2 * b1 + b2


def s5_mixer(u, h0, lam_re, lam_im, log_step, b_re, b_im, c_re, c_im, d_skip, w_glu):
    b_, L, _ = u.shape
    uc = u.astype(F32).reshape(b_, L, S5_GROUPS, S5_GROUP).astype(jnp.complex64)
    y = d_skip * u
    finals = []
    for d in range(2):
        lam = lax.complex(lam_re[d].astype(F32), lam_im[d].astype(F32))
        lam_bar = jnp.exp(lam * jnp.exp(log_step[d].astype(F32))[:, None])
        b_bar = ((lam_bar - 1.0) / lam)[:, :, None] * lax.complex(b_re[d].astype(F32), b_im[d].astype(F32))
        seq = uc if d == 0 else jnp.flip(uc, 1)
        bu = jnp.einsum('blgi,gni->blgn', seq, b_bar)
        bu = bu.at[:, 0].add(lam_bar * h0[d])
        _, xs = lax.associative_scan(_lin_rec, (jnp.broadcast_to(lam_bar, bu.shape), bu), axis=1)
        finals.append(xs[:, -1])
        yd = jnp.einsum('blgn,gin->blgi', xs,
                        lax.complex(c_re[d].astype(F32), c_im[d].astype(F32))).real
        if d == 1:
            yd = jnp.flip(yd, 1)
        y = y + yd.reshape(b_, L, W_BR).astype(u.dtype)
    y = jax.nn.gelu(y)
    return y * jax.nn.sigmoid(y @ w_glu), jnp.stack(finals)


def wkv7(r, w, k, v, kk, a, s0, reverse):
    def step(s, inp):
        r_t, w_t, k_t, v_t, kk_t, a_t = inp
        sa = jnp.einsum('bhvk,bhk->bhv', s, kk_t)
        s = (s * w_t[:, :, None, :] - sa[..., None] * (kk_t * a_t)[:, :, None, :]
             + v_t[..., None] * k_t[:, :, None, :])
        return s, jnp.einsum('bhvk,bhk->bhv', s, r_t)
    xs = tuple(jnp.moveaxis(t, 1, 0) for t in (r, w, k, v, kk, a))
    s_final, y = lax.scan(step, s0, xs, reverse=reverse)
    return jnp.moveaxis(y, 0, 1), s_final


def rwkv_mixer(p, s0, mu, w0, w2, a0, a2, g2, k_k, k_a, r_k, ln_w, ln_b):
    out_dtype = p.dtype
    b_, L, _ = p.shape
    p = (p + (centred_shift(p) - p) * mu).astype(F32)
    r, k, v, wd, ad, gd = _split(p, [W_BR, W_BR, W_BR, 2 * RW_W_RANK, 2 * RW_A_RANK, RW_G_RANK])
    heads = lambda t: t.reshape(b_, L, RW_HEADS, RW_HEAD)
    g = jax.nn.sigmoid(gd) @ g2.astype(F32)
    kk = heads(k * k_k)
    kk = kk * lax.rsqrt(jnp.maximum(jnp.sum(kk * kk, -1, keepdims=True), 1e-24))
    rh, kh, vh = heads(r), heads(k), heads(v)
    y = None
    finals = []
    for d in range(2):
        w = w0[d] + jnp.tanh(wd[..., d * RW_W_RANK:(d + 1) * RW_W_RANK]) @ w2[d].astype(F32)
        decay = jnp.exp(-jnp.exp(-jax.nn.softplus(-w) - 0.5))
        a = jax.nn.sigmoid(a0[d] + ad[..., d * RW_A_RANK:(d + 1) * RW_A_RANK] @ a2[d].astype(F32))
        kd = heads(k * (1 + (a - 1) * k_a))
        yd, sf = wkv7(rh, heads(decay), kd, vh, kk, heads(a), s0[d], d == 1)
        y = yd if d == 0 else y + yd
        finals.append(sf)
    mean = jnp.mean(y, -1, keepdims=True)
    var = jnp.mean(jnp.square(y - mean), -1, keepdims=True)
    y = ((y - mean) * lax.rsqrt(var + RW_GN_EPS)).reshape(b_, L, W_BR) * ln_w + ln_b
    bonus = jnp.sum(rh * kh * r_k, -1, keepdims=True) * vh
    y = (y + bonus.reshape(b_, L, W_BR)) * g
    return y.astype(out_dtype), jnp.stack(finals)


def ssd_scan(xh, dt, a, bm, cm, h0, n_chunks):
    b_, L, H, P = xh.shape
    q = L // n_chunks
    rep = H // M2_GROUPS
    bh = jnp.repeat(bm, rep, axis=2).reshape(b_, n_chunks, q, H, M2_STATE)
    ch = jnp.repeat(cm, rep, axis=2).reshape(b_, n_chunks, q, H, M2_STATE)
    xdt = (xh * dt[..., None]).reshape(b_, n_chunks, q, H, P)
    acs = jnp.cumsum((dt * a).reshape(b_, n_chunks, q, H), axis=2)
    lower = jnp.tril(jnp.ones((q, q), bool))[None, None, :, :, None]
    seg = acs[:, :, :, None, :] - acs[:, :, None, :, :]
    decay_in = jnp.exp(jnp.where(lower, seg, -jnp.inf))
    scores = jnp.einsum('bcihn,bcjhn->bcijh', ch, bh) * decay_in
    y_diag = jnp.einsum('bcijh,bcjhp->bcihp', scores, xdt)
    decay_out = jnp.exp(acs[:, :, -1:, :] - acs)
    states = jnp.einsum('bcjhn,bcjh,bcjhp->bchpn', bh, decay_out, xdt)
    chunk_decay = jnp.exp(acs[:, :, -1, :])

    def step(h, inp):
        st, dec = inp
        return h * dec[:, :, None, None] + st, h

    h_final, h_enter = lax.scan(step, h0, (jnp.moveaxis(states, 1, 0), jnp.moveaxis(chunk_decay, 1, 0)))
    h_enter = jnp.moveaxis(h_enter, 0, 1)
    y_off = jnp.einsum('bcihn,bchpn,bcih->bcihp', ch, h_enter, jnp.exp(acs))
    return (y_diag + y_off).reshape(b_, L, H, P), h_final


def mamba_mixer(p, h0, n_chunks, conv_w, conv_b, a_log, dt_bias, d_skip, norm_w):
    b_, L, _ = p.shape
    z, xbc, dt = _split(p, [W_BR, M2_XBC, 2 * M2_HEADS])
    xbc = jax.nn.silu(centred_conv(xbc, conv_w, conv_b)).astype(F32)
    xs, bm, cm = _split(xbc, [W_BR, M2_GROUPS * M2_STATE, M2_GROUPS * M2_STATE])
    xh = xs.reshape(b_, L, M2_HEADS, M2_HEADDIM)
    bm = bm.reshape(b_, L, M2_GROUPS, M2_STATE)
    cm = cm.reshape(b_, L, M2_GROUPS, M2_STATE)
    y = d_skip.astype(F32)[:, None] * xh
    finals = []
    for d in range(2):
        dtd = jax.nn.softplus(dt[..., d * M2_HEADS:(d + 1) * M2_HEADS].astype(F32) + dt_bias[d].astype(F32))
        a = -jnp.exp(a_log[d].astype(F32))
        seqs = (xh, dtd, bm, cm) if d == 0 else tuple(jnp.flip(t, 1) for t in (xh, dtd, bm, cm))
        yd, hf = ssd_scan(seqs[0], seqs[1], a, seqs[2], seqs[3], h0[d], n_chunks)
        y = y + (yd if d == 0 else jnp.flip(yd, 1))
        finals.append(hf)
    y = y.reshape(b_, L, W_BR).astype(p.dtype) * jax.nn.silu(z)
    return rmsnorm(y, norm_w), jnp.stack(finals)


def merge(ys, gate_cols, w_branch, w_out):
    gates = jax.nn.sigmoid(gate_cols)
    out = sum(gates[..., i * D_MODEL:(i + 1) * D_MODEL] * (ys[i] @ w_branch[i]) for i in range(N_BRANCH))
    return out @ w_out


def moe_ffn(u, w_group, w_expert, w_gate, w_up, w_down):
    uf = u.astype(F32)
    grp_prob = jax.nn.softmax(uf @ w_group.astype(F32), axis=-1)
    grp_p, grp_idx = lax.top_k(grp_prob, 1)
    exp_logits = jnp.einsum('bld,gde->blge', uf, w_expert.astype(F32))
    sel = jnp.einsum('blge,blg->ble', exp_logits, jax.nn.one_hot(grp_idx[..., 0], MOE_GROUPS, dtype=F32))
    top_v, top_i = lax.top_k(sel, MOE_TOPK)
    top_w = jax.nn.softmax(top_v, axis=-1) * grp_p
    e_idx = grp_idx * MOE_PER_GROUP + top_i
    comb = jnp.sum(jax.nn.one_hot(e_idx, MOE_EXPERTS, dtype=F32) * top_w[..., None], axis=-2).astype(u.dtype)
    y = jnp.zeros_like(u)
    for e in range(MOE_EXPERTS):
        h = jax.nn.silu(u @ w_gate[e]) * (u @ w_up[e])
        y = y + comb[..., e:e + 1] * (h @ w_down[e])
    return y


def hybrid_layer(x, cx, c, c_ctx, mod_w, mod_b, norm_mix, norm_ffn, w_in, hy, s5, rw, m2,
                 w_branch, w_out, moe, rows, ctx_rows, ctx_out):
    mod_x = jax.nn.silu(c) @ mod_w + mod_b
    mod_c = jax.nn.silu(c_ctx) @ mod_w + mod_b
    sh1, sc1, g1, sh2, sc2, g2 = jnp.split(mod_x[:, None, :], N_MOD, axis=-1)
    csh1, csc1, cg1, csh2, csc2, cg2 = jnp.split(mod_c, N_MOD, axis=-1)

    px = modulate(x, norm_mix, sh1, sc1) @ w_in
    pc = modulate(cx, norm_mix, csh1, csc1) @ w_in
    sizes = [HY_COLS, S5_COLS, RW_COLS, M2_COLS, GATE_COLS]
    hy_x, s5_x, rw_x, m2_x, gate_x = _split(px, sizes)
    hy_c, s5_c, rw_c, m2_c, gate_c = _split(pc, sizes)

    b_ = x.shape[0]
    s5_0 = jnp.zeros((2, b_, S5_GROUPS, S5_STATE), jnp.complex64)
    rw_0 = jnp.zeros((2, b_, RW_HEADS, RW_HEAD, RW_HEAD), F32)
    m2_0 = jnp.zeros((2, b_, M2_HEADS, M2_HEADDIM, M2_STATE), F32)

    y_s5c, s5_h = s5_mixer(s5_c, s5_0, *s5)
    y_rwc, rw_h = rwkv_mixer(rw_c, rw_0, *rw)
    y_m2c, m2_h = mamba_mixer(m2_c, m2_0, ctx_rows, *m2)

    ys_x = [hyena_mixer(hy_x, *hy),
            s5_mixer(s5_x, s5_h, *s5)[0],
            rwkv_mixer(rw_x, rw_h, *rw)[0],
            mamba_mixer(m2_x, m2_h, rows, *m2)[0]]
    x = x + g1 * merge(ys_x, gate_x, w_branch, w_out)
    x = x + g2 * moe_ffn(modulate(x, norm_ffn, sh2, sc2), *moe)

    if ctx_out:
        ys_c = [hyena_mixer(hy_c, *hy), y_s5c, y_rwc, y_m2c]
        cx = cx + cg1 * merge(ys_c, gate_c, w_branch, w_out)
        cx = cx + cg2 * moe_ffn(modulate(cx, norm_ffn, csh2, csc2), *moe)
    return x, cx


def setup_inputs(seed: int = 0) -> dict:
    key = jax.random.key(seed)
    ks = iter(jax.random.split(key, 64))
    nrm = lambda shape, s=1.0: s * jax.random.normal(next(ks), shape, F32)
    uni = lambda shape, lo, hi: jax.random.uniform(next(ks), shape, F32, lo, hi)
    Ld = DEPTH
    inp = {}
    inp['x'] = nrm((BATCH, SEQ, D_MODEL))
    inp['c'] = nrm((BATCH, D_MODEL))
    inp['ctx'] = nrm((BATCH, CTX_LEN, D_MODEL))
    inp['c_ctx'] = nrm((D_MODEL,))
    inp['mod_w'] = nrm((Ld, D_MODEL, N_MOD * D_MODEL), 0.02)
    inp['mod_b'] = nrm((Ld, N_MOD * D_MODEL), 0.02)
    inp['norm_mix'] = 1.0 + nrm((Ld, D_MODEL), 0.02)
    inp['norm_ffn'] = 1.0 + nrm((Ld, D_MODEL), 0.02)
    inp['w_in'] = nrm((Ld, D_MODEL, IN_COLS), D_MODEL ** -0.5)
    inp['hy_conv_w'] = nrm((Ld, HY_SHORT, HY_COLS), 0.5)
    inp['hy_conv_b'] = nrm((Ld, HY_COLS), 0.02)
    inp['hy_f_w1'] = nrm((Ld, HY_EMB, HY_HID), HY_EMB ** -0.5)
    inp['hy_f_b1'] = nrm((Ld, HY_HID), 0.1)
    inp['hy_f_w2'] = nrm((Ld, HY_HID, HY_HID), HY_HID ** -0.5)
    inp['hy_f_b2'] = nrm((Ld, HY_HID), 0.1)
    inp['hy_f_w3'] = nrm((Ld, HY_HID, 2 * W_BR), 0.02)
    inp['hy_f_freq'] = 1.0 + nrm((Ld, HY_HID), 0.1)
    inp['hy_bias'] = nrm((Ld, W_BR), 0.5)
    inp['s5_lam_re'] = -0.5 + nrm((Ld, 2, S5_GROUPS, S5_STATE), 0.01)
    inp['s5_lam_im'] = math.pi * jnp.arange(S5_STATE, dtype=F32) + nrm((Ld, 2, S5_GROUPS, S5_STATE), 0.01)
    inp['s5_log_step'] = uni((Ld, 2, S5_GROUPS), math.log(1e-3), math.log(1e-1))
    inp['s5_b_re'] = nrm((Ld, 2, S5_GROUPS, S5_STATE, S5_GROUP), (2 * S5_GROUP) ** -0.5)
    inp['s5_b_im'] = nrm((Ld, 2, S5_GROUPS, S5_STATE, S5_GROUP), (2 * S5_GROUP) ** -0.5)
    inp['s5_c_re'] = nrm((Ld, 2, S5_GROUPS, S5_GROUP, S5_STATE), S5_STATE ** -0.5)
    inp['s5_c_im'] = nrm((Ld, 2, S5_GROUPS, S5_GROUP, S5_STATE), S5_STATE ** -0.5)
    inp['s5_d'] = nrm((Ld, W_BR))
    inp['s5_w_glu'] = nrm((Ld, W_BR, W_BR), W_BR ** -0.5)
    ramp = jnp.arange(W_BR, dtype=F32) / (W_BR - 1)
    inp['rw_mu'] = uni((Ld, RW_COLS), 0.0, 1.0)
    inp['rw_w0'] = -6.0 + 5.0 * ramp ** 0.7 + nrm((Ld, 2, W_BR), 0.1)
    inp['rw_w2'] = nrm((Ld, 2, RW_W_RANK, W_BR), 0.1)
    inp['rw_a0'] = nrm((Ld, 2, W_BR), 0.1)
    inp['rw_a2'] = nrm((Ld, 2, RW_A_RANK, W_BR), 0.1)
    inp['rw_g2'] = nrm((Ld, RW_G_RANK, W_BR), RW_G_RANK ** -0.5)
    inp['rw_k_k'] = 0.85 + nrm((Ld, W_BR), 0.02)
    inp['rw_k_a'] = 1.0 + nrm((Ld, W_BR), 0.02)
    inp['rw_r_k'] = nrm((Ld, RW_HEADS, RW_HEAD), 0.1)
    inp['rw_ln_w'] = 1.0 + nrm((Ld, W_BR), 0.02)
    inp['rw_ln_b'] = nrm((Ld, W_BR), 0.02)
    inp['m2_conv_w'] = nrm((Ld, M2_CONV, M2_XBC), 0.5)
    inp['m2_conv_b'] = nrm((Ld, M2_XBC), 0.02)
    inp['m2_a_log'] = jnp.log(uni((Ld, 2, M2_HEADS), 1.0, 16.0))
    dt0 = jnp.exp(uni((Ld, 2, M2_HEADS), math.log(1e-3), math.log(1e-1)))
    inp['m2_dt_bias'] = dt0 + jnp.log(-jnp.expm1(-dt0))
    inp['m2_d'] = 1.0 + nrm((Ld, M2_HEADS), 0.1)
    inp['m2_norm_w'] = 1.0 + nrm((Ld, W_BR), 0.02)
    inp['w_branch'] = nrm((Ld, N_BRANCH, W_BR, D_MODEL), W_BR ** -0.5)
    inp['w_out'] = nrm((Ld, D_MODEL, D_MODEL), D_MODEL ** -0.5)
    inp['moe_w_group'] = nrm((Ld, D_MODEL, MOE_GROUPS), D_MODEL ** -0.5)
    inp['moe_w_expert'] = nrm((Ld, MOE_GROUPS, D_MODEL, MOE_PER_GROUP), D_MODEL ** -0.5)
    inp['moe_w_gate'] = nrm((Ld, MOE_EXPERTS, D_MODEL, MOE_FF), D_MODEL ** -0.5)
    inp['moe_w_up'] = nrm((Ld, MOE_EXPERTS, D_MODEL, MOE_FF), D_MODEL ** -0.5)
    inp['moe_w_down'] = nrm((Ld, MOE_EXPERTS, MOE_FF, D_MODEL), MOE_FF ** -0.5)
    inp['norm_final'] = 1.0 + nrm((D_MODEL,), 0.02)
    return inp


def reference(x, c, ctx, c_ctx, mod_w, mod_b, norm_mix, norm_ffn, w_in,
              hy_conv_w, hy_conv_b, hy_f_w1, hy_f_b1, hy_f_w2, hy_f_b2, hy_f_w3, hy_f_freq, hy_bias,
              s5_lam_re, s5_lam_im, s5_log_step, s5_b_re, s5_b_im, s5_c_re, s5_c_im, s5_d, s5_w_glu,
              rw_mu, rw_w0, rw_w2, rw_a0, rw_a2, rw_g2, rw_k_k, rw_k_a, rw_r_k, rw_ln_w, rw_ln_b,
              m2_conv_w, m2_conv_b, m2_a_log, m2_dt_bias, m2_d, m2_norm_w,
              w_branch, w_out,
              moe_w_group, moe_w_expert, moe_w_gate, moe_w_up, moe_w_down,
              norm_final):
    rows = x.shape[1] // GRID_W
    ctx_rows = ctx.shape[1] // GRID_W
    cx = ctx
    for l in range(DEPTH):
        hy = (hy_conv_w[l], hy_conv_b[l], hy_f_w1[l], hy_f_b1[l], hy_f_w2[l], hy_f_b2[l],
              hy_f_w3[l], hy_f_freq[l], hy_bias[l])
        s5 = (s5_lam_re[l], s5_lam_im[l], s5_log_step[l], s5_b_re[l], s5_b_im[l],
              s5_c_re[l], s5_c_im[l], s5_d[l], s5_w_glu[l])
        rw = (rw_mu[l], rw_w0[l], rw_w2[l], rw_a0[l], rw_a2[l], rw_g2[l], rw_k_k[l], rw_k_a[l],
              rw_r_k[l], rw_ln_w[l], rw_ln_b[l])
        m2 = (m2_conv_w[l], m2_conv_b[l], m2_a_log[l], m2_dt_bias[l], m2_d[l], m2_norm_w[l])
        moe = (moe_w_group[l], moe_w_expert[l], moe_w_gate[l], moe_w_up[l], moe_w_down[l])
        x, cx = hybrid_layer(x, cx, c, c_ctx, mod_w[l], mod_b[l], norm_mix[l], norm_ffn[l], w_in[l],
                             hy, s5, rw, m2, w_branch[l], w_out[l], moe, rows, ctx_rows, l < DEPTH - 1)
    return rmsnorm(x, norm_final)
```

```python
import contextlib
import math
import numpy as np
import ml_dtypes
import concourse.bass as bass
import concourse.mybir as mybir
from concourse.bass_utils import run_bass_kernel_spmd

F32 = mybir.dt.float32
BF16 = mybir.dt.bfloat16
AF = mybir.ActivationFunctionType
ALU = mybir.AluOpType
AX = mybir.AxisListType

D = 1024
LAT = 4096
CTX = 256
T = LAT + CTX
DEPTH = 2
IN_COLS = 7112
NG_COLS = 3016
EPS = 1e-6
SEGS = ((0, CTX), (CTX, T))
BLOCKS = [(0, CTX)] + [(CTX + i * 512, 512) for i in range(8)]
LAST_BLOCKS = [(CTX + i * 512, 512) for i in range(4)]
LOUT = 2048

COMPUTE = ("tensor", "vector", "scalar", "gpsimd")
DMAQ = ("sync", "gpsimd", "scalar")
NSLOT = 12
EPOCH = 30000
NEPOCH = 12


def _key(k):
    if isinstance(k, (str, tuple, int)):
        return k
    t = getattr(k, "tensor", None)
    if t is not None:
        return t.name
    return getattr(k, "name", None) or id(k)


F32R_ON = [True]


class Prog:
    def __init__(self, nc, st):
        self.nc = nc
        self.q = {e: [] for e in ("tensor", "vector", "scalar", "gpsimd", "sync")}
        self.ccnt = {e: 0 for e in COMPUTE}
        self.dcnt = {e: 0 for e in DMAQ}
        self.dslot_tok = {}
        self.waited = {e: {} for e in self.q}
        self.lastw = {}
        self.readers = {}
        self.n_inst = 0
        self.sems = {}
        for e in COMPUTE:
            for i in range(NEPOCH):
                sn = "c_%s_%d" % (e, i)
                self.sems[sn] = st.enter_context(nc.semaphore(sn))
        for qn in DMAQ:
            for i in range(NSLOT):
                sn = "d_%s_%d" % (qn, i)
                self.sems[sn] = st.enter_context(nc.semaphore(sn))

    def _deps(self, reads, writes):
        toks = []
        for k in reads:
            t = self.lastw.get(k)
            if t is not None:
                toks.append(t)
        for k in writes:
            t = self.lastw.get(k)
            if t is not None:
                toks.append(t)
            toks.extend(self.readers.get(k, ()))
        return toks

    def _emit_waits(self, eng, toks, is_dma_issue):
        best = {}
        for (sn, val, e, isd) in toks:
            if (not isd) and e == eng and eng == "tensor" and not is_dma_issue:
                continue
            if self.waited[eng].get(sn, 0) >= val:
                continue
            if best.get(sn, 0) < val:
                best[sn] = val
        for sn, val in best.items():
            self.waited[eng][sn] = val
            self.q[eng].append(("wait", sn, val))

    def _record(self, tok, reads, writes):
        for k in writes:
            self.lastw[k] = tok
            self.readers[k] = []
        for k in reads:
            lst = self.readers.setdefault(k, [])
            lst.append(tok)
            if len(lst) > 16:
                d = {}
                for t in lst:
                    if d.get(t[0], (0, 0))[1] < t[1]:
                        d[t[0]] = t
                self.readers[k] = list(d.values())

    def op(self, eng, fn, reads=(), writes=()):
        reads = [_key(k) for k in reads if k is not None and not isinstance(k, (float,))]
        writes = [_key(k) for k in writes]
        toks = self._deps(reads, writes)
        self._emit_waits(eng, toks, False)
        c = self.ccnt[eng]
        self.ccnt[eng] += 1
        sn = "c_%s_%d" % (eng, c // EPOCH)
        tok = (sn, c % EPOCH + 1, eng, False)
        self.q[eng].append(("op", fn, sn, 1))
        self._record(tok, reads, writes)
        self.n_inst += 1
        return tok

    def dma(self, q, out, in_, rk=None, wk=None, **kw):
        reads = [_key(in_) if rk is None else rk]
        writes = [_key(out) if wk is None else wk]
        i = self.dcnt[q]
        self.dcnt[q] += 1
        slot = i % NSLOT
        sn = "d_%s_%d" % (q, slot)
        val = 16 * (i // NSLOT + 1)
        toks = self._deps(reads, writes)
        prev = self.dslot_tok.get(sn)
        if prev is not None:
            toks.append(prev)
        self._emit_waits(q, toks, True)
        tok = (sn, val, q, True)
        self.dslot_tok[sn] = tok
        self.q[q].append(("op", lambda e, o=out, s=in_, k=kw: e.dma_start(out=o, in_=s, **k), sn, 16))
        self._record(tok, reads, writes)
        self.n_inst += 1
        return tok

    def flush(self):
        for qn in ("sync", "gpsimd", "scalar"):
            toks = [t for t in self.dslot_tok.values() if t[2] == qn]
            self._emit_waits(qn, toks, True)
        last = []
        for e in COMPUTE:
            c = self.ccnt[e]
            if c > 0:
                last.append(("c_%s_%d" % (e, (c - 1) // EPOCH), (c - 1) % EPOCH + 1, e, False))
        for e in self.q:
            self._emit_waits(e, [t for t in last if t[2] != e] + list(self.dslot_tok.values()), True)
        nc = self.nc
        sems = self.sems
        with nc.Block() as block:
            def run(engname):
                items = self.q[engname]

                def body(e):
                    for it in items:
                        if it[0] == "wait":
                            e.wait_ge(sems[it[1]], it[2])
                        else:
                            it[1](e).then_inc(sems[it[2]], it[3])
                return body
            block.sync(run("sync"))
            block.scalar(run("scalar"))
            block.vector(run("vector"))
            block.gpsimd(run("gpsimd"))
            block.tensor(run("tensor"))
        self.q = {e: [] for e in self.q}

    def mm(self, out, lhsT, rhs, start=True, stop=True, r=False):
        return self.op("tensor", lambda e: e.matmul(out, lhsT, rhs, start=start, stop=stop),
                       reads=[lhsT, rhs], writes=[out])

    def tr(self, out, in_, ident):
        return self.op("tensor", lambda e: e.transpose(out, in_, ident), reads=[in_, ident], writes=[out])

    def act(self, out, in_, func, bias=None, scale=None, accum_out=None, eng="scalar"):
        kw = {}
        rd = [in_]
        if bias is not None:
            kw["bias"] = bias
            if not isinstance(bias, float):
                rd.append(bias)
        if scale is not None:
            kw["scale"] = scale
            if not isinstance(scale, float):
                rd.append(scale)
        wr = [out]
        if accum_out is not None:
            kw["accum_out"] = accum_out
            wr.append(accum_out)
        return self.op("scalar", lambda e: e.activation(out, in_, func, **kw), reads=rd, writes=wr)

    def tt(self, out, in0, in1, op, eng="vector"):
        return self.op(eng, lambda e: e.tensor_tensor(out=out, in0=in0, in1=in1, op=op), reads=[in0, in1], writes=[out])

    def ts(self, out, in0, s1, s2=None, op0=ALU.mult, op1=None, eng="vector", accum_out=None):
        rd = [in0] + [s for s in (s1, s2) if s is not None and not isinstance(s, (float, int))]
        kw = {}
        if op1 is not None:
            kw["op1"] = op1
        wr = [out]
        if accum_out is not None:
            kw["accum_out"] = accum_out
            wr.append(accum_out)
        return self.op(eng, lambda e: e.tensor_scalar(out=out, in0=in0, scalar1=s1, scalar2=s2, op0=op0, **kw), reads=rd, writes=wr)

    def stt(self, out, in0, scalar, in1, op0, op1):
        rd = [in0, in1] + ([scalar] if not isinstance(scalar, (float, int)) else [])
        return self.op("vector", lambda e: e.scalar_tensor_tensor(out=out, in0=in0, scalar=scalar, in1=in1, op0=op0, op1=op1),
                       reads=rd, writes=[out])

    def copy(self, out, in_, eng="vector"):
        if eng == "scalar":
            return self.op("scalar", lambda e: e.copy(out, in_), reads=[in_], writes=[out])
        return self.op(eng, lambda e: e.tensor_copy(out=out, in_=in_), reads=[in_], writes=[out])

    def memset(self, ap, v, eng="vector"):
        return self.op(eng, lambda e: e.memset(ap, v), reads=[], writes=[ap])

    def recip(self, out, in_):
        return self.op("vector", lambda e: e.reciprocal(out=out, in_=in_), reads=[in_], writes=[out])

    def reduce(self, out, in_, op, axis=AX.X):
        return self.op("vector", lambda e: e.tensor_reduce(out=out, in_=in_, axis=axis, op=op), reads=[in_], writes=[out])


class Ctx:
    pass


_UID = [0]


def sb(st, nc, name, shape, dt=F32):
    _UID[0] += 1
    return st.enter_context(nc.sbuf_tensor("s%d_%s" % (_UID[0], name), list(shape), dt))


def ps(st, nc, name, shape, dt=F32):
    _UID[0] += 1
    return st.enter_context(nc.psum_tensor("p%d_%s" % (_UID[0], name), list(shape), dt))


def set_layer_weights(g, l):
    g.win16, g.wout16, g.wbr16 = g.win16_all[l], g.wout16_all[l], g.wbr16_all[l]
    g.wg16, g.wu16, g.wd16 = g.wg16_all[l], g.wu16_all[l], g.wd16_all[l]


def stage_precast(P, nc, g, l, flush=True):
    set_layer_weights(g, l)
    todo = []
    for r in range(8):
        todo.append((g.win16[r * 128:(r + 1) * 128, :], g.w_in[l, r * 128:(r + 1) * 128, :], ("win16", l, r)))
        todo.append((g.wout16[r * 128:(r + 1) * 128, :], g.w_out[l, r * 128:(r + 1) * 128, :], ("wout16", l, r)))
        todo.append((g.wbr16[r * 128:(r + 1) * 128, :],
                     g.w_branch[l].rearrange("i k n -> (i k) n")[r * 128:(r + 1) * 128, :], ("wbr16", l, r)))
    for e in range(16):
        for r in range(8):
            todo.append((g.wg16[e, r * 128:(r + 1) * 128, :], g.moe_w_gate[l, e, r * 128:(r + 1) * 128, :], ("wg16", l, e, r)))
            todo.append((g.wu16[e, r * 128:(r + 1) * 128, :], g.moe_w_up[l, e, r * 128:(r + 1) * 128, :], ("wu16", l, e, r)))
        for r in range(4):
            todo.append((g.wd16[e, r * 128:(r + 1) * 128, :], g.moe_w_down[l, e, r * 128:(r + 1) * 128, :], ("wd16", l, e, r)))
    if not flush:
        return todo
    for (o, i, k) in todo[:24]:
        P.dma("gpsimd", o, i, wk=k)
    P.flush()
    g.pending_bg = todo[24:]
    return []


def stage_mod(P, nc, g, l):
    with contextlib.ExitStack() as st:
        c2 = sb(st, nc, "c2", [128, 8, 2])
        sc2 = sb(st, nc, "sc2", [128, 8, 2])
        mb = sb(st, nc, "mb", [128, 48])
        mw = [sb(st, nc, "mw%d" % i, [128, 8, 1024]) for i in range(2)]
        pm = ps(st, nc, "pm", [128, 48, 2])
        P.dma("sync", c2[:], g.c2)
        P.dma("sync", mb[:], g.mod_b[l])
        P.act(sc2[:], c2[:], AF.Silu)
        for jb in range(6):
            w = mw[jb % 2]
            P.dma("sync", w[:], g.mod_w[l].rearrange("(kc p) n -> p kc n", p=128)[:, :, jb * 1024:(jb + 1) * 1024])
            for j in range(8):
                for kc in range(8):
                    P.mm(pm[:, jb * 8 + j, :], w[:, kc, j * 128:(j + 1) * 128], sc2[:, kc, :], start=(kc == 0), stop=(kc == 7))
        M = g.MODT
        for s in range(2):
            P.tt(M[:, s, :], pm[:, :, s], mb[:], ALU.add)
        P.dma("sync", g.NMIX[:], g.norm_mix[l])
        P.dma("sync", g.NFFN[:], g.norm_ffn[l])
        for s in range(2):
            P.stt(g.S1[:, s, :], M[:, s, 8:16], 1.0, g.NMIX[:], ALU.add, ALU.mult)
            P.stt(g.S2[:, s, :], M[:, s, 32:40], 1.0, g.NFFN[:], ALU.add, ALU.mult)
        P.flush()


def modulate_block(P, nc, g, xb, t0, n, s, scale, shift, out_bf, sq, pss, rs, tmp, out_f32=None):
    P.act(sq[:, :, :n], xb[:, :, :n], AF.Square)
    for fc in range(8):
        P.mm(pss[:, :n], g.ONES[:], sq[:, fc, :n], start=(fc == 0), stop=(fc == 7))
    P.act(rs[:, :n], pss[:, :n], AF.Sqrt, bias=g.EPSC[:, 0:1], scale=1.0 / D)
    P.recip(rs[:, :n], rs[:, :n])
    for fc in range(8):
        P.tt(tmp[:, :n], xb[:, fc, :n], rs[:, :n], ALU.mult)
        sh = shift[:, fc:fc + 1] if shift is not None else 0.0
        if out_f32 is not None:
            P.act(out_f32[:, fc, :n], tmp[:, :n], AF.Identity, bias=sh, scale=scale[:, fc:fc + 1])
            P.copy(out_bf[:, fc, t0:t0 + n], out_f32[:, fc, :n], eng="gpsimd")
        else:
            P.act(out_bf[:, fc, t0:t0 + n], tmp[:, :n], AF.Identity, bias=sh, scale=scale[:, fc:fc + 1])


def stage_proj(P, nc, g, l, xsrc):
    with contextlib.ExitStack() as st0:
      g.XM = sb(st0, nc, "XM", [128, 8, T], BF16)
      with contextlib.ExitStack() as st:
        xb = [sb(st, nc, "xb%d" % i, [128, 8, 512]) for i in range(2)]
        sq = sb(st, nc, "sq", [128, 8, 512])
        rs = sb(st, nc, "rs", [128, 512])
        tmp = sb(st, nc, "tmp", [128, 512])
        pss = ps(st, nc, "pss", [128, 512])
        xv = xsrc.rearrange("(fc p) t -> p fc t", p=128)
        for bi, (t0, n) in enumerate(BLOCKS):
            s = 1 if t0 < CTX else 0
            x = xb[bi % 2]
            P.dma("sync", x[:, :, :n], xv[:, :, t0:t0 + n])
            modulate_block(P, nc, g, x, t0, n, s, g.S1[:, s, :], g.MODT[:, s, 0:8], g.XM, sq, pss, rs, tmp)
        P.dma("sync", g.XM16.rearrange("(fc p) t -> p fc t", p=128), g.XM[:])
        P.flush()
      with contextlib.ExitStack() as st:
          wb = [sb(st, nc, "wb%d" % i, [128, 8, 128], BF16) for i in range(2)]
          ob = [sb(st, nc, "ob%d" % i, [128, T]) for i in range(2)]
          pp = [ps(st, nc, "pp%d" % i, [128, 512]) for i in range(4)]
          wv = g.win16.rearrange("(fc p) n -> p fc n", p=128)
          ci = 0
          k = 0
          for c0 in range(0, NG_COLS, 128):
              m = min(128, NG_COLS - c0)
              w = wb[ci % 2]
              o = ob[ci % 2]
              P.dma("sync", w[:, :, :m], wv[:, :, c0:c0 + m], rk=("win16",))
              for (t0, n) in BLOCKS:
                  p_ = pp[k % 4]
                  k += 1
                  for fc in range(8):
                      P.mm(p_[:m, :n], w[:, fc, :m], g.XM[:, fc, t0:t0 + n], start=(fc == 0), stop=(fc == 7))
                  if k % 2:
                      P.copy(o[:m, t0:t0 + n], p_[:m, :n], eng="scalar")
                  else:
                      P.copy(o[:m, t0:t0 + n], p_[:m, :n], eng="vector")
              P.dma("sync", g.PX[c0:c0 + m, :], o[:m, :], wk=("PX", c0 // 128))
              ci += 1
          P.flush()


def conv3(P, y, x, w, b):
    P.ts(y[:], x[:], w[:, 1:2], b, op0=ALU.mult, op1=ALU.add)
    for (a, e) in SEGS:
        P.stt(y[:, a + 1:e], x[:, a:e - 1], w[:, 0:1], y[:, a + 1:e], ALU.mult, ALU.add)
        P.stt(y[:, a:e - 1], x[:, a + 1:e], w[:, 2:3], y[:, a:e - 1], ALU.mult, ALU.add)


def sin_wrap(P, out, tmp, m, pz, fr, frb):
    P.ts(tmp, pz, fr, frb, op0=ALU.mult, op1=ALU.add)
    P.ts(m, tmp, math.pi, -2.0 * math.pi, op0=ALU.is_gt, op1=ALU.mult)
    P.tt(out, tmp, m, ALU.add)
    P.ts(m, tmp, -math.pi, 2.0 * math.pi, op0=ALU.is_lt, op1=ALU.mult)
    P.tt(out, out, m, ALU.add)
    P.act(out, out, AF.Sin)


def hyena_seq(P, nc, g, l, L, toff, UT):
    nt = L // 128
    nb = min(512, L)
    kq = min(8, nt)
    nq = nt // kq
    tabs = g.hy_tabs[L]
    with contextlib.ExitStack() as st:
        HR = sb(st, nc, "HR", [128, nt, 256])
        HI = sb(st, nc, "HI", [128, nt, 256])
        with contextlib.ExitStack() as st2:
            HS = sb(st2, nc, "HS", [128, nt, 256], BF16)
            HD = sb(st2, nc, "HD", [128, nt, 256], BF16)
            with contextlib.ExitStack() as st3:
                feats = sb(st3, nc, "feats", [17, L])
                w1 = sb(st3, nc, "fw1", [17, 64])
                w2 = sb(st3, nc, "fw2", [64, 64])
                w3 = sb(st3, nc, "fw3", [64, 512])
                fv = sb(st3, nc, "fv", [64, 6])
                h1 = sb(st3, nc, "fh1", [64, L])
                h2 = sb(st3, nc, "fh2", [64, L])
                tmp = sb(st3, nc, "ftmp", [64, 512])
                mm_ = sb(st3, nc, "fm", [64, 512])
                dec = [sb(st3, nc, "fdec%d" % i, [128, 256]) for i in range(2)]
                hf = sb(st3, nc, "fhf", [128, 256])
                hb = sb(st3, nc, "fhb", [128, 256])
                pz = ps(st3, nc, "fpz", [64, 512])
                ph = [ps(st3, nc, "fph%d" % i, [128, 512]) for i in range(2)]
                lag0 = sb(st3, nc, "flag0", [1, 2])
                P.dma("sync", lag0[:], g.hy_lag0)
                P.dma("sync", feats[:], tabs["feats"])
                P.dma("sync", w1[:], g.hy_f_w1[l])
                P.dma("sync", w2[:], g.hy_f_w2[l])
                P.dma("sync", w3[:], g.hy_f_w3[l])
                P.dma("sync", fv[:, 0:3], g.hy_fvec[l])
                P.tt(fv[:, 3:4], fv[:, 2:3], fv[:, 0:1], ALU.mult)
                P.tt(fv[:, 4:5], fv[:, 2:3], fv[:, 1:2], ALU.mult)
                for b0 in range(0, L, 512):
                    n = min(512, L - b0)
                    P.mm(pz[:, :n], w1[:], feats[:, b0:b0 + n])
                    sin_wrap(P, h1[:, b0:b0 + n], tmp[:, :n], mm_[:, :n], pz[:, :n], fv[:, 2:3], fv[:, 3:4])
                for b0 in range(0, L, 512):
                    n = min(512, L - b0)
                    P.mm(pz[:, :n], w2[:], h1[:, b0:b0 + n])
                    sin_wrap(P, h2[:, b0:b0 + n], tmp[:, :n], mm_[:, :n], pz[:, :n], fv[:, 2:3], fv[:, 4:5])
                for lt in range(nt):
                    p_ = ph[lt % 2]
                    d_ = dec[lt % 2]
                    P.mm(p_[:], h2[:, lt * 128:(lt + 1) * 128], w3[:])
                    P.dma("sync", d_[:], tabs["dec"][lt * 128:(lt + 1) * 128, :])
                    P.tt(hf[:], p_[:, 0:256], d_[:], ALU.mult)
                    P.tt(hb[:], p_[:, 256:512], d_[:], ALU.mult)
                    if lt == 0:
                        P.ts(hf[0:1, :], hf[0:1, :], lag0[0:1, 0:1], None, op0=ALU.mult)
                        P.ts(hb[0:1, :], hb[0:1, :], lag0[0:1, 1:2], None, op0=ALU.mult)
                    P.tt(HS[:, lt, :], hf[:], hb[:], ALU.add)
                    P.tt(HD[:, lt, :], hf[:], hb[:], ALU.subtract)
                P.flush()
            with contextlib.ExitStack() as st3:
                ct = [sb(st3, nc, "ct%d" % i, [128, nt, 128], BF16) for i in range(2)]
                sn = [sb(st3, nc, "sn%d" % i, [128, nt, 128], BF16) for i in range(2)]
                pr = [ps(st3, nc, "hpr%d" % i, [128, 512]) for i in range(2)]
                pi = [ps(st3, nc, "hpi%d" % i, [128, 512]) for i in range(2)]
                for kt in range(nt):
                    c_, s_ = ct[kt % 2], sn[kt % 2]
                    P.dma("sync", c_[:], tabs["cf"][kt])
                    P.dma("sync", s_[:], tabs["sf"][kt])
                    for dc in range(nt):
                        P.mm(pr[kt % 2][:, :256], c_[:, dc, :], HS[:, dc, :], start=(dc == 0), stop=(dc == nt - 1))
                    for dc in range(nt):
                        P.mm(pi[kt % 2][:, :256], s_[:, dc, :], HD[:, dc, :], start=(dc == 0), stop=(dc == nt - 1))
                    P.copy(HR[:, kt, :], pr[kt % 2][:, :256], eng="scalar")
                    P.copy(HI[:, kt, :], pi[kt % 2][:, :256], eng="vector")
                P.flush()
        YR = sb(st, nc, "YR", [128, nt, 256], BF16)
        YI = sb(st, nc, "YI", [128, nt, 256], BF16)
        with contextlib.ExitStack() as st3:
            ct = [sb(st3, nc, "uct%d" % i, [128, nt, 128], BF16) for i in range(2)]
            sn = [sb(st3, nc, "usn%d" % i, [128, nt, 128], BF16) for i in range(2)]
            t1 = sb(st3, nc, "ut1", [128, 256])
            t2 = sb(st3, nc, "ut2", [128, 256])
            pr = [ps(st3, nc, "upr%d" % i, [128, 512]) for i in range(2)]
            pi = [ps(st3, nc, "upi%d" % i, [128, 512]) for i in range(2)]
            tc0 = toff // 128
            for kt in range(nt):
                c_, s_ = ct[kt % 2], sn[kt % 2]
                P.dma("sync", c_[:], tabs["cf"][kt])
                P.dma("sync", s_[:], tabs["sf"][kt])
                a_, b_ = pr[kt % 2], pi[kt % 2]
                for dc in range(nt):
                    P.mm(a_[:, :256], c_[:, dc, :], UT[:, tc0 + dc, :], start=(dc == 0), stop=(dc == nt - 1))
                for dc in range(nt):
                    P.mm(b_[:, :256], s_[:, dc, :], UT[:, tc0 + dc, :], start=(dc == 0), stop=(dc == nt - 1))
                P.tt(t1[:], a_[:, :256], HR[:, kt, :], ALU.mult)
                P.tt(t2[:], b_[:, :256], HI[:, kt, :], ALU.mult)
                P.tt(YR[:, kt, :], t1[:], t2[:], ALU.subtract)
                P.tt(t1[:], a_[:, :256], HI[:, kt, :], ALU.mult)
                P.tt(t2[:], b_[:, :256], HR[:, kt, :], ALU.mult)
                P.tt(YI[:, kt, :], t1[:], t2[:], ALU.add)
            P.flush()
        with contextlib.ExitStack() as st3:
            ci = [sb(st3, nc, "ci%d" % i, [128, kq, nb], BF16) for i in range(2)]
            si = [sb(st3, nc, "si%d" % i, [128, kq, nb], BF16) for i in range(2)]
            ub = sb(st3, nc, "iub", [128, nb])
            x0 = sb(st3, nc, "ix0", [128, nb])
            yo = sb(st3, nc, "iyo", [128, nb])
            hbias = sb(st3, nc, "ihb", [128, 2])
            py = [ps(st3, nc, "ipy%d" % i, [128, 512]) for i in range(2)]
            P.dma("sync", hbias[:], g.hy_bias[l])
            kk = 0
            for tb in range((LOUT // nb) if (l == DEPTH - 1 and L == LAT) else (L // nb)):
                for q in range(nq):
                    c_, s_ = ci[kk % 2], si[kk % 2]
                    kk += 1
                    P.dma("sync", c_[:], tabs["ci"][tb, q])
                    P.dma("sync", s_[:], tabs["si"][tb, q])
                    for k2 in range(kq):
                        kc = q * kq + k2
                        for cj in range(2):
                            P.mm(py[cj][:, :nb], YR[:, kc, cj * 128:(cj + 1) * 128], c_[:, k2, :], start=(kc == 0), stop=False)
                            P.mm(py[cj][:, :nb], YI[:, kc, cj * 128:(cj + 1) * 128], s_[:, k2, :], start=False, stop=(kc == nt - 1))
                for cj in range(2):
                    c0 = toff + tb * nb
                    P.dma("sync", ub[:], g.HU[cj * 128:(cj + 1) * 128, c0:c0 + nb], rk=("HU",))
                    P.dma("sync", x0[:], g.HX0[cj * 128:(cj + 1) * 128, c0:c0 + nb], rk=("HX0",))
                    P.ts(ub[:], ub[:], hbias[:, cj:cj + 1], None, op0=ALU.mult)
                    P.stt(yo[:], py[cj][:, :nb], 2.0 / (2 * L), ub[:], ALU.mult, ALU.add)
                    P.tt(yo[:], yo[:], x0[:], ALU.mult)
                    P.dma("sync", g.YS[cj * 128:(cj + 1) * 128, c0:c0 + nb], yo[:], wk=("YS", 0, cj, c0))
            P.flush()


def stage_hyena(P, nc, g, l):
    with contextlib.ExitStack() as st:
        UT = sb(st, nc, "UT", [128, T // 128, 256], BF16)
        with contextlib.ExitStack() as st2:
            cw = sb(st2, nc, "hcw", [128, 6, 3])
            cb = sb(st2, nc, "hcb", [128, 6])
            xi = [sb(st2, nc, "hxi%d" % i, [128, T]) for i in range(3)]
            y = [sb(st2, nc, "hyy%d" % i, [128, T]) for i in range(3)]
            pt = [ps(st2, nc, "hpt%d" % i, [128, 128]) for i in range(2)]
            P.dma("sync", cw[:], g.hy_conv_w[l])
            P.dma("sync", cb[:], g.hy_conv_b[l])
            for j in range(2):
                for a in range(3):
                    ti = a * 2 + j
                    r0 = a * 256 + j * 128
                    P.dma("sync", xi[a][:], g.PX[r0:r0 + 128, :], rk=("PX", r0 // 128))
                    conv3(P, y[a], xi[a], cw[:, ti, :], cb[:, ti:ti + 1])
                P.tt(y[1][:], y[1][:], y[2][:], ALU.mult)
                P.dma("sync", g.HX0[j * 128:(j + 1) * 128, :], y[0][:], wk=("HX0",))
                P.dma("sync", g.HU[j * 128:(j + 1) * 128, :], y[1][:], wk=("HU",))
                for tc in range(T // 128):
                    p_ = pt[tc % 2]
                    P.tr(p_[:], y[1][:, tc * 128:(tc + 1) * 128], g.IDENT[:])
                    P.copy(UT[:, tc, j * 128:(j + 1) * 128], p_[:], eng=("scalar" if tc % 2 else "vector"))
            P.flush()
        if l < DEPTH - 1:
            hyena_seq(P, nc, g, l, CTX, 0, UT)
        hyena_seq(P, nc, g, l, LAT, CTX, UT)


def issue_bg(P, g):
    for (o, i, k) in getattr(g, "pending_bg", []):
        P.dma("gpsimd", o, i, wk=k)
    g.pending_bg = []


def stage_s5(P, nc, g, l):
    Q = 512
    PI = math.pi
    issue_bg(P, g)
    with contextlib.ExitStack() as st:
        U = sb(st, nc, "s5U", [128, 2, T])
        YA = sb(st, nc, "s5YA", [128, 2, T])
        dsk = sb(st, nc, "s5d", [128, 2])
        ONQ = sb(st, nc, "s5on", [128, Q])
        COS = sb(st, nc, "s5cos", [128, Q])
        SIN = sb(st, nc, "s5sin", [128, Q])
        RHO = sb(st, nc, "s5rho", [128, Q])
        pv_ = [sb(st, nc, "s5pv%d" % i, [128, 3]) for i in range(2)]
        v = sb(st, nc, "s5v", [128, 24])
        m = sb(st, nc, "s5m", [128, 2])
        Bre_ = [sb(st, nc, "s5Bre%d" % i, [128, 128]) for i in range(2)]
        Bim_ = [sb(st, nc, "s5Bim%d" % i, [128, 128]) for i in range(2)]
        BR = sb(st, nc, "s5BR", [128, 128])
        BI = sb(st, nc, "s5BI", [128, 128])
        BRT = sb(st, nc, "s5BRT", [128, 128])
        BIT = sb(st, nc, "s5BIT", [128, 128])
        CRe_ = [sb(st, nc, "s5CRe%d" % i, [128, 128]) for i in range(2)]
        CIm_ = [sb(st, nc, "s5CIm%d" % i, [128, 128]) for i in range(2)]
        tq = sb(st, nc, "s5tq", [128, Q])
        t1 = sb(st, nc, "s5t1", [128, Q])
        t2 = sb(st, nc, "s5t2", [128, Q])
        t3 = sb(st, nc, "s5t3", [128, Q])
        t4 = sb(st, nc, "s5t4", [128, Q])
        t5 = sb(st, nc, "s5t5", [128, Q])
        t6 = sb(st, nc, "s5t6", [128, Q])
        Wr = sb(st, nc, "s5Wr", [128, Q])
        Wi = sb(st, nc, "s5Wi", [128, Q])
        Zr = sb(st, nc, "s5Zr", [128, Q])
        Zi = sb(st, nc, "s5Zi", [128, Q])
        XR = [sb(st, nc, "s5XR%d" % i, [128, Q]) for i in range(2)]
        XI = [sb(st, nc, "s5XI%d" % i, [128, Q]) for i in range(2)]
        z0 = [sb(st, nc, "s5z0%d" % i, [128, 2]) for i in range(2)]
        AS = [sb(st, nc, "s5AS%d" % i, [128, Q]) for i in range(2)]
        BS = [sb(st, nc, "s5BS%d" % i, [128, Q]) for i in range(2)]
        pt = ps(st, nc, "s5pt", [128, 128])
        pbr = [ps(st, nc, "s5pbr%d" % i, [128, 512]) for i in range(2)]
        pbi = [ps(st, nc, "s5pbi%d" % i, [128, 512]) for i in range(2)]
        py = [ps(st, nc, "s5py%d" % i, [128, 512]) for i in range(2)]
        for ut in range(2):
            P.dma("sync", U[:, ut, :], g.PX[768 + ut * 128:768 + (ut + 1) * 128, :], rk=("PX", 6 + ut))
        P.dma("sync", dsk[:], g.s5_d[l])
        P.memset(ONQ[:], 1.0)
        for ut in range(2):
            P.ts(YA[:, ut, :], U[:, ut, :], dsk[:, ut:ut + 1], None, op0=ALU.mult)
        kb = 0
        for d in range(2):
            for s_ in range(8):
                ut = s_ // 4
                pv, Bre, Bim, CRe, CIm = (t_[(d * 8 + s_) % 2] for t_ in (pv_, Bre_, Bim_, CRe_, CIm_))
                P.dma("sync", pv[:], g.s5_lam[l, d, s_])
                P.dma("sync", Bre[:], g.s5_bre[l, d, s_])
                P.dma("sync", Bim[:], g.s5_bim[l, d, s_])
                P.dma("sync", CRe[:], g.s5_cre[l, d, s_])
                P.dma("sync", CIm[:], g.s5_cim[l, d, s_])
                P.ts(CIm[:], CIm[:], -1.0, None, op0=ALU.mult)
                P.act(v[:, 0:1], pv[:, 2:3], AF.Exp)
                P.tt(v[:, 1:2], pv[:, 0:1], v[:, 0:1], ALU.mult)
                P.tt(v[:, 2:3], pv[:, 1:2], v[:, 0:1], ALU.mult)
                P.act(v[:, 3:4], v[:, 1:2], AF.Exp)
                P.copy(v[:, 4:5], v[:, 2:3])
                P.ts(v[:, 5:6], v[:, 2:3], PI / 2, None, op0=ALU.add)
                for _ in range(5):
                    P.ts(m[:], v[:, 4:6], PI, -2.0 * PI, op0=ALU.is_gt, op1=ALU.mult)
                    P.tt(v[:, 4:6], v[:, 4:6], m[:], ALU.add)
                P.act(v[:, 6:8], v[:, 4:6], AF.Sin)
                P.stt(v[:, 8:9], v[:, 3:4], v[:, 7:8], ONQ[:, 0:1], ALU.mult, ALU.subtract)
                P.tt(v[:, 9:10], v[:, 3:4], v[:, 6:7], ALU.mult)
                P.tt(v[:, 13:14], pv[:, 0:1], pv[:, 0:1], ALU.mult)
                P.stt(v[:, 10:11], pv[:, 1:2], pv[:, 1:2], v[:, 13:14], ALU.mult, ALU.add)
                P.recip(v[:, 10:11], v[:, 10:11])
                P.tt(v[:, 13:14], v[:, 9:10], pv[:, 1:2], ALU.mult)
                P.stt(v[:, 11:12], v[:, 8:9], pv[:, 0:1], v[:, 13:14], ALU.mult, ALU.add)
                P.tt(v[:, 11:12], v[:, 11:12], v[:, 10:11], ALU.mult)
                P.tt(v[:, 13:14], v[:, 8:9], pv[:, 1:2], ALU.mult)
                P.stt(v[:, 12:13], v[:, 9:10], pv[:, 0:1], v[:, 13:14], ALU.mult, ALU.subtract)
                P.tt(v[:, 12:13], v[:, 12:13], v[:, 10:11], ALU.mult)
                P.ts(BR[:], Bim[:], v[:, 12:13], None, op0=ALU.mult)
                P.stt(BR[:], Bre[:], v[:, 11:12], BR[:], ALU.mult, ALU.subtract)
                P.ts(BI[:], Bre[:], v[:, 12:13], None, op0=ALU.mult)
                P.stt(BI[:], Bim[:], v[:, 11:12], BI[:], ALU.mult, ALU.add)
                P.tr(pt[:], BR[:], g.IDENT[:])
                P.copy(BRT[:], pt[:], eng="scalar")
                P.tr(pt[:], BI[:], g.IDENT[:])
                P.copy(BIT[:], pt[:], eng="scalar")
                P.copy(COS[:, 0:1], v[:, 7:8])
                P.copy(SIN[:, 0:1], v[:, 6:7])
                mm_ = 1
                while mm_ < Q:
                    cm, sm = COS[:, mm_ - 1:mm_], SIN[:, mm_ - 1:mm_]
                    P.ts(tq[:, :mm_], SIN[:, 0:mm_], sm, None, op0=ALU.mult)
                    P.stt(COS[:, mm_:2 * mm_], COS[:, 0:mm_], cm, tq[:, :mm_], ALU.mult, ALU.subtract)
                    P.ts(tq[:, :mm_], COS[:, 0:mm_], sm, None, op0=ALU.mult)
                    P.stt(SIN[:, mm_:2 * mm_], SIN[:, 0:mm_], cm, tq[:, :mm_], ALU.mult, ALU.add)
                    mm_ *= 2
                P.ts(RHO[:], ONQ[:], v[:, 3:4], None, op0=ALU.mult)
                order = (BLOCKS[0:5] if l == DEPTH - 1 else BLOCKS) if d == 0 else [BLOCKS[0]] + BLOCKS[:0:-1]
                for bi, (t0, n) in enumerate(order):
                    a_, b_ = pbr[kb % 2], pbi[kb % 2]
                    xr, xi = XR[kb % 2], XI[kb % 2]
                    zin, zout = z0[kb % 2], z0[(kb + 1) % 2]
                    kb += 1
                    P.mm(a_[:, :n], BRT[:], U[:, ut, t0:t0 + n])
                    P.mm(b_[:, :n], BIT[:], U[:, ut, t0:t0 + n])
                    as_, bs_ = AS[kb % 2], BS[kb % 2]
                    P.copy(as_[:, :n], a_[:, :n], eng="scalar")
                    P.copy(bs_[:, :n], b_[:, :n], eng="scalar")
                    if d == 0:
                        av, bv = as_[:, :n], bs_[:, :n]
                        xrv, xiv = xr[:, :n], xi[:, :n]
                        last = n - 1
                    else:
                        av, bv = as_[:, 0:n][:, ::-1], bs_[:, 0:n][:, ::-1]
                        xrv, xiv = xr[:, 0:n][:, ::-1], xi[:, 0:n][:, ::-1]
                        last = 0
                    c_, s2 = COS[:, :n], SIN[:, :n]
                    P.tt(t1[:, :n], c_, av, ALU.mult)
                    P.tt(t2[:, :n], s2, bv, ALU.mult)
                    P.tt(t3[:, :n], c_, bv, ALU.mult)
                    P.tt(t4[:, :n], s2, av, ALU.mult)
                    P.tt(Wr[:, :n], t1[:, :n], t2[:, :n], ALU.add)
                    P.tt(Wi[:, :n], t3[:, :n], t4[:, :n], ALU.subtract)
                    ir = 0.0 if bi == 0 else zin[:, 0:1]
                    ii = 0.0 if bi == 0 else zin[:, 1:2]
                    P.op("vector", lambda e, o=Zr[:, :n], r=RHO[:, :n], w=Wr[:, :n], i0=ir: e.tensor_tensor_scan(out=o, data0=r, data1=w, initial=i0, op0=ALU.mult, op1=ALU.add),
                         reads=[RHO, Wr] + ([zin] if bi else []), writes=[Zr])
                    P.op("vector", lambda e, o=Zi[:, :n], r=RHO[:, :n], w=Wi[:, :n], i0=ii: e.tensor_tensor_scan(out=o, data0=r, data1=w, initial=i0, op0=ALU.mult, op1=ALU.add),
                         reads=[RHO, Wi] + ([zin] if bi else []), writes=[Zi])
                    if l == DEPTH - 1 and d == 1 and t0 >= CTX + LOUT:
                        P.tt(m[:, 0:1], s2[:, n - 1:n], Zi[:, n - 1:n], ALU.mult)
                        P.stt(zout[:, 0:1], Zr[:, n - 1:n], c_[:, n - 1:n], m[:, 0:1], ALU.mult, ALU.subtract)
                        P.tt(m[:, 1:2], c_[:, n - 1:n], Zi[:, n - 1:n], ALU.mult)
                        P.stt(zout[:, 1:2], Zr[:, n - 1:n], s2[:, n - 1:n], m[:, 1:2], ALU.mult, ALU.add)
                        continue
                    P.tt(t5[:, :n], c_, Zr[:, :n], ALU.mult)
                    P.tt(t6[:, :n], s2, Zr[:, :n], ALU.mult)
                    P.tt(t1[:, :n], s2, Zi[:, :n], ALU.mult)
                    P.tt(t2[:, :n], c_, Zi[:, :n], ALU.mult)
                    P.tt(xrv, t5[:, :n], t1[:, :n], ALU.subtract)
                    P.tt(xiv, t6[:, :n], t2[:, :n], ALU.add)
                    P.copy(zout[:, 0:1], xr[:, last:last + 1])
                    P.copy(zout[:, 1:2], xi[:, last:last + 1])
                    y_ = py[kb % 2]
                    P.mm(y_[:, :n], CRe[:], xr[:, :n], start=True, stop=False)
                    P.mm(y_[:, :n], CIm[:], xi[:, :n], start=False, stop=True)
                    P.tt(YA[:, ut, t0:t0 + n], YA[:, ut, t0:t0 + n], y_[:, :n], ALU.add)
        P.flush()
        wgl = sb(st, nc, "s5wgl", [128, 2, 256])
        P.dma("sync", wgl[:], g.s5_w_glu[l].rearrange("(kc p) n -> p kc n", p=128))
        C1 = 2.0 * math.sqrt(2.0 / math.pi)
        for (t0, n) in (LAST_BLOCKS if l == DEPTH - 1 else BLOCKS):
            for ut in range(2):
                x = YA[:, ut, t0:t0 + n]
                P.tt(t1[:, :n], x, x, ALU.mult)
                P.ts(t1[:, :n], t1[:, :n], 0.044715, 1.0, op0=ALU.mult, op1=ALU.add)
                P.tt(t1[:, :n], t1[:, :n], x, ALU.mult)
                P.act(t1[:, :n], t1[:, :n], AF.Sigmoid, scale=C1)
                P.tt(x, x, t1[:, :n], ALU.mult)
            for uo in range(2):
                y_ = py[uo]
                for kc in range(2):
                    P.mm(y_[:, :n], wgl[:, kc, uo * 128:(uo + 1) * 128], YA[:, kc, t0:t0 + n], start=(kc == 0), stop=(kc == 1))
                P.act(t2[:, :n], y_[:, :n], AF.Sigmoid)
                P.tt(Wr[:, :n], t2[:, :n], YA[:, uo, t0:t0 + n], ALU.mult)
                P.dma("sync", g.YS[256 + uo * 128:256 + (uo + 1) * 128, t0:t0 + n], Wr[:, :n], wk=("YS", 1, uo, t0))
        P.flush()


RW0 = 1024
NCH = T // 32


def rwkv_shift(P, nc, g, l):
    with contextlib.ExitStack() as st:
        mu = sb(st, nc, "rmu", [128, 8])
        w3 = sb(st, nc, "rw3", [128, 8, 3])
        xi = [sb(st, nc, "rxi%d" % i, [128, T]) for i in range(2)]
        xo = [sb(st, nc, "rxo%d" % i, [128, T]) for i in range(2)]
        P.dma("sync", mu[:], g.rw_mu[l])
        P.ts(w3[:, :, 0], mu[:], 0.5, None, op0=ALU.mult)
        P.ts(w3[:, :, 2], mu[:], 0.5, None, op0=ALU.mult)
        P.ts(w3[:, :, 1], mu[:], -1.0, 1.0, op0=ALU.mult, op1=ALU.add)
        for ti in range(8):
            m = 128 if ti < 7 else 64
            a, b = xi[ti % 2], xo[ti % 2]
            P.dma("sync", a[:m, :], g.PX[RW0 + ti * 128:RW0 + ti * 128 + m, :], rk=("PX", 8 + ti))
            P.ts(b[:m, :], a[:m, :], w3[:m, ti, 1:2], 0.0, op0=ALU.mult, op1=ALU.add)
            for (s0, e0) in SEGS:
                P.stt(b[:m, s0 + 1:e0], a[:m, s0:e0 - 1], w3[:m, ti, 0:1], b[:m, s0 + 1:e0], ALU.mult, ALU.add)
                P.stt(b[:m, s0:e0 - 1], a[:m, s0 + 1:e0], w3[:m, ti, 2:3], b[:m, s0:e0 - 1], ALU.mult, ALU.add)
            P.dma("sync", g.RWS[ti * 128:ti * 128 + m, :], b[:m, :], wk=("RWS", ti))
        P.flush()


def stage_rwkv(P, nc, g, l):
    rwkv_shift(P, nc, g, l)
    with contextlib.ExitStack() as st:
        NB = 512
        w2 = sb(st, nc, "rw2", [64, 256])
        a2 = sb(st, nc, "ra2", [64, 256])
        g2 = sb(st, nc, "rg2", [64, 256])
        pc = sb(st, nc, "rpc", [128, 2, 8])
        r_ = [sb(st, nc, "rr%d" % i, [128, NB]) for i in range(2)]
        k_ = [sb(st, nc, "rk%d" % i, [128, NB]) for i in range(2)]
        v_ = [sb(st, nc, "rv%d" % i, [128, NB]) for i in range(2)]
        wa = sb(st, nc, "rwa", [64, NB])
        adt = sb(st, nc, "radt", [64, NB])
        gd = sb(st, nc, "rgd", [64, NB])
        tw = sb(st, nc, "rtw", [64, NB])
        sg = sb(st, nc, "rsg", [64, NB])
        kk = sb(st, nc, "rkk", [128, NB])
        t1 = sb(st, nc, "rt1", [128, NB])
        t2 = sb(st, nc, "rt2", [128, NB])
        A = sb(st, nc, "rA", [128, NB])
        X2 = [sb(st, nc, "rX2%d" % i, [128, 2, NB]) for i in range(5)]
        hib = sb(st, nc, "rhib", [128, NB], BF16)
        X2c = [sb(st, nc, "rX2c%d" % i, [128, 16, 2, 32]) for i in range(5)]
        tok = [sb(st, nc, "rtok%d" % i, [64, 16, 640], BF16) for i in range(2)]
        og = sb(st, nc, "rog", [128, NB])
        pm = [ps(st, nc, "rpm%d" % i, [128, 512]) for i in range(3)]
        ptr = [ps(st, nc, "rptr%d" % i, [64, 128]) for i in range(3)]
        P.dma("sync", w2[:], g.rw_w2[l])
        P.dma("sync", a2[:], g.rw_a2[l])
        P.dma("sync", g2[:], g.rw_g2[l])
        P.dma("sync", pc[:], g.rw_pc[l])
        EH = -math.exp(-0.5)
        kt = 0
        for (t0, n) in BLOCKS:
            nch = n // 32
            c0 = t0 // 32
            P.dma("sync", wa[:, :n], g.RWS[768:832, t0:t0 + n], rk=("RWS", 6))
            P.dma("sync", adt[:, :n], g.RWS[832:896, t0:t0 + n], rk=("RWS", 6))
            P.dma("sync", gd[:, :n], g.RWS[896:960, t0:t0 + n], rk=("RWS", 7))
            P.act(tw[:, :n], wa[:, :n], AF.Tanh)
            P.act(sg[:, :n], gd[:, :n], AF.Sigmoid)
            for ct in range(2):
                r, k, v = r_[ct], k_[ct], v_[ct]
                P.dma("sync", r[:, :n], g.RWS[ct * 128:(ct + 1) * 128, t0:t0 + n], rk=("RWS", ct))
                P.dma("sync", k[:, :n], g.RWS[256 + ct * 128:256 + (ct + 1) * 128, t0:t0 + n], rk=("RWS", 2 + ct))
                P.dma("sync", v[:, :n], g.RWS[512 + ct * 128:512 + (ct + 1) * 128, t0:t0 + n], rk=("RWS", 4 + ct))
                P.mm(pm[0][:, :n], g2[:, ct * 128:(ct + 1) * 128], sg[:, :n])
                P.copy(og[:, :n], pm[0][:, :n], eng="scalar")
                P.dma("sync", g.RWG[256 + ct * 128:256 + (ct + 1) * 128, t0:t0 + n], og[:, :n], wk=("RWG", 2 + ct))
                P.stt(t1[:, :n], r[:, :n], pc[:, ct, 6:7], k[:, :n], ALU.mult, ALU.mult)
                P.mm(pm[1][:, :n], g.BLK[:], t1[:, :n])
                P.tt(og[:, :n], pm[1][:, :n], v[:, :n], ALU.mult)
                P.dma("sync", g.RWG[ct * 128:(ct + 1) * 128, t0:t0 + n], og[:, :n], wk=("RWG", ct))
                P.ts(kk[:, :n], k[:, :n], pc[:, ct, 4:5], None, op0=ALU.mult)
                P.tt(t1[:, :n], kk[:, :n], kk[:, :n], ALU.mult)
                P.mm(pm[2][:, :n], g.BLK[:], t1[:, :n])
                P.ts(t1[:, :n], pm[2][:, :n], 1e-24, None, op0=ALU.max)
                P.act(t1[:, :n], t1[:, :n], AF.Sqrt)
                P.recip(t1[:, :n], t1[:, :n])
                P.tt(kk[:, :n], kk[:, :n], t1[:, :n], ALU.mult)
                P.copy(X2[3][:, 0, :n], kk[:, :n], eng="gpsimd")
                P.copy(X2[4][:, 0, :n], r[:, :n], eng="gpsimd")
                for d in range(2):
                    tk = tok[kt % 2]
                    kt += 1
                    P.mm(pm[0][:, :n], w2[32 * d:32 * d + 32, ct * 128:(ct + 1) * 128], tw[32 * d:32 * d + 32, :n])
                    P.act(t1[:, :n], pm[0][:, :n], AF.Sigmoid, bias=pc[:, ct, d:d + 1])
                    P.act(X2[0][:, 0, :n], t1[:, :n], AF.Exp, scale=EH)
                    P.mm(pm[1][:, :n], a2[32 * d:32 * d + 32, ct * 128:(ct + 1) * 128], adt[32 * d:32 * d + 32, :n])
                    P.act(A[:, :n], pm[1][:, :n], AF.Sigmoid, bias=pc[:, ct, 2 + d:3 + d])
                    P.ts(t2[:, :n], A[:, :n], -1.0, pc[:, ct, 5:6], op0=ALU.add, op1=ALU.mult)
                    P.stt(X2[2][:, 0, :n], t2[:, :n], 1.0, k[:, :n], ALU.add, ALU.mult)
                    P.stt(X2[1][:, 0, :n], kk[:, :n], -1.0, A[:, :n], ALU.mult, ALU.mult)
                    for a_ in range(5):
                        x2 = X2[a_]
                        xc = X2c[a_]
                        if d == 0 or a_ < 3:
                            hv = hib[:, :n].rearrange("p (c t) -> p c t", t=32)
                            xv_ = x2[:, 0, :n].rearrange("p (c t) -> p c t", t=32)
                            P.copy(hib[:, :n], x2[:, 0, :n], eng="gpsimd")
                            P.tt(xc[:, :nch, 1, :], xv_, hv, ALU.subtract, eng="gpsimd")
                            P.copy(xc[:, :nch, 0, :], hv, eng="gpsimd")
                        for c in range(nch):
                            p_ = ptr[(a_ * nch + c) % 3]
                            P.tr(p_[:, :], xc[:, c, :, :].rearrange("p a t -> p (a t)"), g.IDENT[:])
                            dst = tk[:, c, :].rearrange("p (h a k) -> p h a k", h=2, a=5)[:, :, a_, :]
                            src = p_[:, :].rearrange("p (h k) -> p h k", h=2)
                            if (a_ + c) % 2:
                                P.copy(dst, src, eng="scalar")
                            else:
                                P.copy(dst, src, eng="vector")
                    P.dma("sync", g.TOKD[d, ct, c0:c0 + nch].rearrange("c p x -> p c x"), tk[:, :nch, :], wk=("TOKD", d, ct, t0))
        P.flush()
    border = list(range(CTX - 1, -1, -1)) + list(range(T - 1, CTX - 1, -1))
    for ct in range(2):
        with contextlib.ExitStack() as st:
            V = sb(st, nc, "rsV", [128, T])
            Y = [sb(st, nc, "rsY%d" % d, [128, T]) for d in range(2)]
            S = [sb(st, nc, "rsS%d" % d, [128, 64]) for d in range(2)]
            sa = [sb(st, nc, "rssa%d" % d, [128, 1]) for d in range(2)]
            jk = [sb(st, nc, "rsjk%d" % d, [128, 64]) for d in range(2)]
            tkb = [[sb(st, nc, "rstk%d_%d" % (d, i), [64, 640], BF16) for i in range(2)] for d in range(2)]
            pb = [[ps(st, nc, "rspb%d_%d" % (d, i), [128, 512]) for i in range(2)] for d in range(2)]
            P.dma("sync", V[:], g.RWS[512 + ct * 128:512 + (ct + 1) * 128, :], rk=("RWS", 4 + ct))
            for d in range(2):
                P.memset(S[d][:], 0.0)
            curch = [None, None]
            nld = [0, 0]
            for i in range(T):
                ts_ = (i, border[i])
                Bv = []
                for d in range(2):
                    t = ts_[d]
                    ch = t // 32
                    if ch != curch[d]:
                        curch[d] = ch
                        nld[d] += 1
                        P.dma("sync", tkb[d][nld[d] % 2][:], g.TOKD[d, ct, ch], rk=("TOKD", d, ct, BLOCKS[0][0] if t < CTX else CTX + ((t - CTX) // 512) * 512))
                    tk = tkb[d][nld[d] % 2]
                    p_ = pb[d][i % 2]
                    for h2 in range(2):
                        P.mm(p_[h2 * 64:(h2 + 1) * 64, 0:320], g.SEL64[:, t % 32, :], tk[:, h2 * 320:(h2 + 1) * 320])
                    Bv.append(p_[:, 0:320].rearrange("p (a k) -> p a k", a=5))
                for d in range(2):
                    P.op("vector", lambda e, o=jk[d][:], a=S[d][:], b=Bv[d][:, 3, :], acc=sa[d][:]: e.scalar_tensor_tensor(
                        out=o, in0=a, scalar=1.0, in1=b, op0=ALU.mult, op1=ALU.mult, accum_out=acc),
                        reads=[S[d], Bv[d]], writes=[jk[d], sa[d]])
                for d in range(2):
                    P.tt(S[d][:], S[d][:], Bv[d][:, 0, :], ALU.mult)
                for d in range(2):
                    P.stt(S[d][:], Bv[d][:, 1, :], sa[d][:, 0:1], S[d][:], ALU.mult, ALU.add)
                for d in range(2):
                    t = ts_[d]
                    P.stt(S[d][:], Bv[d][:, 2, :], V[:, t:t + 1], S[d][:], ALU.mult, ALU.add)
                for d in range(2):
                    t = ts_[d]
                    P.op("vector", lambda e, o=jk[d][:], a=S[d][:], b=Bv[d][:, 4, :], acc=Y[d][:, t:t + 1]: e.scalar_tensor_tensor(
                        out=o, in0=a, scalar=1.0, in1=b, op0=ALU.mult, op1=ALU.mult, accum_out=acc),
                        reads=[S[d], Bv[d]], writes=[jk[d], Y[d]])
            P.flush()
            with contextlib.ExitStack() as st2:
                lnp = sb(st2, nc, "rln", [128, 2, 2])
                bon = sb(st2, nc, "rbon", [128, 512])
                gg = sb(st2, nc, "rgg", [128, 512])
                yc = sb(st2, nc, "ryc", [128, 512])
                sq = sb(st2, nc, "rsq", [128, 512])
                epsg = sb(st2, nc, "repsg", [128, 1])
                pq = [ps(st2, nc, "rpq%d" % i, [128, 512]) for i in range(2)]
                P.dma("sync", lnp[:], g.rw_ln[l])
                P.memset(epsg[:], 64e-5)
                for (t0, n) in BLOCKS:
                    P.dma("sync", bon[:, :n], g.RWG[ct * 128:(ct + 1) * 128, t0:t0 + n], rk=("RWG", ct))
                    P.dma("sync", gg[:, :n], g.RWG[256 + ct * 128:256 + (ct + 1) * 128, t0:t0 + n], rk=("RWG", 2 + ct))
                    P.tt(yc[:, :n], Y[0][:, t0:t0 + n], Y[1][:, t0:t0 + n], ALU.add)
                    P.mm(pq[0][:, :n], g.BLK[:], yc[:, :n])
                    P.stt(yc[:, :n], pq[0][:, :n], -1.0 / 64, yc[:, :n], ALU.mult, ALU.add)
                    P.tt(sq[:, :n], yc[:, :n], yc[:, :n], ALU.mult)
                    P.mm(pq[1][:, :n], g.BLK[:], sq[:, :n])
                    P.act(sq[:, :n], pq[1][:, :n], AF.Sqrt, bias=epsg[:, 0:1], scale=1.0 / 64)
                    P.recip(sq[:, :n], sq[:, :n])
                    P.tt(yc[:, :n], yc[:, :n], sq[:, :n], ALU.mult)
                    P.ts(yc[:, :n], yc[:, :n], lnp[:, ct, 0:1], lnp[:, ct, 1:2], op0=ALU.mult, op1=ALU.add)
                    P.tt(yc[:, :n], yc[:, :n], bon[:, :n], ALU.add)
                    P.tt(yc[:, :n], yc[:, :n], gg[:, :n], ALU.mult)
                    P.dma("sync", g.YS[512 + ct * 128:512 + (ct + 1) * 128, t0:t0 + n], yc[:, :n], wk=("YS", 2, ct, t0))
                P.flush()


RWDBG = [0]
RWJ = [6]


def stage_rwkv_chunked(P, nc, g, l):
    rwkv_shift(P, nc, g, l)
    pending = []
    if getattr(g, "prefetch_next", None) is not None:
        pending = stage_precast(P, nc, g, g.prefetch_next, flush=False)
        set_layer_weights(g, l)
        g.prefetch_next = None
    NB = 512
    EH = -math.exp(-0.5)
    with contextlib.ExitStack() as st:
        w2 = sb(st, nc, "cw2", [64, 256])
        a2 = sb(st, nc, "ca2", [64, 256])
        g2 = sb(st, nc, "cg2", [64, 256])
        pc = sb(st, nc, "cpc", [128, 2, 8])
        mk = sb(st, nc, "cmk", [128, 4, 64])
        wdt = sb(st, nc, "cwdt", [64, NB])
        adt = sb(st, nc, "cadt", [64, NB])
        gd = sb(st, nc, "cgd", [64, NB])
        tw = sb(st, nc, "ctw", [64, NB])
        sg = sb(st, nc, "csg", [64, NB])
        R = [sb(st, nc, "cR%d" % i, [128, NB]) for i in range(2)]
        Kt = [sb(st, nc, "cK%d" % i, [128, NB]) for i in range(2)]
        V = [sb(st, nc, "cV%d" % i, [128, NB]) for i in range(2)]
        KK = [sb(st, nc, "cKK%d" % i, [128, NB]) for i in range(2)]
        LW = [sb(st, nc, "cLW%d" % i, [128, NB]) for i in range(2)]
        CUM = [sb(st, nc, "cCUM%d" % i, [128, NB]) for i in range(2)]
        BN = [sb(st, nc, "cBN%d" % i, [128, NB]) for i in range(2)]
        KD = [sb(st, nc, "cKD%d" % i, [128, NB]) for i in range(2)]
        t1 = sb(st, nc, "ct1", [128, NB])
        t2 = sb(st, nc, "ct2", [128, NB])
        A = sb(st, nc, "cA", [128, NB])
        og = sb(st, nc, "cog", [128, NB])
        ONB = sb(st, nc, "cONB", [128, NB])
        Y = [sb(st, nc, "cY%d" % i, [128, T]) for i in range(2)]
        ST = [sb(st, nc, "cST%d" % i, [128, 64]) for i in range(2)]
        cum = [sb(st, nc, "ccum%d" % i, [128, 64]) for i in range(2)]
        e0 = [sb(st, nc, "ce0%d" % i, [128, 64]) for i in range(2)]
        e1 = [sb(st, nc, "ce1%d" % i, [128, 64]) for i in range(2)]
        e2 = [sb(st, nc, "ce2%d" % i, [128, 64]) for i in range(2)]
        RT = [[sb(st, nc, "cRT%d_%d" % (i, q), [128, 64]) for q in range(2)] for i in range(2)]
        ptot = [[sb(st, nc, "cpt%d_%d" % (i, q), [128, 2]) for q in range(2)] for i in range(2)]
        ATb = [[sb(st, nc, "cATb%d_%d" % (i, q), [128, 128]) for q in range(2)] for i in range(2)]
        BTb = [sb(st, nc, "cBTb%d" % i, [128, 128]) for i in range(2)]
        KTb = [sb(st, nc, "cKTb%d" % i, [128, 128]) for i in range(2)]
        Vb = [sb(st, nc, "cVb%d" % i, [128, 128]) for i in range(2)]
        ZTb = [sb(st, nc, "cZTb%d" % i, [128, 128]) for i in range(2)]
        STb = [sb(st, nc, "cSTb%d" % i, [128, 128]) for i in range(2)]
        MB = [sb(st, nc, "cMB%d" % i, [128, 128]) for i in range(2)]
        MBT = [sb(st, nc, "cMBT%d" % i, [128, 128]) for i in range(2)]
        MK = [[sb(st, nc, "cMK%d_%d" % (i, q), [128, 128]) for q in range(2)] for i in range(2)]
        WB = [[sb(st, nc, "cWB%d_%d" % (i, q), [128, 64]) for q in range(2)] for i in range(2)]
        WK = [[sb(st, nc, "cWK%d_%d" % (i, q), [128, 64]) for q in range(2)] for i in range(2)]
        Btb = [[sb(st, nc, "cBtb%d_%d" % (i, q), [128, 128]) for q in range(2)] for i in range(2)]
        Ktb = [[sb(st, nc, "cKtb%d_%d" % (i, q), [128, 128]) for q in range(2)] for i in range(2)]
        VTb = [[sb(st, nc, "cVTb%d_%d" % (i, q), [128, 128]) for q in range(2)] for i in range(2)]
        Ma = [[sb(st, nc, "cMa%d_%d" % (c_, i), [128, 128]) for i in range(2)] for c_ in range(2)]
        MTa = [[sb(st, nc, "cMTa%d_%d" % (c_, i), [128, 128]) for i in range(2)] for c_ in range(2)]
        Nn = [[sb(st, nc, "cNn%d_%d" % (i, q), [128, 128]) for q in range(2)] for i in range(2)]
        VTs = [[sb(st, nc, "cVTs%d_%d" % (i, q), [128, 64]) for q in range(2)] for i in range(2)]
        GTs = [sb(st, nc, "cGTs%d" % i, [128, 64]) for i in range(2)]
        ZTs = [sb(st, nc, "cZTs%d" % i, [128, 64]) for i in range(2)]
        mbd = sb(st, nc, "cmbd", [128, 2, 128])
        I2 = sb(st, nc, "cI2", [128, 64])
        B1 = [ps(st, nc, "cB1_%d" % i, [128, 512]) for i in range(2)]
        B2 = [ps(st, nc, "cB2_%d" % i, [128, 512]) for i in range(2)]
        B3 = [ps(st, nc, "cB3_%d" % i, [128, 512]) for i in range(2)]
        B4 = [ps(st, nc, "cB4_%d" % i, [128, 512]) for i in range(2)]
        PM = [B1[0]]
        PI0, PI12 = B1[0], B1[1]
        P.dma("sync", mbd[:], g.rw_mbd)
        P.dma("sync", I2[:], g.rw_i2)
        for tl in (BTb, KTb, Vb, ZTb, STb):
            for i in range(2):
                P.memset(tl[i][:], 0.0, eng="gpsimd")
        for i in range(2):
            for q in range(2):
                P.memset(ATb[i][q][:], 0.0, eng="gpsimd")
        P.dma("sync", w2[:], g.rw_w2[l])
        P.dma("sync", a2[:], g.rw_a2[l])
        P.dma("sync", g2[:], g.rw_g2[l])
        P.dma("sync", pc[:], g.rw_pc[l])
        P.dma("sync", mk[:], g.rw_masks)
        P.memset(ONB[:], 1.0)
        for d in range(2):
            if d == 0:
                msk = [3, 1, 3, 0, 0]
            else:
                msk = [1, 3, 1, 2, 2]
            for ct in range(2):
                P.memset(ST[ct][:], 0.0)
                P.memset(STb[ct][:], 0.0)
            blocks = (BLOCKS[0:5] if l == DEPTH - 1 else BLOCKS) if d == 0 else [BLOCKS[0]] + BLOCKS[:0:-1]
            for (t0, n) in blocks:
                nck = n // 64
                P.dma("sync", wdt[:, :n], g.RWS[768:832, t0:t0 + n], rk=("RWS", 6))
                P.dma("sync", adt[:, :n], g.RWS[832:896, t0:t0 + n], rk=("RWS", 6))
                P.act(tw[:, :n], wdt[:, :n], AF.Tanh)
                if d == 0:
                    P.dma("sync", gd[:, :n], g.RWS[896:960, t0:t0 + n], rk=("RWS", 7))
                    P.act(sg[:, :n], gd[:, :n], AF.Sigmoid)
                for ct in range(2):
                    r, k, v, kk = R[ct], Kt[ct], V[ct], KK[ct]
                    P.dma("sync", r[:, :n], g.RWS[ct * 128:(ct + 1) * 128, t0:t0 + n], rk=("RWS", ct))
                    P.dma("sync", k[:, :n], g.RWS[256 + ct * 128:256 + (ct + 1) * 128, t0:t0 + n], rk=("RWS", 2 + ct))
                    P.dma("sync", v[:, :n], g.RWS[512 + ct * 128:512 + (ct + 1) * 128, t0:t0 + n], rk=("RWS", 4 + ct))
                    if d == 0:
                        P.mm(PM[0][:, :n], g2[:, ct * 128:(ct + 1) * 128], sg[:, :n])
                        P.copy(og[:, :n], PM[0][:, :n], eng="scalar")
                        P.dma("sync", g.RWG[256 + ct * 128:256 + (ct + 1) * 128, t0:t0 + n], og[:, :n], wk=("RWG", 2 + ct))
                        P.stt(t1[:, :n], r[:, :n], pc[:, ct, 6:7], k[:, :n], ALU.mult, ALU.mult)
                        P.mm(PM[0][:, :n], g.BLK[:], t1[:, :n])
                        P.tt(og[:, :n], PM[0][:, :n], v[:, :n], ALU.mult)
                        P.dma("sync", g.RWG[ct * 128:(ct + 1) * 128, t0:t0 + n], og[:, :n], wk=("RWG", ct))
                    P.ts(kk[:, :n], k[:, :n], pc[:, ct, 4:5], None, op0=ALU.mult)
                    P.tt(t1[:, :n], kk[:, :n], kk[:, :n], ALU.mult)
                    P.mm(PM[0][:, :n], g.BLK[:], t1[:, :n])
                    P.ts(t1[:, :n], PM[0][:, :n], 1e-24, None, op0=ALU.max)
                    P.act(t1[:, :n], t1[:, :n], AF.Sqrt)
                    P.recip(t1[:, :n], t1[:, :n])
                    P.tt(kk[:, :n], kk[:, :n], t1[:, :n], ALU.mult)
                    P.mm(PM[0][:, :n], w2[32 * d:32 * d + 32, ct * 128:(ct + 1) * 128], tw[32 * d:32 * d + 32, :n])
                    P.act(t1[:, :n], PM[0][:, :n], AF.Sigmoid, bias=pc[:, ct, d:d + 1])
                    P.ts(LW[ct][:, :n], t1[:, :n], EH, None, op0=ALU.mult)
                    P.op("vector", lambda e, o=CUM[ct][:, :n], on=ONB[:, :n], w=LW[ct][:, :n]: e.tensor_tensor_scan(out=o, data0=on, data1=w, initial=0.0, op0=ALU.mult, op1=ALU.add),
                         reads=[ONB, LW[ct]], writes=[CUM[ct]])
                    P.mm(PM[0][:, :n], a2[32 * d:32 * d + 32, ct * 128:(ct + 1) * 128], adt[32 * d:32 * d + 32, :n])
                    P.act(A[:, :n], PM[0][:, :n], AF.Sigmoid, bias=pc[:, ct, 2 + d:3 + d])
                    P.ts(t2[:, :n], A[:, :n], -1.0, pc[:, ct, 5:6], op0=ALU.add, op1=ALU.mult)
                    P.stt(KD[ct][:, :n], t2[:, :n], 1.0, k[:, :n], ALU.add, ALU.mult)
                    P.stt(BN[ct][:, :n], kk[:, :n], -1.0, A[:, :n], ALU.mult, ALU.mult)
                chunks = list(range(nck)) if d == 0 else list(range(nck - 1, -1, -1))
                if RWDBG[0] == 1:
                    chunks = []
                H = (slice(0, 64), slice(64, 128))
                i_s = 0 if d == 0 else 1
                last = 63 if d == 0 else 0

                def indep_gen(c, ct, par):
                    sl = slice(c * 64, (c + 1) * 64)
                    cm, E0, E1, E2 = cum[ct], e0[ct], e1[ct], e2[ct]
                    PSA = B1[ct][:, 0:384].rearrange("p (m k) -> p m k", m=3)
                    PSB = B2[ct][:, 0:128].rearrange("p (m k) -> p m k", m=2)
                    PSY = B2[ct][:, 128:192]
                    PSS = B2[ct][:, 192:256]
                    PI1 = B2[ct][:, 256:384]
                    PI2 = B2[ct][:, 384:512]
                    PST = B3[ct]
                    PSG = B3[ct][:, 448:512]
                    PI0 = B4[ct][:, 0:128]
                    PSZ = B4[ct][:, 128:192]
                    if c == 0:
                        P.copy(cm[:], CUM[ct][:, sl])
                    else:
                        P.ts(cm[:], CUM[ct][:, sl], CUM[ct][:, c * 64 - 1:c * 64], None, op0=ALU.subtract)
                    if d == 1:
                        P.copy(ptot[ct][par][:, 1:2], cm[:, 63:64])
                        P.stt(cm[:], cm[:], -1.0, LW[ct][:, sl], ALU.mult, ALU.add)
                        P.ts(cm[:], cm[:], ptot[ct][par][:, 1:2], None, op0=ALU.add)
                    P.tt(E0[:], cm[:], LW[ct][:, sl], ALU.subtract)
                    yield
                    P.act(E1[:], cm[:], AF.Exp)
                    P.act(E2[:], cm[:], AF.Exp, scale=-1.0)
                    P.act(E0[:], E0[:], AF.Exp)
                    yield
                    P.copy(ptot[ct][par][:, 0:1], E1[:, last:last + 1])
                    for h2 in range(2):
                        hs = H[h2]
                        P.tt(ATb[ct][par][hs, hs], KK[ct][hs, sl], E0[hs, :], ALU.mult)
                        P.tt(BTb[ct][hs, hs], BN[ct][hs, sl], E2[hs, :], ALU.mult)
                        P.tt(KTb[ct][hs, hs], KD[ct][hs, sl], E2[hs, :], ALU.mult)
                        P.copy(Vb[ct][hs, hs], V[ct][hs, sl], eng="gpsimd")
                    P.tt(RT[ct][par][:], R[ct][:, sl], E1[:], ALU.mult, eng="gpsimd")
                    yield
                    P.mm(PSA[:, 0, :], BTb[ct][:], ATb[ct][par][:], r=True)
                    P.mm(PSA[:, 1, :], ATb[ct][par][:], BTb[ct][:], r=True)
                    P.mm(PSA[:, 2, :], KTb[ct][:], ATb[ct][par][:], r=True)
                    P.mm(PSB[:, 0, :], BTb[ct][:], RT[ct][par][:], r=True)
                    P.mm(PSB[:, 1, :], KTb[ct][:], RT[ct][par][:], r=True)
                    P.tr(PST[:, 0:128], BTb[ct][:], g.IDENT[:])
                    P.tr(PST[:, 128:256], KTb[ct][:], g.IDENT[:])
                    P.tr(PST[:, 256:384], Vb[ct][:], g.IDENT[:])
                    P.mm(PST[:, 384:448], Vb[ct][:], I2[:])
                    yield
                    P.tt(MB[ct][:], PSA[:, 0, :], mbd[:, i_s, :], ALU.mult)
                    P.tt(MBT[ct][:], PSA[:, 1, :], mbd[:, 1 - i_s, :], ALU.mult)
                    P.tt(Nn[ct][par][:], MB[ct][:], g.IDENT[:], ALU.add)
                    P.tt(MK[ct][par][:], PSA[:, 2, :], mbd[:, i_s, :], ALU.mult)
                    P.tt(WB[ct][par][:], PSB[:, 0, :], mk[:, msk[3], :], ALU.mult)
                    P.tt(WK[ct][par][:], PSB[:, 1, :], mk[:, msk[4], :], ALU.mult)
                    P.copy(Btb[ct][par][:], PST[:, 0:128], eng="scalar")
                    P.copy(Ktb[ct][par][:], PST[:, 128:256], eng="scalar")
                    P.copy(VTb[ct][par][:], PST[:, 256:384], eng="scalar")
                    P.copy(VTs[ct][par][:], PST[:, 384:448], eng="scalar")
                    yield
                    Mc, MTc = MB[ct], MBT[ct]
                    for j in range(1, 6):
                        if j < 5:
                            P.mm(PI0, MTc[:], Mc[:], r=True)
                        P.mm(PI1, Mc[:], MTc[:], r=True)
                        yield
                        Mn, MTn = Ma[ct][j % 2], MTa[ct][j % 2]
                        if j < 5:
                            P.copy(Mn[:], PI0, eng="scalar")
                        P.copy(MTn[:], PI1, eng="vector")
                        yield
                        P.mm(PI2, MTn[:], Nn[ct][par][:], r=True)
                        yield
                        P.tt(Nn[ct][par][:], Nn[ct][par][:], PI2, ALU.add)
                        yield
                        Mc, MTc = Mn, MTn

                def dep_gen(c, ct, par):
                    PSY = B2[ct][:, 128:192]
                    PSS = B2[ct][:, 192:256]
                    PSG = B3[ct][:, 448:512]
                    PSZ = B4[ct][:, 128:192]
                    P.mm(PSG, ATb[ct][par][:], ST[ct][:], start=True, stop=False)
                    P.mm(PSG, MK[ct][par][:], VTs[ct][par][:], start=False, stop=True)
                    yield
                    P.copy(GTs[ct][:], PSG, eng="scalar")
                    yield
                    P.mm(PSZ, Nn[ct][par][:], GTs[ct][:], r=True)
                    yield
                    P.copy(ZTs[ct][:], PSZ, eng="scalar")
                    for h2 in range(2):
                        P.copy(ZTb[ct][H[h2], H[h2]], B4[ct][H[h2], 128:192], eng="scalar")
                    yield
                    P.mm(PSY, STb[ct][:], RT[ct][par][:], start=True, stop=False)
                    P.mm(PSY, ZTb[ct][:], WB[ct][par][:], start=False, stop=False)
                    P.mm(PSY, VTb[ct][par][:], WK[ct][par][:], start=False, stop=True)
                    P.mm(PSS, Btb[ct][par][:], ZTs[ct][:], start=True, stop=False)
                    P.mm(PSS, Ktb[ct][par][:], VTs[ct][par][:], start=False, stop=True)
                    yield
                    P.copy(Y[ct][:, t0 + c * 64:t0 + (c + 1) * 64], PSY, eng="vector")
                    P.ts(ST[ct][:], ST[ct][:], ptot[ct][par][:, 0:1], None, op0=ALU.mult)
                    P.stt(ST[ct][:], PSS, ptot[ct][par][:, 0:1], ST[ct][:], ALU.mult, ALU.add)
                    yield
                    for h2 in range(2):
                        P.copy(STb[ct][H[h2], H[h2]], ST[ct][H[h2], :], eng="scalar")

                def drive(gens):
                    while gens:
                        for gen in list(gens):
                            try:
                                next(gen)
                            except StopIteration:
                                gens.remove(gen)

                drive([indep_gen(chunks[0], 0, 0), indep_gen(chunks[0], 1, 0)])
                for ii, c in enumerate(chunks):
                    for _ in range(6):
                        if pending:
                            o_, i_, k_ = pending.pop(0)
                            P.dma("gpsimd", o_, i_, wk=k_)
                    gens = [dep_gen(c, 0, ii % 2), dep_gen(c, 1, ii % 2)]
                    if ii + 1 < len(chunks):
                        gens += [indep_gen(chunks[ii + 1], 0, (ii + 1) % 2), indep_gen(chunks[ii + 1], 1, (ii + 1) % 2)]
                    drive(gens)
            if d == 0:
                for (o_, i_, k_) in pending:
                    P.dma("gpsimd", o_, i_, wk=k_)
                pending = []
                for ct in range(2):
                    P.dma("sync", g.RWY[ct * 128:(ct + 1) * 128, :], Y[ct][:], wk=("RWY", ct))
        P.flush()
        with contextlib.ExitStack() as st2:
            lnp = sb(st2, nc, "cln", [128, 2, 2])
            epsg = sb(st2, nc, "cepsg", [128, 1])
            y0 = t2
            P.dma("sync", lnp[:], g.rw_ln[l])
            P.memset(epsg[:], 64e-5)
            for ct in range(2):
                for (t0, n) in (LAST_BLOCKS if l == DEPTH - 1 else BLOCKS):
                    bon, gg, yc, sq = og, A, t1, CUM[0]
                    P.dma("sync", bon[:, :n], g.RWG[ct * 128:(ct + 1) * 128, t0:t0 + n], rk=("RWG", ct))
                    P.dma("sync", gg[:, :n], g.RWG[256 + ct * 128:256 + (ct + 1) * 128, t0:t0 + n], rk=("RWG", 2 + ct))
                    P.dma("sync", y0[:, :n], g.RWY[ct * 128:(ct + 1) * 128, t0:t0 + n], rk=("RWY", ct))
                    P.tt(yc[:, :n], y0[:, :n], Y[ct][:, t0:t0 + n], ALU.add)
                    P.mm(PI12[:, :n], g.BLK[:], yc[:, :n])
                    P.stt(yc[:, :n], PI12[:, :n], -1.0 / 64, yc[:, :n], ALU.mult, ALU.add)
                    P.tt(sq[:, :n], yc[:, :n], yc[:, :n], ALU.mult)
                    P.mm(PI0[:, :n], g.BLK[:], sq[:, :n])
                    P.act(sq[:, :n], PI0[:, :n], AF.Sqrt, bias=epsg[:, 0:1], scale=1.0 / 64)
                    P.recip(sq[:, :n], sq[:, :n])
                    P.tt(yc[:, :n], yc[:, :n], sq[:, :n], ALU.mult)
                    P.ts(yc[:, :n], yc[:, :n], lnp[:, ct, 0:1], lnp[:, ct, 1:2], op0=ALU.mult, op1=ALU.add)
                    P.tt(yc[:, :n], yc[:, :n], bon[:, :n], ALU.add)
                    P.tt(yc[:, :n], yc[:, :n], gg[:, :n], ALU.mult)
                    P.dma("sync", g.YS[512 + ct * 128:512 + (ct + 1) * 128, t0:t0 + n], yc[:, :n], wk=("YS", 2, ct, t0))
            P.flush()


M0 = 1984
NCK = T // 64


def stage_ssd(P, nc, g, l):
    with contextlib.ExitStack() as st:
        cw = sb(st, nc, "mcw", [128, 6, 3])
        cb = sb(st, nc, "mcb", [128, 6])
        xi = [sb(st, nc, "mxi%d" % i, [128, T]) for i in range(2)]
        y = [sb(st, nc, "my%d" % i, [128, T]) for i in range(2)]
        tms = [sb(st, nc, "mtms%d" % i, [128, T // 128, 128]) for i in range(2)]
        pt = [ps(st, nc, "mpt%d" % i, [128, 128]) for i in range(2)]
        P.dma("sync", cw[:], g.m2_conv_w[l])
        P.dma("sync", cb[:], g.m2_conv_b[l])
        sxv = g.SXT.rearrange("(tt p) c -> p tt c", p=128)
        srcs = [(M0 + 256, 128, 0, 0, None), (M0 + 384, 128, 1, 128, None),
                (M0 + 512, 128, 2, 256, 0), (M0 + 640, 128, 3, 384, 128),
                (M0 + 768, 128, 4, None, 256), (M0 + 896, 128, 5, None, 384),
                (M0 + 0, 128, None, 512, None), (M0 + 128, 128, None, 640, None),
                (M0 + 1024, 8, "dt", 768, None)]
        for si, (r0, m, ci, col, brow) in enumerate(srcs):
            a, b = xi[si % 2], y[si % 2]
            tm = tms[si % 2]
            P.dma("sync", a[:m, :], g.PX[r0:r0 + m, :], rk=("PXm", si))
            if ci == "dt":
                b = a
            elif ci is None:
                P.act(b[:m, :], a[:m, :], AF.Silu)
            else:
                conv3(P, b, a, cw[:, ci, :], cb[:, ci:ci + 1])
                P.act(b[:, :], b[:, :], AF.Silu)
            if brow is not None:
                P.dma("sync", g.SBC[brow:brow + 128, :], b[:, :], wk=("SBC", brow))
            if col is not None:
                for tt_ in range(T // 128):
                    p_ = pt[tt_ % 2]
                    P.tr(p_[:, :m], b[:m, tt_ * 128:(tt_ + 1) * 128], g.IDENT[:m, :m])
                    P.copy(tm[:, tt_, :m], p_[:, :m], eng=("scalar" if tt_ % 2 else "vector"))
                P.dma("sync", sxv[:, :, col:col + m], tm[:, :, :m], wk=("SXT", col))
        P.flush()
    with contextlib.ExitStack() as st:
        mk = sb(st, nc, "mmk", [64, 4, 64])
        MFB = [sb(st, nc, "mMF%d" % d, [64, 4, 64]) for d in range(2)]
        bias8 = sb(st, nc, "mb8", [64, 8])
        a8 = sb(st, nc, "ma8", [64, 8])
        STd = [sb(st, nc, "mST%d" % d, [128, 4, 64]) for d in range(2)]
        tmc_ = [[sb(st, nc, "mtmc%d_%d" % (d, i), [64, 776]) for i in range(2)] for d in range(2)]
        bc_ = [[sb(st, nc, "mbc%d_%d" % (d, i), [128, 4, 64]) for i in range(2)] for d in range(2)]
        dtd_ = [sb(st, nc, "mdtd%d" % d, [64, 8]) for d in range(2)]
        adt_ = [sb(st, nc, "madt%d" % d, [64, 8]) for d in range(2)]
        rh_ = [sb(st, nc, "mrh%d" % d, [64, 4, 64]) for d in range(2)]
        E_ = [sb(st, nc, "mE%d" % d, [64, 4, 64]) for d in range(2)]
        SdT_ = [sb(st, nc, "mSdT%d" % d, [64, 4, 64]) for d in range(2)]
        xdt_ = [sb(st, nc, "mxdt%d" % d, [64, 4, 64]) for d in range(2)]
        xdw_ = [sb(st, nc, "mxdw%d" % d, [64, 4, 64]) for d in range(2)]
        sm_ = [sb(st, nc, "msm%d" % d, [128, 12]) for d in range(2)]
        Yd_ = [[sb(st, nc, "mYd%d_%d" % (d, i), [64, 256]) for i in range(2)] for d in range(2)]
        bkA = [ps(st, nc, "mbkA%d" % d, [128, 512]) for d in range(2)]
        bkB = [ps(st, nc, "mbkB%d" % d, [128, 512]) for d in range(2)]
        bkC = [ps(st, nc, "mbkC%d" % d, [128, 512]) for d in range(2)]
        P.dma("sync", mk[:], g.m2_masks)
        for h in range(4):
            P.copy(MFB[0][:, h, :], mk[:, 0, :])
            P.copy(MFB[1][:, h, :], mk[:, 2, :])
        P.dma("sync", bias8[:], g.m2_dt_bias[l:l + 1, :].partition_broadcast(64))
        P.dma("sync", a8[:], g.m2_a_log[l:l + 1, :].partition_broadcast(64))
        P.act(a8[:], a8[:], AF.Exp)
        P.ts(a8[:], a8[:], -1.0, None, op0=ALU.mult)
        for d in range(2):
            P.memset(STd[d][:], 0.0)
        orders = [list(range(NCK)), [3, 2, 1, 0] + list(range(NCK - 1, 3, -1))]
        for ci in range(NCK):
            for d in range(2):
                if l == DEPTH - 1 and d == 0 and ci >= (CTX + LOUT) // 64:
                    continue
                c = orders[d][ci]
                A_lhs = mk[:, 1, :] if d == 0 else mk[:, 3, :]
                A_rhs = mk[:, 0, :] if d == 0 else mk[:, 2, :]
                MM = MFB[d]
                t0 = c * 64
                tmc = tmc_[d][ci % 2]
                bc = bc_[d][ci % 2]
                dtd, adt, rh, E, SdT, xdt, xdw, sm = dtd_[d], adt_[d], rh_[d], E_[d], SdT_[d], xdt_[d], xdw_[d], sm_[d]
                Yd = Yd_[d][ci % 2]
                p_seg = bkA[d][0:64, 0:256].rearrange("p (h k) -> p h k", h=4)
                p_smA = bkA[d][:, 256:268]
                p_sc = bkB[d][0:64, 0:128].rearrange("p (h k) -> p h k", h=2)
                p_yo = bkB[d][0:64, 128:384].rearrange("p (h k) -> p h k", h=4)
                p_st = bkC[d][:, 0:256].rearrange("p (h k) -> p h k", h=4)
                p_yd = bkC[d][0:64, 256:512].rearrange("p (h k) -> p h k", h=4)
                P.dma("sync", tmc[:], g.SXT[t0:t0 + 64, :], rk=("SXTall",))
                P.dma("sync", bc[:], g.SBC.rearrange("(a n) t -> n a t", n=128)[:, :, t0:t0 + 64], rk=("SBCall",))
                P.tt(dtd[:], tmc[:, 768:776], bias8[:], ALU.add)
                P.act(dtd[:], dtd[:], AF.Exp)
                P.ts(dtd[:], dtd[:], 1.0, None, op0=ALU.add)
                P.act(dtd[:], dtd[:], AF.Ln)
                P.tt(adt[:], dtd[:], a8[:], ALU.mult)
                o4 = 4 * d
                bc4 = lambda ap: ap.unsqueeze(2).broadcast_to([ap.shape[0], 4, 64])
                light = (l == DEPTH - 1 and d == 1 and t0 >= CTX + LOUT)
                if light:
                    P.tt(xdt[:], tmc[:, 0:256].rearrange("p (h k) -> p h k", h=4), bc4(dtd[:, o4:o4 + 4]), ALU.mult)
                    P.mm(p_smA[0:64, 4:8], A_lhs, adt[:, o4:o4 + 4])
                    P.mm(p_smA[:, 8:12], g.ONES[0:64, :], adt[:, o4:o4 + 4])
                    P.act(sm[0:64, 4:8], p_smA[0:64, 4:8], AF.Exp)
                    P.act(sm[:, 8:12], p_smA[:, 8:12], AF.Exp)
                    P.tt(xdw[:], xdt[:], bc4(sm[0:64, 4:8]), ALU.mult)
                    for h in range(4):
                        P.mm(p_st[:, h, :], tmc[:, 256 + (h // 2) * 128:256 + (h // 2 + 1) * 128], xdw[:, h, :])
                    P.tt(STd[d][:], STd[d][:], bc4(sm[:, 8:12]), ALU.mult)
                    P.tt(STd[d][:], STd[d][:], p_st, ALU.add)
                    continue
                P.tt(rh[:], A_rhs.unsqueeze(1).broadcast_to([64, 4, 64]), bc4(adt[:, o4:o4 + 4]), ALU.mult)
                for h in range(4):
                    P.mm(p_seg[:, h, :], A_lhs, rh[:, h, :])
                P.act(E[:], p_seg, AF.Exp)
                P.tt(E[:], E[:], MM[:], ALU.mult)
                for gi in range(2):
                    P.mm(p_sc[:, gi, :], bc[:, gi, :], bc[:, 2 + gi, :])
                P.tt(SdT[:].rearrange("p (g h) k -> p g h k", g=2), E[:].rearrange("p (g h) k -> p g h k", g=2),
                     p_sc.unsqueeze(2).broadcast_to([64, 2, 2, 64]), ALU.mult)
                P.tt(xdt[:], tmc[:, 0:256].rearrange("p (h k) -> p h k", h=4), bc4(dtd[:, o4:o4 + 4]), ALU.mult)
                for h in range(4):
                    P.mm(p_yd[:, h, :], SdT[:, h, :], xdt[:, h, :])
                P.mm(p_smA[0:64, 0:4], A_rhs, adt[:, o4:o4 + 4])
                P.mm(p_smA[0:64, 4:8], A_lhs, adt[:, o4:o4 + 4])
                P.mm(p_smA[:, 8:12], g.ONES[0:64, :], adt[:, o4:o4 + 4])
                P.act(sm[0:64, 0:8], p_smA[0:64, 0:8], AF.Exp)
                P.act(sm[:, 8:12], p_smA[:, 8:12], AF.Exp)
                for h in range(4):
                    P.mm(p_yo[:, h, :], bc[:, 2 + h // 2, :], STd[d][:, h, :])
                Yv = Yd[:].rearrange("p (h k) -> p h k", h=4)
                P.tt(rh[:], p_yo, bc4(sm[0:64, 0:4]), ALU.mult)
                P.tt(Yv, p_yd, rh[:], ALU.add)
                P.tt(xdw[:], xdt[:], bc4(sm[0:64, 4:8]), ALU.mult)
                for h in range(4):
                    P.mm(p_st[:, h, :], tmc[:, 256 + (h // 2) * 128:256 + (h // 2 + 1) * 128], xdw[:, h, :])
                P.tt(STd[d][:], STd[d][:], bc4(sm[:, 8:12]), ALU.mult)
                P.tt(STd[d][:], STd[d][:], p_st, ALU.add)
                P.dma("sync", g.SYF[d, t0:t0 + 64, :], Yd[:], wk=("SYF", d, c))
        P.flush()
    with contextlib.ExitStack() as st:
        dsk = sb(st, nc, "mdsk", [128, 4])
        nw = sb(st, nc, "mnw", [128, 2])
        epsm = sb(st, nc, "mepsm", [128, 1])
        tm_ = [sb(st, nc, "m3tm%d" % i, [128, 776]) for i in range(2)]
        ya_ = [sb(st, nc, "m3ya%d" % i, [128, 256]) for i in range(2)]
        yb_ = [sb(st, nc, "m3yb%d" % i, [128, 256]) for i in range(2)]
        Yo = sb(st, nc, "m3Yo", [128, 256])
        ss = sb(st, nc, "m3ss", [128, 2])
        ot_ = [sb(st, nc, "m3ot%d" % i, [128, 128]) for i in range(2)]
        p_tr = [ps(st, nc, "m3ptr%d" % i, [128, 512]) for i in range(2)]
        P.dma("sync", dsk[:], g.m2_d[l:l + 1, :].partition_broadcast(128))
        P.dma("sync", nw[:], g.m2_norm_w[l])
        P.memset(epsm[:], EPS)
        for tt_ in (range(CTX // 128, (CTX + LOUT) // 128) if l == DEPTH - 1 else range(T // 128)):
            t0 = tt_ * 128
            tm, ya, yb = tm_[tt_ % 2], ya_[tt_ % 2], yb_[tt_ % 2]
            P.dma("sync", tm[:], g.SXT[t0:t0 + 128, :], rk=("SXTall",))
            P.dma("sync", ya[:], g.SYF[0, t0:t0 + 128, :], rk=("SYFall",))
            P.dma("sync", yb[:], g.SYF[1, t0:t0 + 128, :], rk=("SYFall",))
            P.tt(ya[:], ya[:], yb[:], ALU.add)
            for h in range(4):
                P.stt(ya[:, h * 64:(h + 1) * 64], tm[:, h * 64:(h + 1) * 64], dsk[:, h:h + 1], ya[:, h * 64:(h + 1) * 64], ALU.mult, ALU.add)
            P.tt(ya[:], ya[:], tm[:, 512:768], ALU.mult)
            P.act(Yo[:], ya[:], AF.Square, accum_out=ss[:, 0:1])
            P.act(ss[:, 1:2], ss[:, 0:1], AF.Sqrt, bias=epsm[:, 0:1], scale=1.0 / 256)
            P.recip(ss[:, 1:2], ss[:, 1:2])
            P.ts(ya[:], ya[:], ss[:, 1:2], None, op0=ALU.mult)
            for ct in range(2):
                p_ = p_tr[ct]
                o_ = ot_[ct]
                P.tr(p_[:, 0:128], ya[:, ct * 128:(ct + 1) * 128], g.IDENT[:])
                P.act(o_[:], p_[:, 0:128], AF.Identity, scale=nw[:, ct:ct + 1])
                P.dma("sync", g.YS[768 + ct * 128:768 + (ct + 1) * 128, t0:t0 + 128], o_[:], wk=("YS", 3, ct, tt_))
        P.flush()


ONLY = [None]


def stage_mixers(P, nc, g, l):
    if ONLY[0] in (None, "hy"):
        stage_hyena(P, nc, g, l)
    if ONLY[0] in (None, "s5"):
        stage_s5(P, nc, g, l)
    if ONLY[0] in (None, "rw"):
        stage_rwkv_chunked(P, nc, g, l)
    if ONLY[0] in (None, "m2"):
        stage_ssd(P, nc, g, l)


def stage_merge(P, nc, g, l, xsrc, xdst):
    issue_bg(P, g)
    P.flush()
    with contextlib.ExitStack() as st:
        wbr = sb(st, nc, "wbr", [128, 8, D], BF16)
        wo = sb(st, nc, "wo", [128, 8, D], BF16)
        wgt = [sb(st, nc, "wgt%d" % i, [128, 8, 512], BF16) for i in range(2)]
        ysf = sb(st, nc, "ysf", [128, 8, 512])
        xmb = sb(st, nc, "xmb", [128, 8, 512], BF16)
        xmv = g.XM16.rearrange("(fc p) t -> p fc t", p=128)
        ysb = sb(st, nc, "ysb", [128, 8, 512], BF16)
        mg = sb(st, nc, "mg", [128, 8, 512], BF16)
        acc = sb(st, nc, "acc", [128, 512])
        sg = sb(st, nc, "sg", [128, 512])
        xb = sb(st, nc, "xbm", [128, 8, 512])
        xo = sb(st, nc, "xom", [128, 8, 512])
        pb = [ps(st, nc, "pb%d" % i, [128, 512]) for i in range(2)]
        pg = [ps(st, nc, "pg%d" % i, [128, 512]) for i in range(2)]
        po = [ps(st, nc, "po%d" % i, [128, 512]) for i in range(2)]
        P.dma("sync", wbr[:], g.wbr16.rearrange("(c p) n -> p c n", p=128), rk=("wbr16",))
        P.dma("sync", wo[:], g.wout16.rearrange("(c p) n -> p c n", p=128), rk=("wout16",))
        wv = g.win16.rearrange("(fc p) n -> p fc n", p=128)
        xv = xsrc.rearrange("(fc p) t -> p fc t", p=128)
        xdv = xdst.rearrange("(fc p) t -> p fc t", p=128)
        ysv = g.YS.rearrange("(c p) t -> p c t", p=128)
        k = 0
        wi = 0
        for (t0, n) in (LAST_BLOCKS if l == DEPTH - 1 else BLOCKS):
            s = 1 if t0 < CTX else 0
            P.dma("sync", ysf[:, :, :n], ysv[:, :, t0:t0 + n], rk=("YS",))
            P.copy(ysb[:, :, :n], ysf[:, :, :n], eng="gpsimd")
            P.dma("sync", xb[:, :, :n], xv[:, :, t0:t0 + n])
            P.dma("sync", xmb[:, :, :n], xmv[:, :, t0:t0 + n], rk=("XM16",))
            for ft in range(8):
                w = wgt[wi % 2]
                wi += 1
                for i in range(4):
                    c0 = NG_COLS + i * D + ft * 128
                    P.dma("sync", w[:, :, i * 128:(i + 1) * 128], wv[:, :, c0:c0 + 128], rk=("win16",))
                for i in range(4):
                    b_ = pb[k % 2]
                    g_ = pg[k % 2]
                    k += 1
                    for kc in range(2):
                        P.mm(b_[:, :n], wbr[:, i * 2 + kc, ft * 128:(ft + 1) * 128], ysb[:, i * 2 + kc, :n], start=(kc == 0), stop=(kc == 1))
                    for fc in range(8):
                        P.mm(g_[:, :n], w[:, fc, i * 128:(i + 1) * 128], xmb[:, fc, :n], start=(fc == 0), stop=(fc == 7))
                    P.act(sg[:, :n], g_[:, :n], AF.Sigmoid)
                    if i == 0:
                        P.tt(acc[:, :n], sg[:, :n], b_[:, :n], ALU.mult)
                    else:
                        P.tt(sg[:, :n], sg[:, :n], b_[:, :n], ALU.mult)
                        P.tt(acc[:, :n], acc[:, :n], sg[:, :n], ALU.add)
                P.copy(mg[:, ft, :n], acc[:, :n], eng="gpsimd")
            for fo in range(8):
                o_ = po[fo % 2]
                for fc in range(8):
                    P.mm(o_[:, :n], wo[:, fc, fo * 128:(fo + 1) * 128], mg[:, fc, :n], start=(fc == 0), stop=(fc == 7))
                P.stt(xo[:, fo, :n], o_[:, :n], g.MODT[:, s, 16 + fo:17 + fo], xb[:, fo, :n], ALU.mult, ALU.add)
            P.dma("sync", xdv[:, :, t0:t0 + n], xo[:, :, :n])
        P.flush()


def stage_moe(P, nc, g, l, xsrc, xdst):
    HALF = [LAST_BLOCKS] if l == DEPTH - 1 else [BLOCKS[0:5], BLOCKS[5:9]]
    for hb in HALF:
        h0 = hb[0][0]
        hn = sum(n for _, n in hb)
        with contextlib.ExitStack() as st:
            u2 = sb(st, nc, "u2", [128, 8, hn], BF16)
            combT = sb(st, nc, "combT", [16, hn])
            with contextlib.ExitStack() as st2:
                xb = [sb(st2, nc, "xb%d" % i, [128, 8, 512]) for i in range(2)]
                u2f = sb(st2, nc, "u2f", [128, 8, 512])
                sq = sb(st2, nc, "sq", [128, 8, 512])
                rs = sb(st2, nc, "rs", [128, 512])
                tmp = sb(st2, nc, "tmp", [128, 512])
                wr = sb(st2, nc, "wr", [128, 8, 20])
                lg = sb(st2, nc, "lg", [128, 20])
                sm = sb(st2, nc, "sm", [128, 16])
                sel = sb(st2, nc, "sel", [128, 4])
                sel2 = sb(st2, nc, "sel2", [128, 4])
                oh = sb(st2, nc, "oh", [128, 4])
                oh1 = sb(st2, nc, "oh1", [128, 4])
                oh2 = sb(st2, nc, "oh2", [128, 4])
                we = sb(st2, nc, "we", [128, 4])
                comb = sb(st2, nc, "comb", [128, 16])
                pss = ps(st2, nc, "pss", [128, 512])
                plg = ps(st2, nc, "plg", [128, 20])
                pct = ps(st2, nc, "pct", [16, 128])
                P.dma("sync", wr[:], g.w_router[l])
                xv = xsrc.rearrange("(fc p) t -> p fc t", p=128)
                for bi, (t0, n) in enumerate(hb):
                    s = 1 if t0 < CTX else 0
                    x = xb[bi % 2]
                    P.dma("sync", x[:, :, :n], xv[:, :, t0:t0 + n])
                    modulate_block(P, nc, g, x, t0 - h0, n, s, g.S2[:, s, :], g.MODT[:, s, 24:32], u2, sq, pss, rs, tmp, out_f32=u2f)
                    for tt_ in range(n // 128):
                        for fc in range(8):
                            P.mm(plg[:], u2f[:, fc, tt_ * 128:(tt_ + 1) * 128], wr[:, fc, :], start=(fc == 0), stop=(fc == 7))
                        P.copy(lg[:], plg[:])
                        A = sm
                        P.reduce(A[:, 0:1], lg[:, 0:4], ALU.max)
                        P.ts(oh[:], lg[:, 0:4], A[:, 0:1], None, op0=ALU.is_equal)
                        P.ts(A[:, 1:2], A[:, 0:1], -1.0, None, op0=ALU.mult)
                        P.act(A[:, 4:8], lg[:, 0:4], AF.Exp, bias=A[:, 1:2], scale=1.0)
                        P.reduce(A[:, 2:3], A[:, 4:8], ALU.add)
                        P.recip(A[:, 3:4], A[:, 2:3])
                        P.ts(sel[:], lg[:, 4:8], oh[:, 0:1], None, op0=ALU.mult)
                        for gi in range(1, 4):
                            P.stt(sel[:], lg[:, 4 + 4 * gi:8 + 4 * gi], oh[:, gi:gi + 1], sel[:], ALU.mult, ALU.add)
                        P.reduce(A[:, 8:9], sel[:], ALU.max)
                        P.ts(oh1[:], sel[:], A[:, 8:9], None, op0=ALU.is_equal)
                        P.stt(sel2[:], oh1[:], -1e30, sel[:], ALU.mult, ALU.add)
                        P.reduce(A[:, 9:10], sel2[:], ALU.max)
                        P.ts(oh2[:], sel2[:], A[:, 9:10], None, op0=ALU.is_equal)
                        P.tt(A[:, 10:11], A[:, 9:10], A[:, 8:9], ALU.subtract)
                        P.act(A[:, 11:12], A[:, 10:11], AF.Exp)
                        P.ts(A[:, 12:13], A[:, 11:12], 1.0, None, op0=ALU.add)
                        P.recip(A[:, 13:14], A[:, 12:13])
                        P.tt(A[:, 14:15], A[:, 11:12], A[:, 13:14], ALU.mult)
                        P.ts(we[:], oh1[:], A[:, 13:14], None, op0=ALU.mult)
                        P.stt(we[:], oh2[:], A[:, 14:15], we[:], ALU.mult, ALU.add)
                        P.ts(we[:], we[:], A[:, 3:4], None, op0=ALU.mult)
                        for gi in range(4):
                            P.ts(comb[:, gi * 4:gi * 4 + 4], we[:], oh[:, gi:gi + 1], None, op0=ALU.mult)
                        P.tr(pct[:], comb[:], g.IDENT[:])
                        c0 = t0 - h0 + tt_ * 128
                        P.copy(combT[:, c0:c0 + 128], pct[:], eng="scalar")
                P.dma("sync", g.COMBD[:, 0:hn], combT[:, :], wk=("COMBD",))
                P.flush()
            yacc = sb(st, nc, "yacc", [128, 8, hn])
            with contextlib.ExitStack() as st2:
                wg = [sb(st2, nc, "wg%d" % i, [128, 8, 512], BF16) for i in range(2)]
                wu = [sb(st2, nc, "wu%d" % i, [128, 8, 512], BF16) for i in range(2)]
                wd = [sb(st2, nc, "wd%d" % i, [128, 4, D], BF16) for i in range(1)]
                hT = [sb(st2, nc, "hT%d" % i, [128, 4, 512], BF16) for i in range(2)]
                cbs = [sb(st2, nc, "cb%d" % i, [128, 512]) for i in range(2)]
                KD = [0]
                t1 = [sb(st2, nc, "t1_%d" % i, [128, 512]) for i in range(2)]
                pgm = [ps(st2, nc, "pgm%d" % i, [128, 512]) for i in range(2)]
                pum = [ps(st2, nc, "pum%d" % i, [128, 512]) for i in range(2)]
                pdm = [ps(st2, nc, "pdm%d" % i, [128, 512]) for i in range(2)]
                pcm = ps(st2, nc, "pcm", [128, 512])
                k = 0
                kd = 0
                for e in range(16):
                    a, b_, c_ = wg[e % 2], wu[e % 2], wd[0]
                    P.dma("sync", a[:], g.wg16[e].rearrange("(fc p) n -> p fc n", p=128), rk=("wg16", e))
                    P.dma("sync", b_[:], g.wu16[e].rearrange("(fc p) n -> p fc n", p=128), rk=("wu16", e))
                    P.dma("sync", c_[:], g.wd16[e].rearrange("(fc p) n -> p fc n", p=128), rk=("wd16", e))
                    def emit_back(o0, n, h_, e=e, c_=c_):
                        nonlocal_kd = KD
                        for fo in range(8):
                            pd_ = pdm[nonlocal_kd[0] % 2]
                            nonlocal_kd[0] += 1
                            for ff in range(4):
                                P.mm(pd_[:, :n], c_[:, ff, fo * 128:(fo + 1) * 128], h_[:, ff, :n], start=(ff == 0), stop=(ff == 3))
                            if e == 0:
                                P.copy(yacc[:, fo, o0:o0 + n], pd_[:, :n])
                            else:
                                P.tt(yacc[:, fo, o0:o0 + n], yacc[:, fo, o0:o0 + n], pd_[:, :n], ALU.add)

                    prev = None
                    for bi, (t0, n) in enumerate(hb):
                        o0 = t0 - h0
                        h_ = hT[bi % 2]
                        cbt = cbs[bi % 2]
                        P.dma("sync", cbt[:, :n], g.COMBD[e:e + 1, o0:o0 + n].partition_broadcast(128), rk=("COMBD",))
                        for ff in range(4):
                            pg_, pu_ = pgm[k % 2], pum[k % 2]
                            tt1 = t1[k % 2]
                            k += 1
                            for fc in range(8):
                                P.mm(pg_[:, :n], a[:, fc, ff * 128:(ff + 1) * 128], u2[:, fc, o0:o0 + n], start=(fc == 0), stop=(fc == 7))
                            for fc in range(8):
                                P.mm(pu_[:, :n], b_[:, fc, ff * 128:(ff + 1) * 128], u2[:, fc, o0:o0 + n], start=(fc == 0), stop=(fc == 7))
                            P.act(tt1[:, :n], pg_[:, :n], AF.Silu)
                            P.tt(tt1[:, :n], tt1[:, :n], pu_[:, :n], ALU.mult)
                            P.tt(h_[:, ff, :n], tt1[:, :n], cbt[:, :n], ALU.mult, eng="gpsimd")
                        if prev is not None:
                            emit_back(*prev)
                        prev = (o0, n, h_)
                    emit_back(*prev)
                P.flush()
            with contextlib.ExitStack() as st2:
                xb = [sb(st2, nc, "xr%d" % i, [128, 8, 512]) for i in range(2)]
                xv = xsrc.rearrange("(fc p) t -> p fc t", p=128)
                xdv = xdst.rearrange("(fc p) t -> p fc t", p=128)
                for bi, (t0, n) in enumerate(hb):
                    s = 1 if t0 < CTX else 0
                    o0 = t0 - h0
                    x = xb[bi % 2]
                    P.dma("sync", x[:, :, :n], xv[:, :, t0:t0 + n])
                    for fo in range(8):
                        P.stt(x[:, fo, :n], yacc[:, fo, o0:o0 + n], g.MODT[:, s, 40 + fo:41 + fo], x[:, fo, :n], ALU.mult, ALU.add)
                    P.dma("sync", xdv[:, :, t0:t0 + n], x[:, :, :n])
                P.flush()


def stage_final(P, nc, g, xsrc):
    with contextlib.ExitStack() as st:
        xb = [sb(st, nc, "xf%d" % i, [128, 8, 512]) for i in range(2)]
        ob = [sb(st, nc, "of%d" % i, [128, 8, 512]) for i in range(2)]
        sq = sb(st, nc, "sqf", [128, 8, 512])
        rs = sb(st, nc, "rsf", [128, 512])
        nf = sb(st, nc, "nf", [128, 8])
        pss = ps(st, nc, "pssf", [128, 512])
        P.dma("sync", nf[:], g.norm_final)
        xv = xsrc.rearrange("(fc p) t -> p fc t", p=128)
        ov = g.out.rearrange("(fc p) t -> p fc t", p=128)
        for bi, (t0, n) in enumerate(LAST_BLOCKS):
            x, o = xb[bi % 2], ob[bi % 2]
            P.dma("sync", x[:], xv[:, :, t0:t0 + n])
            P.act(sq[:], x[:], AF.Square)
            for fc in range(8):
                P.mm(pss[:], g.ONES[:], sq[:, fc, :], start=(fc == 0), stop=(fc == 7))
            P.act(rs[:], pss[:], AF.Sqrt, bias=g.EPSC[:, 0:1], scale=1.0 / D)
            P.recip(rs[:], rs[:])
            for fc in range(8):
                P.stt(o[:, fc, :], x[:, fc, :], nf[:, fc:fc + 1], rs[:], ALU.mult, ALU.mult)
            P.dma("sync", ov[:, :, t0 - CTX:t0 - CTX + n], o[:])
        P.flush()


def build(nc, debug=None):
    g = Ctx()
    dt = nc.dram_tensor

    def inp(name, shape, dtype=F32):
        return dt(name, list(shape), dtype, kind="ExternalInput").ap()

    def scr(name, shape, dtype=F32):
        return dt(name, list(shape), dtype, kind="Internal").ap()

    g.xin = inp("xin", [D, T])
    g.c2 = inp("c2", [128, 8, 2])
    g.mod_w = inp("mod_w", [DEPTH, D, 6 * D])
    g.mod_b = inp("mod_b", [DEPTH, 128, 48])
    g.norm_mix = inp("norm_mix", [DEPTH, 128, 8])
    g.norm_ffn = inp("norm_ffn", [DEPTH, 128, 8])
    g.norm_final = inp("norm_final", [128, 8])
    g.w_in = inp("w_in", [DEPTH, D, IN_COLS])
    g.w_branch = inp("w_branch", [DEPTH, 4, 256, D])
    g.w_out = inp("w_out", [DEPTH, D, D])
    g.moe_w_gate = inp("moe_w_gate", [DEPTH, 16, D, 512])
    g.moe_w_up = inp("moe_w_up", [DEPTH, 16, D, 512])
    g.moe_w_down = inp("moe_w_down", [DEPTH, 16, 512, D])
    g.ident_in = inp("ident_in", [128, 128])
    g.sel_in = inp("sel_in", [16, 16 * 128])
    g.w_router = inp("w_router", [DEPTH, 128, 8, 20])
    g.out = dt("out", [D, LOUT], F32, kind="ExternalOutput").ap()
    g.hy_conv_w = inp("hy_conv_w", [DEPTH, 128, 6, 3])
    g.hy_conv_b = inp("hy_conv_b", [DEPTH, 128, 6])
    g.hy_f_w1 = inp("hy_f_w1", [DEPTH, 17, 64])
    g.hy_f_w2 = inp("hy_f_w2", [DEPTH, 64, 64])
    g.hy_f_w3 = inp("hy_f_w3", [DEPTH, 64, 512])
    g.hy_fvec = inp("hy_fvec", [DEPTH, 64, 3])
    g.hy_bias = inp("hy_bias", [DEPTH, 128, 2])
    g.hy_lag0 = inp("hy_lag0", [1, 2])
    g.hy_tabs = {}
    for L_ in (CTX, LAT):
        nt_ = L_ // 128
        nb_ = min(512, L_)
        kq_ = min(8, nt_)
        g.hy_tabs[L_] = dict(
            feats=inp("hyt_feats%d" % L_, [17, L_]),
            dec=inp("hyt_dec%d" % L_, [L_, 256]),
            cf=inp("hyt_cf%d" % L_, [nt_, 128, nt_, 128], BF16),
            sf=inp("hyt_sf%d" % L_, [nt_, 128, nt_, 128], BF16),
            ci=inp("hyt_ci%d" % L_, [L_ // nb_, nt_ // kq_, 128, kq_, nb_], BF16),
            si=inp("hyt_si%d" % L_, [L_ // nb_, nt_ // kq_, 128, kq_, nb_], BF16),
        )
    g.s5_d = inp("s5_d", [DEPTH, 128, 2])
    g.s5_lam = inp("s5_lam", [DEPTH, 2, 8, 128, 3])
    g.s5_bre = inp("s5_bre", [DEPTH, 2, 8, 128, 128])
    g.s5_bim = inp("s5_bim", [DEPTH, 2, 8, 128, 128])
    g.s5_cre = inp("s5_cre", [DEPTH, 2, 8, 128, 128])
    g.s5_cim = inp("s5_cim", [DEPTH, 2, 8, 128, 128])
    g.s5_w_glu = inp("s5_w_glu", [DEPTH, 256, 256])
    g.rw_mu = inp("rw_mu", [DEPTH, 128, 8])
    g.rw_w2 = inp("rw_w2", [DEPTH, 64, 256])
    g.rw_a2 = inp("rw_a2", [DEPTH, 64, 256])
    g.rw_g2 = inp("rw_g2", [DEPTH, 64, 256])
    g.rw_pc = inp("rw_pc", [DEPTH, 128, 2, 8])
    g.rw_ln = inp("rw_ln", [DEPTH, 128, 2, 2])
    g.blk_in = inp("blk_in", [128, 128])
    g.sel64_in = inp("sel64_in", [64, 32, 64], BF16)
    g.rw_masks = inp("rw_masks", [128, 4, 64])
    g.rw_mbd = inp("rw_mbd", [128, 2, 128])
    g.rw_i2 = inp("rw_i2", [128, 64])
    g.RWY = scr("RWY", [256, T])
    g.RWS = scr("RWS", [960, T])
    g.RWG = scr("RWG", [512, T])
    g.TOKD = scr("TOKD", [2, 2, NCH, 64, 640], BF16)
    g.m2_conv_w = inp("m2_conv_w", [DEPTH, 128, 6, 3])
    g.m2_conv_b = inp("m2_conv_b", [DEPTH, 128, 6])
    g.m2_masks = inp("m2_masks", [64, 4, 64])
    g.m2_dt_bias = inp("m2_dt_bias", [DEPTH, 8])
    g.m2_a_log = inp("m2_a_log", [DEPTH, 8])
    g.m2_d = inp("m2_d", [DEPTH, 4])
    g.m2_norm_w = inp("m2_norm_w", [DEPTH, 128, 2])
    g.SXT = scr("SXT", [T, 776])
    g.SBC = scr("SBC", [512, T])
    g.SYF = scr("SYF", [2, T, 256])
    g.HU = scr("HU", [256, T])
    g.HX0 = scr("HX0", [256, T])
    g.XM16 = scr("XM16", [D, T], BF16)
    g.COMBD = scr("COMBD", [16, 2304])
    if debug == "ys":
        g.ys_dbg = inp("ys_dbg", [DEPTH, D, T])
    if debug == "mix":
        g.dbg = dt("dbg", [D, T], F32, kind="ExternalOutput").ap()
    elif debug and debug != "ys":
        g.dbg = dt("dbg", list(debug), F32, kind="ExternalOutput").ap()

    g.win16_all = scr("win16", [DEPTH, D, IN_COLS], BF16)
    g.wout16_all = scr("wout16", [DEPTH, D, D], BF16)
    g.wbr16_all = scr("wbr16", [DEPTH, D, D], BF16)
    g.wg16_all = scr("wg16", [DEPTH, 16, D, 512], BF16)
    g.wu16_all = scr("wu16", [DEPTH, 16, D, 512], BF16)
    g.wd16_all = scr("wd16", [DEPTH, 16, 512, D], BF16)
    g.PX = scr("PX", [NG_COLS, T])
    g.YS = scr("YS", [D, T])
    g.XA = scr("XA", [D, T])
    g.XB = scr("XB", [D, T])

    with contextlib.ExitStack() as st:
        P = Prog(nc, st)
        g.P = P
        g.ONES = sb(st, nc, "ONES", [128, 128])
        g.EPSC = sb(st, nc, "EPSC", [128, 1])
        g.MODT = sb(st, nc, "MODT", [128, 2, 48])
        g.S1 = sb(st, nc, "S1", [128, 2, 8])
        g.S2 = sb(st, nc, "S2", [128, 2, 8])
        g.NMIX = sb(st, nc, "NMIX", [128, 8])
        g.NFFN = sb(st, nc, "NFFN", [128, 8])
        g.IDENT = sb(st, nc, "IDENT", [128, 128])
        g.SEL = sb(st, nc, "SEL", [16, 16 * 128])
        P.dma("sync", g.IDENT[:], g.ident_in)
        P.dma("sync", g.SEL[:], g.sel_in)
        g.BLK = sb(st, nc, "BLK", [128, 128])
        g.SEL64 = sb(st, nc, "SEL64", [64, 32, 64], BF16)
        P.dma("sync", g.BLK[:], g.blk_in)
        P.dma("sync", g.SEL64[:], g.sel64_in)
        P.memset(g.ONES[:], 1.0)
        P.memset(g.EPSC[:], EPS)
        for l in range(DEPTH):
            xsrc = g.xin if l == 0 else g.XB
            if l == 0:
                stage_precast(P, nc, g, 0)
            else:
                set_layer_weights(g, l)
            g.prefetch_next = (l + 1) if (l + 1 < DEPTH and not debug) else None
            stage_mod(P, nc, g, l)
            stage_proj(P, nc, g, l, xsrc)
            if debug == "ys":
                for r in range(8):
                    P.dma("sync", g.YS[r * 128:(r + 1) * 128, :], g.ys_dbg[l, r * 128:(r + 1) * 128, :], wk=("YS",))
                P.flush()
            elif debug == "mix":
                stage_mixers(P, nc, g, l)
                break
            elif debug:
                break
            else:
                stage_mixers(P, nc, g, l)
            stage_merge(P, nc, g, l, xsrc, g.XA)
            stage_moe(P, nc, g, l, g.XA, g.XB)
        if debug == "mix":
            P.flush()
            for r in range(8):
                P.dma("sync", g.dbg[r * 128:(r + 1) * 128, :], g.YS[r * 128:(r + 1) * 128, :], rk=("YSall",))
        elif debug and debug != "ys":
            P.dma("sync", g.dbg, g.PX[0:debug[0], :], rk=("PXall",))
        else:
            stage_final(P, nc, g, g.XB)
        P.flush()
    return nc


_TABS = {}


def hyena_tables():
    if _TABS:
        return _TABS
    out = {}
    for L_ in (CTX, LAT):
        n = 2 * L_
        nt = L_ // 128
        nb = min(512, L_)
        kq = min(8, nt)
        nq = nt // kq
        d = np.arange(L_, dtype=np.float64)
        ang = 2.0 * np.pi * np.outer(d, d + 0.5) / n
        for nm, fn in (("c", np.cos), ("s", np.sin)):
            M = fn(ang)
            fwd = M.reshape(nt, 128, nt, 128).transpose(2, 1, 0, 3)
            out["hyt_%sf%d" % (nm, L_)] = np.ascontiguousarray(fwd).astype(ml_dtypes.bfloat16)
            MT = M.T
            inv = MT.reshape(nq, kq, 128, L_ // nb, nb).transpose(3, 0, 2, 1, 4)
            out["hyt_%si%d" % (nm, L_)] = np.ascontiguousarray(inv).astype(ml_dtypes.bfloat16)
        t = np.linspace(0.0, 1.0, L_, dtype=np.float32)[:, None]
        w = np.float32(2.0 * math.pi) * np.arange(L_, dtype=np.float32)[:, None] / np.float32(L_)
        fq = np.linspace(1e-4, 7, 8, dtype=np.float32)[None, :]
        feats = np.concatenate([t, np.cos(fq * w), -np.sin(fq * w)], axis=-1).astype(np.float32)
        out["hyt_feats%d" % L_] = np.ascontiguousarray(feats.T)
        deltas = np.abs(np.linspace(math.log(1e-2) / 1.5, math.log(1e-2) / 0.3, 256, dtype=np.float32))
        out["hyt_dec%d" % L_] = np.exp(-t * deltas).astype(np.float32)
    _TABS.update(out)
    return _TABS


def hyena_host(inputs):
    f = lambda a: np.ascontiguousarray(np.asarray(a, dtype=np.float32))
    o = dict(hyena_tables())
    o["hy_conv_w"] = f(np.asarray(inputs["hy_conv_w"]).reshape(DEPTH, 3, 6, 128).transpose(0, 3, 2, 1))
    o["hy_conv_b"] = f(np.asarray(inputs["hy_conv_b"]).reshape(DEPTH, 6, 128).transpose(0, 2, 1))
    o["hy_f_w1"] = f(inputs["hy_f_w1"])
    o["hy_f_w2"] = f(inputs["hy_f_w2"])
    o["hy_f_w3"] = f(inputs["hy_f_w3"])
    o["hy_fvec"] = f(np.stack([np.asarray(inputs["hy_f_b1"]), np.asarray(inputs["hy_f_b2"]), np.asarray(inputs["hy_f_freq"])], axis=-1))
    o["hy_bias"] = f(np.asarray(inputs["hy_bias"]).reshape(DEPTH, 2, 128).transpose(0, 2, 1))
    return o


def s5_host(inputs):
    f = lambda a: np.ascontiguousarray(np.asarray(a, dtype=np.float32))
    o = {}
    o["s5_d"] = f(np.asarray(inputs["s5_d"]).reshape(DEPTH, 2, 128).transpose(0, 2, 1))
    o["s5_w_glu"] = f(inputs["s5_w_glu"])
    lr = np.asarray(inputs["s5_lam_re"]).reshape(DEPTH, 2, 8, 128)
    li = np.asarray(inputs["s5_lam_im"]).reshape(DEPTH, 2, 8, 128)
    ls = np.repeat(np.asarray(inputs["s5_log_step"])[..., None], 64, axis=-1).reshape(DEPTH, 2, 8, 128)
    o["s5_lam"] = f(np.stack([lr, li, ls], axis=-1))
    for nm, src, tr in (("s5_bre", "s5_b_re", False), ("s5_bim", "s5_b_im", False), ("s5_cre", "s5_c_re", True), ("s5_cim", "s5_c_im", True)):
        a = np.asarray(inputs[src])
        if tr:
            a = a.transpose(0, 1, 2, 4, 3)
        pad = np.zeros((DEPTH, 2, 8, 2, 64, 128), np.float32)
        for gg in range(16):
            st_, g2 = gg // 2, gg % 2
            c0 = 16 * (gg % 8)
            pad[:, :, st_, g2, :, c0:c0 + 16] = a[:, :, gg]
        o[nm] = f(pad.reshape(DEPTH, 2, 8, 128, 128))
    return o


def rw_host(inputs):
    f = lambda a: np.ascontiguousarray(np.asarray(a, dtype=np.float32))
    o = {}
    mu = np.zeros((DEPTH, 1024), np.float32)
    mu[:, :960] = np.asarray(inputs["rw_mu"])
    o["rw_mu"] = f(mu.reshape(DEPTH, 8, 128).transpose(0, 2, 1))
    o["rw_w2"] = f(np.asarray(inputs["rw_w2"]).reshape(DEPTH, 64, 256))
    o["rw_a2"] = f(np.asarray(inputs["rw_a2"]).reshape(DEPTH, 64, 256))
    o["rw_g2"] = f(inputs["rw_g2"])
    chan = lambda a: np.asarray(a).reshape(DEPTH, 2, 128).transpose(0, 2, 1)
    w0 = np.asarray(inputs["rw_w0"])
    a0 = np.asarray(inputs["rw_a0"])
    cols = [chan(w0[:, 0]), chan(w0[:, 1]), chan(a0[:, 0]), chan(a0[:, 1]), chan(inputs["rw_k_k"]), chan(inputs["rw_k_a"]),
            chan(np.asarray(inputs["rw_r_k"]).reshape(DEPTH, 256)), np.zeros((DEPTH, 128, 2), np.float32)]
    o["rw_pc"] = f(np.stack(cols, axis=-1))
    o["rw_ln"] = f(np.stack([chan(inputs["rw_ln_w"]), chan(inputs["rw_ln_b"])], axis=-1))
    blk = np.zeros((128, 128), np.float32)
    blk[:64, :64] = 1.0
    blk[64:, 64:] = 1.0
    o["blk_in"] = blk
    sel = np.zeros((64, 32, 64), np.float32)
    for r in range(64):
        sel[r, r % 32, :] = 1.0
    o["sel64_in"] = sel.astype(ml_dtypes.bfloat16)
    a_ = np.arange(64)
    ms = np.stack([(a_[:, None] <= a_[None, :]), (a_[:, None] > a_[None, :]), (a_[:, None] >= a_[None, :]), (a_[:, None] < a_[None, :])], axis=1).astype(np.float32)
    o["rw_masks"] = np.ascontiguousarray(np.concatenate([ms, ms], axis=0))
    mbd = np.zeros((128, 2, 128), np.float32)
    for h in range(2):
        mbd[h * 64:(h + 1) * 64, 0, h * 64:(h + 1) * 64] = ms[:, 3, :]
        mbd[h * 64:(h + 1) * 64, 1, h * 64:(h + 1) * 64] = ms[:, 1, :]
    o["rw_mbd"] = mbd
    o["rw_i2"] = np.ascontiguousarray(np.concatenate([np.eye(64, dtype=np.float32)] * 2, axis=0))
    return o


def m2_host(inputs):
    f = lambda a: np.ascontiguousarray(np.asarray(a, dtype=np.float32))
    o = {}
    o["m2_conv_w"] = f(np.asarray(inputs["m2_conv_w"]).reshape(DEPTH, 3, 6, 128).transpose(0, 3, 2, 1))
    o["m2_conv_b"] = f(np.asarray(inputs["m2_conv_b"]).reshape(DEPTH, 6, 128).transpose(0, 2, 1))
    a = np.arange(64)
    le = (a[:, None] <= a[None, :]).astype(np.float32)
    gt = (a[:, None] > a[None, :]).astype(np.float32)
    ge = (a[:, None] >= a[None, :]).astype(np.float32)
    lt = (a[:, None] < a[None, :]).astype(np.float32)
    o["m2_masks"] = f(np.stack([le, gt, ge, lt], axis=1))
    o["m2_dt_bias"] = f(np.asarray(inputs["m2_dt_bias"]).reshape(DEPTH, 8))
    o["m2_a_log"] = f(np.asarray(inputs["m2_a_log"]).reshape(DEPTH, 8))
    o["m2_d"] = f(inputs["m2_d"])
    o["m2_norm_w"] = f(np.asarray(inputs["m2_norm_w"]).reshape(DEPTH, 2, 128).transpose(0, 2, 1))
    return o


def host_inputs(inputs, lag0=None):
    f = lambda a: np.ascontiguousarray(np.asarray(a, dtype=np.float32))
    shared = {}
    shared["mod_w"] = f(inputs["mod_w"])
    shared["mod_b"] = f(np.asarray(inputs["mod_b"]).reshape(DEPTH, 48, 128).transpose(0, 2, 1))
    shared["norm_mix"] = f(np.asarray(inputs["norm_mix"]).reshape(DEPTH, 8, 128).transpose(0, 2, 1))
    shared["norm_ffn"] = f(np.asarray(inputs["norm_ffn"]).reshape(DEPTH, 8, 128).transpose(0, 2, 1))
    shared["norm_final"] = f(np.asarray(inputs["norm_final"]).reshape(8, 128).T)
    shared["ident_in"] = np.eye(128, dtype=np.float32)
    sel = np.zeros((16, 16, 128), np.float32)
    for e in range(16):
        sel[e, e, :] = 1.0
    shared["sel_in"] = sel.reshape(16, 16 * 128)
    wr = np.concatenate([np.asarray(inputs["moe_w_group"]),
                         np.asarray(inputs["moe_w_expert"]).transpose(0, 2, 1, 3).reshape(DEPTH, D, 16)], axis=-1)
    shared["w_router"] = f(wr.reshape(DEPTH, 8, 128, 20).transpose(0, 2, 1, 3))
    for k in ("w_in", "w_branch", "w_out", "moe_w_gate", "moe_w_up", "moe_w_down"):
        shared[k] = f(inputs[k])
    shared["hy_lag0"] = np.array([[1.0, 0.0]], np.float32) if lag0 is None else lag0
    shared.update(hyena_host(inputs))
    shared.update(s5_host(inputs))
    shared.update(rw_host(inputs))
    shared.update(m2_host(inputs))
    maps = []
    for b in range(4):
        m = dict(shared)
        xcat = np.concatenate([np.asarray(inputs["ctx"][b]), np.asarray(inputs["x"][b])], axis=0)
        m["xin"] = f(xcat.T)
        c2 = np.stack([np.asarray(inputs["c"][b]).reshape(8, 128).T, np.asarray(inputs["c_ctx"]).reshape(8, 128).T], axis=-1)
        m["c2"] = f(c2)
        maps.append(m)
    return maps


def make_reversed(inputs):
    r = {k: np.asarray(v) for k, v in inputs.items()}
    r["x"] = r["x"][:, ::-1]
    r["ctx"] = r["ctx"][:, ::-1]
    idx = np.arange(IN_COLS)
    def swap(a, b, n):
        t = idx[a:a + n].copy(); idx[a:a + n] = idx[b:b + n]; idx[b:b + n] = t
    swap(1792, 1824, 32)
    swap(1856, 1888, 32)
    swap(3008, 3012, 4)
    r["w_in"] = r["w_in"][:, :, idx]
    mi = np.arange(960)
    for a_, b_ in ((768, 800), (832, 864)):
        t_ = mi[a_:a_ + 32].copy(); mi[a_:a_ + 32] = mi[b_:b_ + 32]; mi[b_:b_ + 32] = t_
    r["rw_mu"] = r["rw_mu"][:, mi]
    r["hy_conv_w"] = r["hy_conv_w"][:, ::-1]
    r["m2_conv_w"] = r["m2_conv_w"][:, ::-1]
    r["hy_f_w3"] = np.concatenate([r["hy_f_w3"][:, :, 256:], r["hy_f_w3"][:, :, :256]], axis=-1)
    for k in ("s5_lam_re", "s5_lam_im", "s5_log_step", "s5_b_re", "s5_b_im", "s5_c_re", "s5_c_im",
              "rw_w0", "rw_w2", "rw_a0", "rw_a2", "m2_a_log", "m2_dt_bias"):
        r[k] = r[k][:, ::-1]
    return r


def kernel(**inputs):
    nc = bass.Bass("TRN2", target_bir_lowering=False)
    build(nc)
    maps = host_inputs(inputs, np.array([[1.0, 0.0]], np.float32))
    maps += host_inputs(make_reversed(inputs), np.array([[0.0, 1.0]], np.float32))
    res = run_bass_kernel_spmd(nc, maps, core_ids=list(range(8)))
    out = np.empty((4, LAT, D), np.float32)
    for b in range(4):
        out[b, :LOUT] = res.results[b]["out"].T
        out[b, LOUT:] = res.results[4 + b]["out"].T[::-1]
    return out
```

```python
import contextlib
import math
import numpy as np
import ml_dtypes
import concourse.bass as bass
import concourse.mybir as mybir
from concourse.bass_utils import run_bass_kernel_spmd

F32 = mybir.dt.float32
BF16 = mybir.dt.bfloat16
AF = mybir.ActivationFunctionType
ALU = mybir.AluOpType
AX = mybir.AxisListType

D = 1024
LAT = 4096
CTX = 256
T = LAT + CTX
DEPTH = 2
IN_COLS = 7112
NG_COLS = 3016
EPS = 1e-6
SEGS = ((0, CTX), (CTX, T))
BLOCKS = [(0, CTX)] + [(CTX + i * 512, 512) for i in range(8)]
LAST_BLOCKS = [(CTX + i * 512, 512) for i in range(4)]
LOUT = 2048

COMPUTE = ("tensor", "vector", "scalar", "gpsimd")
DMAQ = ("sync", "gpsimd", "scalar")
NSLOT = 12
EPOCH = 30000
NEPOCH = 12


def _key(k):
    if isinstance(k, (str, tuple, int)):
        return k
    t = getattr(k, "tensor", None)
    if t is not None:
        return t.name
    return getattr(k, "name", None) or id(k)


F32R_ON = [True]


class Prog:
    def __init__(self, nc, st):
        self.nc = nc
        self.q = {e: [] for e in ("tensor", "vector", "scalar", "gpsimd", "sync")}
        self.ccnt = {e: 0 for e in COMPUTE}
        self.dcnt = {e: 0 for e in DMAQ}
        self.dslot_tok = {}
        self.waited = {e: {} for e in self.q}
        self.lastw = {}
        self.readers = {}
        self.n_inst = 0
        self.sems = {}
        for e in COMPUTE:
            for i in range(NEPOCH):
                sn = "c_%s_%d" % (e, i)
                self.sems[sn] = st.enter_context(nc.semaphore(sn))
        for qn in DMAQ:
            for i in range(NSLOT):
                sn = "d_%s_%d" % (qn, i)
                self.sems[sn] = st.enter_context(nc.semaphore(sn))

    def _deps(self, reads, writes):
        toks = []
        for k in reads:
            t = self.lastw.get(k)
            if t is not None:
                toks.append(t)
        for k in writes:
            t = self.lastw.get(k)
            if t is not None:
                toks.append(t)
            toks.extend(self.readers.get(k, ()))
        return toks

    def _emit_waits(self, eng, toks, is_dma_issue):
        best = {}
        for (sn, val, e, isd) in toks:
            if (not isd) and e == eng and eng == "tensor" and not is_dma_issue:
                continue
            if self.waited[eng].get(sn, 0) >= val:
                continue
            if best.get(sn, 0) < val:
                best[sn] = val
        for sn, val in best.items():
            self.waited[eng][sn] = val
            self.q[eng].append(("wait", sn, val))

    def _record(self, tok, reads, writes):
        for k in writes:
            self.lastw[k] = tok
            self.readers[k] = []
        for k in reads:
            lst = self.readers.setdefault(k, [])
            lst.append(tok)
            if len(lst) > 16:
                d = {}
                for t in lst:
                    if d.get(t[0], (0, 0))[1] < t[1]:
                        d[t[0]] = t
                self.readers[k] = list(d.values())

    def op(self, eng, fn, reads=(), writes=()):
        reads = [_key(k) for k in reads if k is not None and not isinstance(k, (float,))]
        writes = [_key(k) for k in writes]
        toks = self._deps(reads, writes)
        self._emit_waits(eng, toks, False)
        c = self.ccnt[eng]
        self.ccnt[eng] += 1
        sn = "c_%s_%d" % (eng, c // EPOCH)
        tok = (sn, c % EPOCH + 1, eng, False)
        self.q[eng].append(("op", fn, sn, 1))
        self._record(tok, reads, writes)
        self.n_inst += 1
        return tok

    def dma(self, q, out, in_, rk=None, wk=None, **kw):
        reads = [_key(in_) if rk is None else rk]
        writes = [_key(out) if wk is None else wk]
        i = self.dcnt[q]
        self.dcnt[q] += 1
        slot = i % NSLOT
        sn = "d_%s_%d" % (q, slot)
        val = 16 * (i // NSLOT + 1)
        toks = self._deps(reads, writes)
        prev = self.dslot_tok.get(sn)
        if prev is not None:
            toks.append(prev)
        self._emit_waits(q, toks, True)
        tok = (sn, val, q, True)
        self.dslot_tok[sn] = tok
        self.q[q].append(("op", lambda e, o=out, s=in_, k=kw: e.dma_start(out=o, in_=s, **k), sn, 16))
        self._record(tok, reads, writes)
        self.n_inst += 1
        return tok

    def flush(self):
        for qn in ("sync", "gpsimd", "scalar"):
            toks = [t for t in self.dslot_tok.values() if t[2] == qn]
            self._emit_waits(qn, toks, True)
        last = []
        for e in COMPUTE:
            c = self.ccnt[e]
            if c > 0:
                last.append(("c_%s_%d" % (e, (c - 1) // EPOCH), (c - 1) % EPOCH + 1, e, False))
        for e in self.q:
            self._emit_waits(e, [t for t in last if t[2] != e] + list(self.dslot_tok.values()), True)
        nc = self.nc
        sems = self.sems
        with nc.Block() as block:
            def run(engname):
                items = self.q[engname]

                def body(e):
                    for it in items:
                        if it[0] == "wait":
                            e.wait_ge(sems[it[1]], it[2])
                        else:
                            it[1](e).then_inc(sems[it[2]], it[3])
                return body
            block.sync(run("sync"))
            block.scalar(run("scalar"))
            block.vector(run("vector"))
            block.gpsimd(run("gpsimd"))
            block.tensor(run("tensor"))
        self.q = {e: [] for e in self.q}

    def mm(self, out, lhsT, rhs, start=True, stop=True, r=False):
        return self.op("tensor", lambda e: e.matmul(out, lhsT, rhs, start=start, stop=stop),
                       reads=[lhsT, rhs], writes=[out])

    def tr(self, out, in_, ident):
        return self.op("tensor", lambda e: e.transpose(out, in_, ident), reads=[in_, ident], writes=[out])

    def act(self, out, in_, func, bias=None, scale=None, accum_out=None, eng="scalar"):
        kw = {}
        rd = [in_]
        if bias is not None:
            kw["bias"] = bias
            if not isinstance(bias, float):
                rd.append(bias)
        if scale is not None:
            kw["scale"] = scale
            if not isinstance(scale, float):
                rd.append(scale)
        wr = [out]
        if accum_out is not None:
            kw["accum_out"] = accum_out
            wr.append(accum_out)
        return self.op("scalar", lambda e: e.activation(out, in_, func, **kw), reads=rd, writes=wr)

    def tt(self, out, in0, in1, op, eng="vector"):
        return self.op(eng, lambda e: e.tensor_tensor(out=out, in0=in0, in1=in1, op=op), reads=[in0, in1], writes=[out])

    def ts(self, out, in0, s1, s2=None, op0=ALU.mult, op1=None, eng="vector", accum_out=None):
        rd = [in0] + [s for s in (s1, s2) if s is not None and not isinstance(s, (float, int))]
        kw = {}
        if op1 is not None:
            kw["op1"] = op1
        wr = [out]
        if accum_out is not None:
            kw["accum_out"] = accum_out
            wr.append(accum_out)
        return self.op(eng, lambda e: e.tensor_scalar(out=out, in0=in0, scalar1=s1, scalar2=s2, op0=op0, **kw), reads=rd, writes=wr)

    def stt(self, out, in0, scalar, in1, op0, op1):
        rd = [in0, in1] + ([scalar] if not isinstance(scalar, (float, int)) else [])
        return self.op("vector", lambda e: e.scalar_tensor_tensor(out=out, in0=in0, scalar=scalar, in1=in1, op0=op0, op1=op1),
                       reads=rd, writes=[out])

    def copy(self, out, in_, eng="vector"):
        if eng == "scalar":
            return self.op("scalar", lambda e: e.copy(out, in_), reads=[in_], writes=[out])
        return self.op(eng, lambda e: e.tensor_copy(out=out, in_=in_), reads=[in_], writes=[out])

    def memset(self, ap, v, eng="vector"):
        return self.op(eng, lambda e: e.memset(ap, v), reads=[], writes=[ap])

    def recip(self, out, in_):
        return self.op("vector", lambda e: e.reciprocal(out=out, in_=in_), reads=[in_], writes=[out])

    def reduce(self, out, in_, op, axis=AX.X):
        return self.op("vector", lambda e: e.tensor_reduce(out=out, in_=in_, axis=axis, op=op), reads=[in_], writes=[out])


class Ctx:
    pass


_UID = [0]


def sb(st, nc, name, shape, dt=F32):
    _UID[0] += 1
    return st.enter_context(nc.sbuf_tensor("s%d_%s" % (_UID[0], name), list(shape), dt))


def ps(st, nc, name, shape, dt=F32):
    _UID[0] += 1
    return st.enter_context(nc.psum_tensor("p%d_%s" % (_UID[0], name), list(shape), dt))


def set_layer_weights(g, l):
    g.win16, g.wout16, g.wbr16 = g.win16_all[l], g.wout16_all[l], g.wbr16_all[l]
    g.wg16, g.wu16, g.wd16 = g.wg16_all[l], g.wu16_all[l], g.wd16_all[l]


def stage_precast(P, nc, g, l, flush=True):
    set_layer_weights(g, l)
    todo = []
    for r in range(8):
        todo.append((g.win16[r * 128:(r + 1) * 128, :], g.w_in[l, r * 128:(r + 1) * 128, :], ("win16", l, r)))
        todo.append((g.wout16[r * 128:(r + 1) * 128, :], g.w_out[l, r * 128:(r + 1) * 128, :], ("wout16", l, r)))
        todo.append((g.wbr16[r * 128:(r + 1) * 128, :],
                     g.w_branch[l].rearrange("i k n -> (i k) n")[r * 128:(r + 1) * 128, :], ("wbr16", l, r)))
    for e in range(16):
        for r in range(8):
            todo.append((g.wg16[e, r * 128:(r + 1) * 128, :], g.moe_w_gate[l, e, r * 128:(r + 1) * 128, :], ("wg16", l, e, r)))
            todo.append((g.wu16[e, r * 128:(r + 1) * 128, :], g.moe_w_up[l, e, r * 128:(r + 1) * 128, :], ("wu16", l, e, r)))
        for r in range(4):
            todo.append((g.wd16[e, r * 128:(r + 1) * 128, :], g.moe_w_down[l, e, r * 128:(r + 1) * 128, :], ("wd16", l, e, r)))
    if not flush:
        return todo
    for (o, i, k) in todo[:24]:
        P.dma("gpsimd", o, i, wk=k)
    P.flush()
    g.pending_bg = todo[24:]
    return []


def stage_mod(P, nc, g, l):
    with contextlib.ExitStack() as st:
        c2 = sb(st, nc, "c2", [128, 8, 2])
        sc2 = sb(st, nc, "sc2", [128, 8, 2])
        mb = sb(st, nc, "mb", [128, 48])
        mw = [sb(st, nc, "mw%d" % i, [128, 8, 1024]) for i in range(2)]
        pm = ps(st, nc, "pm", [128, 48, 2])
        P.dma("sync", c2[:], g.c2)
        P.dma("sync", mb[:], g.mod_b[l])
        P.act(sc2[:], c2[:], AF.Silu)
        for jb in range(6):
            w = mw[jb % 2]
            P.dma("sync", w[:], g.mod_w[l].rearrange("(kc p) n -> p kc n", p=128)[:, :, jb * 1024:(jb + 1) * 1024])
            for j in range(8):
                for kc in range(8):
                    P.mm(pm[:, jb * 8 + j, :], w[:, kc, j * 128:(j + 1) * 128], sc2[:, kc, :], start=(kc == 0), stop=(kc == 7))
        M = g.MODT
        for s in range(2):
            P.tt(M[:, s, :], pm[:, :, s], mb[:], ALU.add)
        P.dma("sync", g.NMIX[:], g.norm_mix[l])
        P.dma("sync", g.NFFN[:], g.norm_ffn[l])
        for s in range(2):
            P.stt(g.S1[:, s, :], M[:, s, 8:16], 1.0, g.NMIX[:], ALU.add, ALU.mult)
            P.stt(g.S2[:, s, :], M[:, s, 32:40], 1.0, g.NFFN[:], ALU.add, ALU.mult)
        P.flush()


def modulate_block(P, nc, g, xb, t0, n, s, scale, shift, out_bf, sq, pss, rs, tmp, out_f32=None):
    P.act(sq[:, :, :n], xb[:, :, :n], AF.Square)
    for fc in range(8):
        P.mm(pss[:, :n], g.ONES16[:], sq[:, fc, :n], start=(fc == 0), stop=(fc == 7))
    P.act(rs[:, :n], pss[:, :n], AF.Sqrt, bias=g.EPSC[:, 0:1], scale=1.0 / D)
    P.recip(rs[:, :n], rs[:, :n])
    for fc in range(8):
        P.tt(tmp[:, :n], xb[:, fc, :n], rs[:, :n], ALU.mult)
        sh = shift[:, fc:fc + 1] if shift is not None else 0.0
        if out_f32 is not None:
            P.act(out_f32[:, fc, :n], tmp[:, :n], AF.Identity, bias=sh, scale=scale[:, fc:fc + 1])
            P.copy(out_bf[:, fc, t0:t0 + n], out_f32[:, fc, :n], eng="gpsimd")
        else:
            P.act(out_bf[:, fc, t0:t0 + n], tmp[:, :n], AF.Identity, bias=sh, scale=scale[:, fc:fc + 1])


def stage_proj(P, nc, g, l, xsrc):
    with contextlib.ExitStack() as st0:
      g.XM = sb(st0, nc, "XM", [128, 8, T], BF16)
      with contextlib.ExitStack() as st:
        xb = [sb(st, nc, "xb%d" % i, [128, 8, 512]) for i in range(2)]
        sq = sb(st, nc, "sq", [128, 8, 512], BF16)
        rs = sb(st, nc, "rs", [128, 512])
        tmp = sb(st, nc, "tmp", [128, 512])
        pss = ps(st, nc, "pss", [128, 512])
        xv = xsrc.rearrange("(fc p) t -> p fc t", p=128)
        for bi, (t0, n) in enumerate(BLOCKS):
            s = 1 if t0 < CTX else 0
            x = xb[bi % 2]
            P.dma("sync", x[:, :, :n], xv[:, :, t0:t0 + n])
            modulate_block(P, nc, g, x, t0, n, s, g.S1[:, s, :], g.MODT[:, s, 0:8], g.XM, sq, pss, rs, tmp)
        P.dma("sync", g.XM16.rearrange("(fc p) t -> p fc t", p=128), g.XM[:])
        P.flush()
      with contextlib.ExitStack() as st:
          wb = [sb(st, nc, "wb%d" % i, [128, 8, 128], BF16) for i in range(2)]
          ob = [sb(st, nc, "ob%d" % i, [128, T]) for i in range(2)]
          pp = [ps(st, nc, "pp%d" % i, [128, 512]) for i in range(4)]
          wv = g.win16.rearrange("(fc p) n -> p fc n", p=128)
          ci = 0
          k = 0
          for c0 in range(0, NG_COLS, 128):
              m = min(128, NG_COLS - c0)
              w = wb[ci % 2]
              o = ob[ci % 2]
              P.dma("sync", w[:, :, :m], wv[:, :, c0:c0 + m], rk=("win16",))
              for (t0, n) in BLOCKS:
                  p_ = pp[k % 4]
                  k += 1
                  for fc in range(8):
                      P.mm(p_[:m, :n], w[:, fc, :m], g.XM[:, fc, t0:t0 + n], start=(fc == 0), stop=(fc == 7))
                  if k % 2:
                      P.copy(o[:m, t0:t0 + n], p_[:m, :n], eng="scalar")
                  else:
                      P.copy(o[:m, t0:t0 + n], p_[:m, :n], eng="vector")
              P.dma("sync", g.PX[c0:c0 + m, :], o[:m, :], wk=("PX", c0 // 128))
              ci += 1
          P.flush()


def conv3(P, y, x, w, b):
    P.ts(y[:], x[:], w[:, 1:2], b, op0=ALU.mult, op1=ALU.add)
    for (a, e) in SEGS:
        P.stt(y[:, a + 1:e], x[:, a:e - 1], w[:, 0:1], y[:, a + 1:e], ALU.mult, ALU.add)
        P.stt(y[:, a:e - 1], x[:, a + 1:e], w[:, 2:3], y[:, a:e - 1], ALU.mult, ALU.add)


def sin_wrap(P, out, tmp, m, pz, fr, frb):
    P.ts(tmp, pz, fr, frb, op0=ALU.mult, op1=ALU.add)
    P.ts(m, tmp, math.pi, -2.0 * math.pi, op0=ALU.is_gt, op1=ALU.mult)
    P.tt(out, tmp, m, ALU.add)
    P.ts(m, tmp, -math.pi, 2.0 * math.pi, op0=ALU.is_lt, op1=ALU.mult)
    P.tt(out, out, m, ALU.add)
    P.act(out, out, AF.Sin)


def hyena_seq(P, nc, g, l, L, toff, UT):
    nt = L // 128
    nb = min(512, L)
    kq = min(8, nt)
    nq = nt // kq
    tabs = g.hy_tabs[L]
    with contextlib.ExitStack() as st:
        HR = sb(st, nc, "HR", [128, nt, 256])
        HI = sb(st, nc, "HI", [128, nt, 256])
        with contextlib.ExitStack() as st2:
            HS = sb(st2, nc, "HS", [128, nt, 256], BF16)
            HD = sb(st2, nc, "HD", [128, nt, 256], BF16)
            with contextlib.ExitStack() as st3:
                feats = sb(st3, nc, "feats", [17, L])
                w1 = sb(st3, nc, "fw1", [17, 64])
                w2 = sb(st3, nc, "fw2", [64, 64])
                w3 = sb(st3, nc, "fw3", [64, 512])
                fv = sb(st3, nc, "fv", [64, 6])
                h1 = sb(st3, nc, "fh1", [64, L])
                h2 = sb(st3, nc, "fh2", [64, L])
                tmp = sb(st3, nc, "ftmp", [64, 512])
                mm_ = sb(st3, nc, "fm", [64, 512])
                dec = [sb(st3, nc, "fdec%d" % i, [128, 256]) for i in range(2)]
                hf = sb(st3, nc, "fhf", [128, 256])
                hb = sb(st3, nc, "fhb", [128, 256])
                pz = ps(st3, nc, "fpz", [64, 512])
                ph = [ps(st3, nc, "fph%d" % i, [128, 512]) for i in range(2)]
                lag0 = sb(st3, nc, "flag0", [1, 2])
                P.dma("sync", lag0[:], g.hy_lag0)
                P.dma("sync", feats[:], tabs["feats"])
                P.dma("sync", w1[:], g.hy_f_w1[l])
                P.dma("sync", w2[:], g.hy_f_w2[l])
                P.dma("sync", w3[:], g.hy_f_w3[l])
                P.dma("sync", fv[:, 0:3], g.hy_fvec[l])
                P.tt(fv[:, 3:4], fv[:, 2:3], fv[:, 0:1], ALU.mult)
                P.tt(fv[:, 4:5], fv[:, 2:3], fv[:, 1:2], ALU.mult)
                for b0 in range(0, L, 512):
                    n = min(512, L - b0)
                    P.mm(pz[:, :n], w1[:], feats[:, b0:b0 + n])
                    sin_wrap(P, h1[:, b0:b0 + n], tmp[:, :n], mm_[:, :n], pz[:, :n], fv[:, 2:3], fv[:, 3:4])
                for b0 in range(0, L, 512):
                    n = min(512, L - b0)
                    P.mm(pz[:, :n], w2[:], h1[:, b0:b0 + n])
                    sin_wrap(P, h2[:, b0:b0 + n], tmp[:, :n], mm_[:, :n], pz[:, :n], fv[:, 2:3], fv[:, 4:5])
                for lt in range(nt):
                    p_ = ph[lt % 2]
                    d_ = dec[lt % 2]
                    P.mm(p_[:], h2[:, lt * 128:(lt + 1) * 128], w3[:])
                    P.dma("sync", d_[:], tabs["dec"][lt * 128:(lt + 1) * 128, :])
                    P.tt(hf[:], p_[:, 0:256], d_[:], ALU.mult)
                    P.tt(hb[:], p_[:, 256:512], d_[:], ALU.mult)
                    if lt == 0:
                        P.ts(hf[0:1, :], hf[0:1, :], lag0[0:1, 0:1], None, op0=ALU.mult)
                        P.ts(hb[0:1, :], hb[0:1, :], lag0[0:1, 1:2], None, op0=ALU.mult)
                    P.tt(HS[:, lt, :], hf[:], hb[:], ALU.add)
                    P.tt(HD[:, lt, :], hf[:], hb[:], ALU.subtract)
                P.flush()
            with contextlib.ExitStack() as st3:
                ct = [sb(st3, nc, "ct%d" % i, [128, nt, 128], BF16) for i in range(2)]
                sn = [sb(st3, nc, "sn%d" % i, [128, nt, 128], BF16) for i in range(2)]
                pr = [ps(st3, nc, "hpr%d" % i, [128, 512]) for i in range(2)]
                pi = [ps(st3, nc, "hpi%d" % i, [128, 512]) for i in range(2)]
                for kt in range(nt):
                    c_, s_ = ct[kt % 2], sn[kt % 2]
                    P.dma("sync", c_[:], tabs["cf"][kt])
                    P.dma("sync", s_[:], tabs["sf"][kt])
                    for dc in range(nt):
                        P.mm(pr[kt % 2][:, :256], c_[:, dc, :], HS[:, dc, :], start=(dc == 0), stop=(dc == nt - 1))
                    for dc in range(nt):
                        P.mm(pi[kt % 2][:, :256], s_[:, dc, :], HD[:, dc, :], start=(dc == 0), stop=(dc == nt - 1))
                    P.copy(HR[:, kt, :], pr[kt % 2][:, :256], eng="scalar")
                    P.copy(HI[:, kt, :], pi[kt % 2][:, :256], eng="vector")
                P.flush()
        YR = sb(st, nc, "YR", [128, nt, 256], BF16)
        YI = sb(st, nc, "YI", [128, nt, 256], BF16)
        with contextlib.ExitStack() as st3:
            ct = [sb(st3, nc, "uct%d" % i, [128, nt, 128], BF16) for i in range(2)]
            sn = [sb(st3, nc, "usn%d" % i, [128, nt, 128], BF16) for i in range(2)]
            t1 = sb(st3, nc, "ut1", [128, 256])
            t2 = sb(st3, nc, "ut2", [128, 256])
            pr = [ps(st3, nc, "upr%d" % i, [128, 512]) for i in range(2)]
            pi = [ps(st3, nc, "upi%d" % i, [128, 512]) for i in range(2)]
            tc0 = toff // 128
            for kt in range(nt):
                c_, s_ = ct[kt % 2], sn[kt % 2]
                P.dma("sync", c_[:], tabs["cf"][kt])
                P.dma("sync", s_[:], tabs["sf"][kt])
                a_, b_ = pr[kt % 2], pi[kt % 2]
                for dc in range(nt):
                    P.mm(a_[:, :256], c_[:, dc, :], UT[:, tc0 + dc, :], start=(dc == 0), stop=(dc == nt - 1))
                for dc in range(nt):
                    P.mm(b_[:, :256], s_[:, dc, :], UT[:, tc0 + dc, :], start=(dc == 0), stop=(dc == nt - 1))
                P.tt(t1[:], a_[:, :256], HR[:, kt, :], ALU.mult)
                P.tt(t2[:], b_[:, :256], HI[:, kt, :], ALU.mult)
                P.tt(YR[:, kt, :], t1[:], t2[:], ALU.subtract)
                P.tt(t1[:], a_[:, :256], HI[:, kt, :], ALU.mult)
                P.tt(t2[:], b_[:, :256], HR[:, kt, :], ALU.mult)
                P.tt(YI[:, kt, :], t1[:], t2[:], ALU.add)
            P.flush()
        with contextlib.ExitStack() as st3:
            ci = [sb(st3, nc, "ci%d" % i, [128, kq, nb], BF16) for i in range(2)]
            si = [sb(st3, nc, "si%d" % i, [128, kq, nb], BF16) for i in range(2)]
            ub = sb(st3, nc, "iub", [128, nb])
            x0 = sb(st3, nc, "ix0", [128, nb])
            yo = sb(st3, nc, "iyo", [128, nb])
            hbias = sb(st3, nc, "ihb", [128, 2])
            py = [ps(st3, nc, "ipy%d" % i, [128, 512]) for i in range(2)]
            P.dma("sync", hbias[:], g.hy_bias[l])
            kk = 0
            for tb in range((LOUT // nb) if (l == DEPTH - 1 and L == LAT) else (L // nb)):
                for q in range(nq):
                    c_, s_ = ci[kk % 2], si[kk % 2]
                    kk += 1
                    P.dma("sync", c_[:], tabs["ci"][tb, q])
                    P.dma("sync", s_[:], tabs["si"][tb, q])
                    for k2 in range(kq):
                        kc = q * kq + k2
                        for cj in range(2):
                            P.mm(py[cj][:, :nb], YR[:, kc, cj * 128:(cj + 1) * 128], c_[:, k2, :], start=(kc == 0), stop=False)
                            P.mm(py[cj][:, :nb], YI[:, kc, cj * 128:(cj + 1) * 128], s_[:, k2, :], start=False, stop=(kc == nt - 1))
                for cj in range(2):
                    c0 = toff + tb * nb
                    P.dma("sync", ub[:], g.HU[cj * 128:(cj + 1) * 128, c0:c0 + nb], rk=("HU",))
                    P.dma("sync", x0[:], g.HX0[cj * 128:(cj + 1) * 128, c0:c0 + nb], rk=("HX0",))
                    P.ts(ub[:], ub[:], hbias[:, cj:cj + 1], None, op0=ALU.mult)
                    P.stt(yo[:], py[cj][:, :nb], 2.0 / (2 * L), ub[:], ALU.mult, ALU.add)
                    P.tt(yo[:], yo[:], x0[:], ALU.mult)
                    P.dma("sync", g.YS[cj * 128:(cj + 1) * 128, c0:c0 + nb], yo[:], wk=("YS", 0, cj, c0))
            P.flush()


def stage_hyena(P, nc, g, l):
    with contextlib.ExitStack() as st:
        UT = sb(st, nc, "UT", [128, T // 128, 256], BF16)
        with contextlib.ExitStack() as st2:
            cw = sb(st2, nc, "hcw", [128, 6, 3])
            cb = sb(st2, nc, "hcb", [128, 6])
            xi = [sb(st2, nc, "hxi%d" % i, [128, T]) for i in range(3)]
            y = [sb(st2, nc, "hyy%d" % i, [128, T]) for i in range(3)]
            pt = [ps(st2, nc, "hpt%d" % i, [128, 128]) for i in range(2)]
            P.dma("sync", cw[:], g.hy_conv_w[l])
            P.dma("sync", cb[:], g.hy_conv_b[l])
            for j in range(2):
                for a in range(3):
                    ti = a * 2 + j
                    r0 = a * 256 + j * 128
                    P.dma("sync", xi[a][:], g.PX[r0:r0 + 128, :], rk=("PX", r0 // 128))
                    conv3(P, y[a], xi[a], cw[:, ti, :], cb[:, ti:ti + 1])
                P.tt(y[1][:], y[1][:], y[2][:], ALU.mult)
                P.dma("sync", g.HX0[j * 128:(j + 1) * 128, :], y[0][:], wk=("HX0",))
                P.dma("sync", g.HU[j * 128:(j + 1) * 128, :], y[1][:], wk=("HU",))
                for tc in range(T // 128):
                    p_ = pt[tc % 2]
                    P.tr(p_[:], y[1][:, tc * 128:(tc + 1) * 128], g.IDENT[:])
                    P.copy(UT[:, tc, j * 128:(j + 1) * 128], p_[:], eng=("scalar" if tc % 2 else "vector"))
            P.flush()
        if l < DEPTH - 1:
            hyena_seq(P, nc, g, l, CTX, 0, UT)
        hyena_seq(P, nc, g, l, LAT, CTX, UT)


def issue_bg(P, g):
    for (o, i, k) in getattr(g, "pending_bg", []):
        P.dma("gpsimd", o, i, wk=k)
    g.pending_bg = []


def stage_s5(P, nc, g, l):
    Q = 512
    PI = math.pi
    issue_bg(P, g)
    with contextlib.ExitStack() as st:
        U = sb(st, nc, "s5U", [128, 2, T])
        YA = sb(st, nc, "s5YA", [128, 2, T])
        dsk = sb(st, nc, "s5d", [128, 2])
        ONQ = sb(st, nc, "s5on", [128, Q])
        COS = sb(st, nc, "s5cos", [128, Q])
        SIN = sb(st, nc, "s5sin", [128, Q])
        RHO = sb(st, nc, "s5rho", [128, Q])
        pv_ = [sb(st, nc, "s5pv%d" % i, [128, 3]) for i in range(2)]
        v = sb(st, nc, "s5v", [128, 24])
        m = sb(st, nc, "s5m", [128, 2])
        Bre_ = [sb(st, nc, "s5Bre%d" % i, [128, 128]) for i in range(2)]
        Bim_ = [sb(st, nc, "s5Bim%d" % i, [128, 128]) for i in range(2)]
        BR = sb(st, nc, "s5BR", [128, 128])
        BI = sb(st, nc, "s5BI", [128, 128])
        BRT = sb(st, nc, "s5BRT", [128, 128])
        BIT = sb(st, nc, "s5BIT", [128, 128])
        CRe_ = [sb(st, nc, "s5CRe%d" % i, [128, 128]) for i in range(2)]
        CIm_ = [sb(st, nc, "s5CIm%d" % i, [128, 128]) for i in range(2)]
        tq = sb(st, nc, "s5tq", [128, Q])
        t1 = sb(st, nc, "s5t1", [128, Q])
        t2 = sb(st, nc, "s5t2", [128, Q])
        t3 = sb(st, nc, "s5t3", [128, Q])
        t4 = sb(st, nc, "s5t4", [128, Q])
        t5 = sb(st, nc, "s5t5", [128, Q])
        t6 = sb(st, nc, "s5t6", [128, Q])
        Wr = sb(st, nc, "s5Wr", [128, Q])
        Wi = sb(st, nc, "s5Wi", [128, Q])
        Zr = sb(st, nc, "s5Zr", [128, Q])
        Zi = sb(st, nc, "s5Zi", [128, Q])
        XR = [sb(st, nc, "s5XR%d" % i, [128, Q]) for i in range(2)]
        XI = [sb(st, nc, "s5XI%d" % i, [128, Q]) for i in range(2)]
        z0 = [sb(st, nc, "s5z0%d" % i, [128, 2]) for i in range(2)]
        AS = [sb(st, nc, "s5AS%d" % i, [128, Q]) for i in range(2)]
        BS = [sb(st, nc, "s5BS%d" % i, [128, Q]) for i in range(2)]
        pt = ps(st, nc, "s5pt", [128, 128])
        pbr = [ps(st, nc, "s5pbr%d" % i, [128, 512]) for i in range(2)]
        pbi = [ps(st, nc, "s5pbi%d" % i, [128, 512]) for i in range(2)]
        py = [ps(st, nc, "s5py%d" % i, [128, 512]) for i in range(2)]
        for ut in range(2):
            P.dma("sync", U[:, ut, :], g.PX[768 + ut * 128:768 + (ut + 1) * 128, :], rk=("PX", 6 + ut))
        P.dma("sync", dsk[:], g.s5_d[l])
        P.memset(ONQ[:], 1.0)
        for ut in range(2):
            P.ts(YA[:, ut, :], U[:, ut, :], dsk[:, ut:ut + 1], None, op0=ALU.mult)
        kb = 0
        for d in range(2):
            for s_ in range(8):
                ut = s_ // 4
                pv, Bre, Bim, CRe, CIm = (t_[(d * 8 + s_) % 2] for t_ in (pv_, Bre_, Bim_, CRe_, CIm_))
                P.dma("sync", pv[:], g.s5_lam[l, d, s_])
                P.dma("sync", Bre[:], g.s5_bre[l, d, s_])
                P.dma("sync", Bim[:], g.s5_bim[l, d, s_])
                P.dma("sync", CRe[:], g.s5_cre[l, d, s_])
                P.dma("sync", CIm[:], g.s5_cim[l, d, s_])
                P.ts(CIm[:], CIm[:], -1.0, None, op0=ALU.mult)
                P.act(v[:, 0:1], pv[:, 2:3], AF.Exp)
                P.tt(v[:, 1:2], pv[:, 0:1], v[:, 0:1], ALU.mult)
                P.tt(v[:, 2:3], pv[:, 1:2], v[:, 0:1], ALU.mult)
                P.act(v[:, 3:4], v[:, 1:2], AF.Exp)
                P.copy(v[:, 4:5], v[:, 2:3])
                P.ts(v[:, 5:6], v[:, 2:3], PI / 2, None, op0=ALU.add)
                for _ in range(5):
                    P.ts(m[:], v[:, 4:6], PI, -2.0 * PI, op0=ALU.is_gt, op1=ALU.mult)
                    P.tt(v[:, 4:6], v[:, 4:6], m[:], ALU.add)
                P.act(v[:, 6:8], v[:, 4:6], AF.Sin)
                P.stt(v[:, 8:9], v[:, 3:4], v[:, 7:8], ONQ[:, 0:1], ALU.mult, ALU.subtract)
                P.tt(v[:, 9:10], v[:, 3:4], v[:, 6:7], ALU.mult)
                P.tt(v[:, 13:14], pv[:, 0:1], pv[:, 0:1], ALU.mult)
                P.stt(v[:, 10:11], pv[:, 1:2], pv[:, 1:2], v[:, 13:14], ALU.mult, ALU.add)
                P.recip(v[:, 10:11], v[:, 10:11])
                P.tt(v[:, 13:14], v[:, 9:10], pv[:, 1:2], ALU.mult)
                P.stt(v[:, 11:12], v[:, 8:9], pv[:, 0:1], v[:, 13:14], ALU.mult, ALU.add)
                P.tt(v[:, 11:12], v[:, 11:12], v[:, 10:11], ALU.mult)
                P.tt(v[:, 13:14], v[:, 8:9], pv[:, 1:2], ALU.mult)
                P.stt(v[:, 12:13], v[:, 9:10], pv[:, 0:1], v[:, 13:14], ALU.mult, ALU.subtract)
                P.tt(v[:, 12:13], v[:, 12:13], v[:, 10:11], ALU.mult)
                P.ts(BR[:], Bim[:], v[:, 12:13], None, op0=ALU.mult)
                P.stt(BR[:], Bre[:], v[:, 11:12], BR[:], ALU.mult, ALU.subtract)
                P.ts(BI[:], Bre[:], v[:, 12:13], None, op0=ALU.mult)
                P.stt(BI[:], Bim[:], v[:, 11:12], BI[:], ALU.mult, ALU.add)
                P.tr(pt[:], BR[:], g.IDENT[:])
                P.copy(BRT[:], pt[:], eng="scalar")
                P.tr(pt[:], BI[:], g.IDENT[:])
                P.copy(BIT[:], pt[:], eng="scalar")
                P.copy(COS[:, 0:1], v[:, 7:8])
                P.copy(SIN[:, 0:1], v[:, 6:7])
                mm_ = 1
                while mm_ < Q:
                    cm, sm = COS[:, mm_ - 1:mm_], SIN[:, mm_ - 1:mm_]
                    P.ts(tq[:, :mm_], SIN[:, 0:mm_], sm, None, op0=ALU.mult)
                    P.stt(COS[:, mm_:2 * mm_], COS[:, 0:mm_], cm, tq[:, :mm_], ALU.mult, ALU.subtract)
                    P.ts(tq[:, :mm_], COS[:, 0:mm_], sm, None, op0=ALU.mult)
                    P.stt(SIN[:, mm_:2 * mm_], SIN[:, 0:mm_], cm, tq[:, :mm_], ALU.mult, ALU.add)
                    mm_ *= 2
                P.ts(RHO[:], ONQ[:], v[:, 3:4], None, op0=ALU.mult)
                order = (BLOCKS[0:5] if l == DEPTH - 1 else BLOCKS) if d == 0 else [BLOCKS[0]] + BLOCKS[:0:-1]
                for bi, (t0, n) in enumerate(order):
                    a_, b_ = pbr[kb % 2], pbi[kb % 2]
                    xr, xi = XR[kb % 2], XI[kb % 2]
                    zin, zout = z0[kb % 2], z0[(kb + 1) % 2]
                    kb += 1
                    P.mm(a_[:, :n], BRT[:], U[:, ut, t0:t0 + n])
                    P.mm(b_[:, :n], BIT[:], U[:, ut, t0:t0 + n])
                    as_, bs_ = AS[kb % 2], BS[kb % 2]
                    P.copy(as_[:, :n], a_[:, :n], eng="scalar")
                    P.copy(bs_[:, :n], b_[:, :n], eng="scalar")
                    if d == 0:
                        av, bv = as_[:, :n], bs_[:, :n]
                        xrv, xiv = xr[:, :n], xi[:, :n]
                        last = n - 1
                    else:
                        av, bv = as_[:, 0:n][:, ::-1], bs_[:, 0:n][:, ::-1]
                        xrv, xiv = xr[:, 0:n][:, ::-1], xi[:, 0:n][:, ::-1]
                        last = 0
                    c_, s2 = COS[:, :n], SIN[:, :n]
                    P.tt(t1[:, :n], c_, av, ALU.mult)
                    P.tt(t2[:, :n], s2, bv, ALU.mult)
                    P.tt(t3[:, :n], c_, bv, ALU.mult)
                    P.tt(t4[:, :n], s2, av, ALU.mult)
                    P.tt(Wr[:, :n], t1[:, :n], t2[:, :n], ALU.add)
                    P.tt(Wi[:, :n], t3[:, :n], t4[:, :n], ALU.subtract)
                    ir = 0.0 if bi == 0 else zin[:, 0:1]
                    ii = 0.0 if bi == 0 else zin[:, 1:2]
                    P.op("vector", lambda e, o=Zr[:, :n], r=RHO[:, :n], w=Wr[:, :n], i0=ir: e.tensor_tensor_scan(out=o, data0=r, data1=w, initial=i0, op0=ALU.mult, op1=ALU.add),
                         reads=[RHO, Wr] + ([zin] if bi else []), writes=[Zr])
                    P.op("vector", lambda e, o=Zi[:, :n], r=RHO[:, :n], w=Wi[:, :n], i0=ii: e.tensor_tensor_scan(out=o, data0=r, data1=w, initial=i0, op0=ALU.mult, op1=ALU.add),
                         reads=[RHO, Wi] + ([zin] if bi else []), writes=[Zi])
                    if l == DEPTH - 1 and d == 1 and t0 >= CTX + LOUT:
                        P.tt(m[:, 0:1], s2[:, n - 1:n], Zi[:, n - 1:n], ALU.mult)
                        P.stt(zout[:, 0:1], Zr[:, n - 1:n], c_[:, n - 1:n], m[:, 0:1], ALU.mult, ALU.subtract)
                        P.tt(m[:, 1:2], c_[:, n - 1:n], Zi[:, n - 1:n], ALU.mult)
                        P.stt(zout[:, 1:2], Zr[:, n - 1:n], s2[:, n - 1:n], m[:, 1:2], ALU.mult, ALU.add)
                        continue
                    P.tt(t5[:, :n], c_, Zr[:, :n], ALU.mult)
                    P.tt(t6[:, :n], s2, Zr[:, :n], ALU.mult)
                    P.tt(t1[:, :n], s2, Zi[:, :n], ALU.mult)
                    P.tt(t2[:, :n], c_, Zi[:, :n], ALU.mult)
                    P.tt(xrv, t5[:, :n], t1[:, :n], ALU.subtract)
                    P.tt(xiv, t6[:, :n], t2[:, :n], ALU.add)
                    P.copy(zout[:, 0:1], xr[:, last:last + 1])
                    P.copy(zout[:, 1:2], xi[:, last:last + 1])
                    y_ = py[kb % 2]
                    P.mm(y_[:, :n], CRe[:], xr[:, :n], start=True, stop=False)
                    P.mm(y_[:, :n], CIm[:], xi[:, :n], start=False, stop=True)
                    P.tt(YA[:, ut, t0:t0 + n], YA[:, ut, t0:t0 + n], y_[:, :n], ALU.add)
        P.flush()
        wgl = sb(st, nc, "s5wgl", [128, 2, 256])
        P.dma("sync", wgl[:], g.s5_w_glu[l].rearrange("(kc p) n -> p kc n", p=128))
        C1 = 2.0 * math.sqrt(2.0 / math.pi)
        for (t0, n) in (LAST_BLOCKS if l == DEPTH - 1 else BLOCKS):
            for ut in range(2):
                x = YA[:, ut, t0:t0 + n]
                P.tt(t1[:, :n], x, x, ALU.mult)
                P.ts(t1[:, :n], t1[:, :n], 0.044715, 1.0, op0=ALU.mult, op1=ALU.add)
                P.tt(t1[:, :n], t1[:, :n], x, ALU.mult)
                P.act(t1[:, :n], t1[:, :n], AF.Sigmoid, scale=C1)
                P.tt(x, x, t1[:, :n], ALU.mult)
            for uo in range(2):
                y_ = py[uo]
                for kc in range(2):
                    P.mm(y_[:, :n], wgl[:, kc, uo * 128:(uo + 1) * 128], YA[:, kc, t0:t0 + n], start=(kc == 0), stop=(kc == 1))
                P.act(t2[:, :n], y_[:, :n], AF.Sigmoid)
                P.tt(Wr[:, :n], t2[:, :n], YA[:, uo, t0:t0 + n], ALU.mult)
                P.dma("sync", g.YS[256 + uo * 128:256 + (uo + 1) * 128, t0:t0 + n], Wr[:, :n], wk=("YS", 1, uo, t0))
        P.flush()


RW0 = 1024
NCH = T // 32


def rwkv_shift(P, nc, g, l):
    with contextlib.ExitStack() as st:
        mu = sb(st, nc, "rmu", [128, 8])
        w3 = sb(st, nc, "rw3", [128, 8, 3])
        xi = [sb(st, nc, "rxi%d" % i, [128, T]) for i in range(2)]
        xo = [sb(st, nc, "rxo%d" % i, [128, T]) for i in range(2)]
        P.dma("sync", mu[:], g.rw_mu[l])
        P.ts(w3[:, :, 0], mu[:], 0.5, None, op0=ALU.mult)
        P.ts(w3[:, :, 2], mu[:], 0.5, None, op0=ALU.mult)
        P.ts(w3[:, :, 1], mu[:], -1.0, 1.0, op0=ALU.mult, op1=ALU.add)
        for ti in range(8):
            m = 128 if ti < 7 else 64
            a, b = xi[ti % 2], xo[ti % 2]
            P.dma("sync", a[:m, :], g.PX[RW0 + ti * 128:RW0 + ti * 128 + m, :], rk=("PX", 8 + ti))
            P.ts(b[:m, :], a[:m, :], w3[:m, ti, 1:2], 0.0, op0=ALU.mult, op1=ALU.add)
            for (s0, e0) in SEGS:
                P.stt(b[:m, s0 + 1:e0], a[:m, s0:e0 - 1], w3[:m, ti, 0:1], b[:m, s0 + 1:e0], ALU.mult, ALU.add)
                P.stt(b[:m, s0:e0 - 1], a[:m, s0 + 1:e0], w3[:m, ti, 2:3], b[:m, s0:e0 - 1], ALU.mult, ALU.add)
            P.dma("sync", g.RWS[ti * 128:ti * 128 + m, :], b[:m, :], wk=("RWS", ti))
        P.flush()


def stage_rwkv(P, nc, g, l):
    rwkv_shift(P, nc, g, l)
    with contextlib.ExitStack() as st:
        NB = 512
        w2 = sb(st, nc, "rw2", [64, 256])
        a2 = sb(st, nc, "ra2", [64, 256])
        g2 = sb(st, nc, "rg2", [64, 256])
        pc = sb(st, nc, "rpc", [128, 2, 8])
        r_ = [sb(st, nc, "rr%d" % i, [128, NB]) for i in range(2)]
        k_ = [sb(st, nc, "rk%d" % i, [128, NB]) for i in range(2)]
        v_ = [sb(st, nc, "rv%d" % i, [128, NB]) for i in range(2)]
        wa = sb(st, nc, "rwa", [64, NB])
        adt = sb(st, nc, "radt", [64, NB])
        gd = sb(st, nc, "rgd", [64, NB])
        tw = sb(st, nc, "rtw", [64, NB])
        sg = sb(st, nc, "rsg", [64, NB])
        kk = sb(st, nc, "rkk", [128, NB])
        t1 = sb(st, nc, "rt1", [128, NB])
        t2 = sb(st, nc, "rt2", [128, NB])
        A = sb(st, nc, "rA", [128, NB])
        X2 = [sb(st, nc, "rX2%d" % i, [128, 2, NB]) for i in range(5)]
        hib = sb(st, nc, "rhib", [128, NB], BF16)
        X2c = [sb(st, nc, "rX2c%d" % i, [128, 16, 2, 32]) for i in range(5)]
        tok = [sb(st, nc, "rtok%d" % i, [64, 16, 640], BF16) for i in range(2)]
        og = sb(st, nc, "rog", [128, NB])
        pm = [ps(st, nc, "rpm%d" % i, [128, 512]) for i in range(3)]
        ptr = [ps(st, nc, "rptr%d" % i, [64, 128]) for i in range(3)]
        P.dma("sync", w2[:], g.rw_w2[l])
        P.dma("sync", a2[:], g.rw_a2[l])
        P.dma("sync", g2[:], g.rw_g2[l])
        P.dma("sync", pc[:], g.rw_pc[l])
        EH = -math.exp(-0.5)
        kt = 0
        for (t0, n) in BLOCKS:
            nch = n // 32
            c0 = t0 // 32
            P.dma("sync", wa[:, :n], g.RWS[768:832, t0:t0 + n], rk=("RWS", 6))
            P.dma("sync", adt[:, :n], g.RWS[832:896, t0:t0 + n], rk=("RWS", 6))
            P.dma("sync", gd[:, :n], g.RWS[896:960, t0:t0 + n], rk=("RWS", 7))
            P.act(tw[:, :n], wa[:, :n], AF.Tanh)
            P.act(sg[:, :n], gd[:, :n], AF.Sigmoid)
            for ct in range(2):
                r, k, v = r_[ct], k_[ct], v_[ct]
                P.dma("sync", r[:, :n], g.RWS[ct * 128:(ct + 1) * 128, t0:t0 + n], rk=("RWS", ct))
                P.dma("sync", k[:, :n], g.RWS[256 + ct * 128:256 + (ct + 1) * 128, t0:t0 + n], rk=("RWS", 2 + ct))
                P.dma("sync", v[:, :n], g.RWS[512 + ct * 128:512 + (ct + 1) * 128, t0:t0 + n], rk=("RWS", 4 + ct))
                P.mm(pm[0][:, :n], g2[:, ct * 128:(ct + 1) * 128], sg[:, :n])
                P.copy(og[:, :n], pm[0][:, :n], eng="scalar")
                P.dma("sync", g.RWG[256 + ct * 128:256 + (ct + 1) * 128, t0:t0 + n], og[:, :n], wk=("RWG", 2 + ct))
                P.stt(t1[:, :n], r[:, :n], pc[:, ct, 6:7], k[:, :n], ALU.mult, ALU.mult)
                P.mm(pm[1][:, :n], g.BLK[:], t1[:, :n])
                P.tt(og[:, :n], pm[1][:, :n], v[:, :n], ALU.mult)
                P.dma("sync", g.RWG[ct * 128:(ct + 1) * 128, t0:t0 + n], og[:, :n], wk=("RWG", ct))
                P.ts(kk[:, :n], k[:, :n], pc[:, ct, 4:5], None, op0=ALU.mult)
                P.tt(t1[:, :n], kk[:, :n], kk[:, :n], ALU.mult)
                P.mm(pm[2][:, :n], g.BLK[:], t1[:, :n])
                P.ts(t1[:, :n], pm[2][:, :n], 1e-24, None, op0=ALU.max)
                P.act(t1[:, :n], t1[:, :n], AF.Sqrt)
                P.recip(t1[:, :n], t1[:, :n])
                P.tt(kk[:, :n], kk[:, :n], t1[:, :n], ALU.mult)
                P.copy(X2[3][:, 0, :n], kk[:, :n], eng="gpsimd")
                P.copy(X2[4][:, 0, :n], r[:, :n], eng="gpsimd")
                for d in range(2):
                    tk = tok[kt % 2]
                    kt += 1
                    P.mm(pm[0][:, :n], w2[32 * d:32 * d + 32, ct * 128:(ct + 1) * 128], tw[32 * d:32 * d + 32, :n])
                    P.act(t1[:, :n], pm[0][:, :n], AF.Sigmoid, bias=pc[:, ct, d:d + 1])
                    P.act(X2[0][:, 0, :n], t1[:, :n], AF.Exp, scale=EH)
                    P.mm(pm[1][:, :n], a2[32 * d:32 * d + 32, ct * 128:(ct + 1) * 128], adt[32 * d:32 * d + 32, :n])
                    P.act(A[:, :n], pm[1][:, :n], AF.Sigmoid, bias=pc[:, ct, 2 + d:3 + d])
                    P.ts(t2[:, :n], A[:, :n], -1.0, pc[:, ct, 5:6], op0=ALU.add, op1=ALU.mult)
                    P.stt(X2[2][:, 0, :n], t2[:, :n], 1.0, k[:, :n], ALU.add, ALU.mult)
                    P.stt(X2[1][:, 0, :n], kk[:, :n], -1.0, A[:, :n], ALU.mult, ALU.mult)
                    for a_ in range(5):
                        x2 = X2[a_]
                        xc = X2c[a_]
                        if d == 0 or a_ < 3:
                            hv = hib[:, :n].rearrange("p (c t) -> p c t", t=32)
                            xv_ = x2[:, 0, :n].rearrange("p (c t) -> p c t", t=32)
                            P.copy(hib[:, :n], x2[:, 0, :n], eng="gpsimd")
                            P.tt(xc[:, :nch, 1, :], xv_, hv, ALU.subtract, eng="gpsimd")
                            P.copy(xc[:, :nch, 0, :], hv, eng="gpsimd")
                        for c in range(nch):
                            p_ = ptr[(a_ * nch + c) % 3]
                            P.tr(p_[:, :], xc[:, c, :, :].rearrange("p a t -> p (a t)"), g.IDENT[:])
                            dst = tk[:, c, :].rearrange("p (h a k) -> p h a k", h=2, a=5)[:, :, a_, :]
                            src = p_[:, :].rearrange("p (h k) -> p h k", h=2)
                            if (a_ + c) % 2:
                                P.copy(dst, src, eng="scalar")
                            else:
                                P.copy(dst, src, eng="vector")
                    P.dma("sync", g.TOKD[d, ct, c0:c0 + nch].rearrange("c p x -> p c x"), tk[:, :nch, :], wk=("TOKD", d, ct, t0))
        P.flush()
    border = list(range(CTX - 1, -1, -1)) + list(range(T - 1, CTX - 1, -1))
    for ct in range(2):
        with contextlib.ExitStack() as st:
            V = sb(st, nc, "rsV", [128, T])
            Y = [sb(st, nc, "rsY%d" % d, [128, T]) for d in range(2)]
            S = [sb(st, nc, "rsS%d" % d, [128, 64]) for d in range(2)]
            sa = [sb(st, nc, "rssa%d" % d, [128, 1]) for d in range(2)]
            jk = [sb(st, nc, "rsjk%d" % d, [128, 64]) for d in range(2)]
            tkb = [[sb(st, nc, "rstk%d_%d" % (d, i), [64, 640], BF16) for i in range(2)] for d in range(2)]
            pb = [[ps(st, nc, "rspb%d_%d" % (d, i), [128, 512]) for i in range(2)] for d in range(2)]
            P.dma("sync", V[:], g.RWS[512 + ct * 128:512 + (ct + 1) * 128, :], rk=("RWS", 4 + ct))
            for d in range(2):
                P.memset(S[d][:], 0.0)
            curch = [None, None]
            nld = [0, 0]
            for i in range(T):
                ts_ = (i, border[i])
                Bv = []
                for d in range(2):
                    t = ts_[d]
                    ch = t // 32
                    if ch != curch[d]:
                        curch[d] = ch
                        nld[d] += 1
                        P.dma("sync", tkb[d][nld[d] % 2][:], g.TOKD[d, ct, ch], rk=("TOKD", d, ct, BLOCKS[0][0] if t < CTX else CTX + ((t - CTX) // 512) * 512))
                    tk = tkb[d][nld[d] % 2]
                    p_ = pb[d][i % 2]
                    for h2 in range(2):
                        P.mm(p_[h2 * 64:(h2 + 1) * 64, 0:320], g.SEL64[:, t % 32, :], tk[:, h2 * 320:(h2 + 1) * 320])
                    Bv.append(p_[:, 0:320].rearrange("p (a k) -> p a k", a=5))
                for d in range(2):
                    P.op("vector", lambda e, o=jk[d][:], a=S[d][:], b=Bv[d][:, 3, :], acc=sa[d][:]: e.scalar_tensor_tensor(
                        out=o, in0=a, scalar=1.0, in1=b, op0=ALU.mult, op1=ALU.mult, accum_out=acc),
                        reads=[S[d], Bv[d]], writes=[jk[d], sa[d]])
                for d in range(2):
                    P.tt(S[d][:], S[d][:], Bv[d][:, 0, :], ALU.mult)
                for d in range(2):
                    P.stt(S[d][:], Bv[d][:, 1, :], sa[d][:, 0:1], S[d][:], ALU.mult, ALU.add)
                for d in range(2):
                    t = ts_[d]
                    P.stt(S[d][:], Bv[d][:, 2, :], V[:, t:t + 1], S[d][:], ALU.mult, ALU.add)
                for d in range(2):
                    t = ts_[d]
                    P.op("vector", lambda e, o=jk[d][:], a=S[d][:], b=Bv[d][:, 4, :], acc=Y[d][:, t:t + 1]: e.scalar_tensor_tensor(
                        out=o, in0=a, scalar=1.0, in1=b, op0=ALU.mult, op1=ALU.mult, accum_out=acc),
                        reads=[S[d], Bv[d]], writes=[jk[d], Y[d]])
            P.flush()
            with contextlib.ExitStack() as st2:
                lnp = sb(st2, nc, "rln", [128, 2, 2])
                bon = sb(st2, nc, "rbon", [128, 512])
                gg = sb(st2, nc, "rgg", [128, 512])
                yc = sb(st2, nc, "ryc", [128, 512])
                sq = sb(st2, nc, "rsq", [128, 512])
                epsg = sb(st2, nc, "repsg", [128, 1])
                pq = [ps(st2, nc, "rpq%d" % i, [128, 512]) for i in range(2)]
                P.dma("sync", lnp[:], g.rw_ln[l])
                P.memset(epsg[:], 64e-5)
                for (t0, n) in BLOCKS:
                    P.dma("sync", bon[:, :n], g.RWG[ct * 128:(ct + 1) * 128, t0:t0 + n], rk=("RWG", ct))
                    P.dma("sync", gg[:, :n], g.RWG[256 + ct * 128:256 + (ct + 1) * 128, t0:t0 + n], rk=("RWG", 2 + ct))
                    P.tt(yc[:, :n], Y[0][:, t0:t0 + n], Y[1][:, t0:t0 + n], ALU.add)
                    P.mm(pq[0][:, :n], g.BLK[:], yc[:, :n])
                    P.stt(yc[:, :n], pq[0][:, :n], -1.0 / 64, yc[:, :n], ALU.mult, ALU.add)
                    P.tt(sq[:, :n], yc[:, :n], yc[:, :n], ALU.mult)
                    P.mm(pq[1][:, :n], g.BLK[:], sq[:, :n])
                    P.act(sq[:, :n], pq[1][:, :n], AF.Sqrt, bias=epsg[:, 0:1], scale=1.0 / 64)
                    P.recip(sq[:, :n], sq[:, :n])
                    P.tt(yc[:, :n], yc[:, :n], sq[:, :n], ALU.mult)
                    P.ts(yc[:, :n], yc[:, :n], lnp[:, ct, 0:1], lnp[:, ct, 1:2], op0=ALU.mult, op1=ALU.add)
                    P.tt(yc[:, :n], yc[:, :n], bon[:, :n], ALU.add)
                    P.tt(yc[:, :n], yc[:, :n], gg[:, :n], ALU.mult)
                    P.dma("sync", g.YS[512 + ct * 128:512 + (ct + 1) * 128, t0:t0 + n], yc[:, :n], wk=("YS", 2, ct, t0))
                P.flush()


RWDBG = [0]
RWJ = [6]


def stage_rwkv_chunked(P, nc, g, l):
    rwkv_shift(P, nc, g, l)
    pending = []
    if getattr(g, "prefetch_next", None) is not None:
        pending = stage_precast(P, nc, g, g.prefetch_next, flush=False)
        set_layer_weights(g, l)
        g.prefetch_next = None
    NB = 512
    EH = -math.exp(-0.5)
    with contextlib.ExitStack() as st:
        w2 = sb(st, nc, "cw2", [64, 256])
        a2 = sb(st, nc, "ca2", [64, 256])
        g2 = sb(st, nc, "cg2", [64, 256])
        pc = sb(st, nc, "cpc", [128, 2, 8])
        mk = sb(st, nc, "cmk", [128, 4, 64])
        wdt = sb(st, nc, "cwdt", [64, NB])
        adt = sb(st, nc, "cadt", [64, NB])
        gd = sb(st, nc, "cgd", [64, NB])
        tw = sb(st, nc, "ctw", [64, NB])
        sg = sb(st, nc, "csg", [64, NB])
        R = [sb(st, nc, "cR%d" % i, [128, NB]) for i in range(2)]
        Kt = [sb(st, nc, "cK%d" % i, [128, NB]) for i in range(2)]
        V = [sb(st, nc, "cV%d" % i, [128, NB]) for i in range(2)]
        KK = [sb(st, nc, "cKK%d" % i, [128, NB]) for i in range(2)]
        LW = [sb(st, nc, "cLW%d" % i, [128, NB]) for i in range(2)]
        CUM = [sb(st, nc, "cCUM%d" % i, [128, NB]) for i in range(2)]
        BN = [sb(st, nc, "cBN%d" % i, [128, NB]) for i in range(2)]
        KD = [sb(st, nc, "cKD%d" % i, [128, NB]) for i in range(2)]
        t1 = sb(st, nc, "ct1", [128, NB])
        t2 = sb(st, nc, "ct2", [128, NB])
        A = sb(st, nc, "cA", [128, NB])
        og = sb(st, nc, "cog", [128, NB])
        ONB = sb(st, nc, "cONB", [128, NB])
        Y = [sb(st, nc, "cY%d" % i, [128, T]) for i in range(2)]
        ST = [sb(st, nc, "cST%d" % i, [128, 64]) for i in range(2)]
        cum = [sb(st, nc, "ccum%d" % i, [128, 64]) for i in range(2)]
        e0 = [sb(st, nc, "ce0%d" % i, [128, 64]) for i in range(2)]
        e1 = [sb(st, nc, "ce1%d" % i, [128, 64]) for i in range(2)]
        e2 = [sb(st, nc, "ce2%d" % i, [128, 64]) for i in range(2)]
        RT = [[sb(st, nc, "cRT%d_%d" % (i, q), [128, 64]) for q in range(2)] for i in range(2)]
        ptot = [[sb(st, nc, "cpt%d_%d" % (i, q), [128, 2]) for q in range(2)] for i in range(2)]
        ATb = [[sb(st, nc, "cATb%d_%d" % (i, q), [128, 128]) for q in range(2)] for i in range(2)]
        BTb = [sb(st, nc, "cBTb%d" % i, [128, 128]) for i in range(2)]
        KTb = [sb(st, nc, "cKTb%d" % i, [128, 128]) for i in range(2)]
        Vb = [sb(st, nc, "cVb%d" % i, [128, 128]) for i in range(2)]
        ZTb = [sb(st, nc, "cZTb%d" % i, [128, 128]) for i in range(2)]
        STb = [sb(st, nc, "cSTb%d" % i, [128, 128]) for i in range(2)]
        MB = [sb(st, nc, "cMB%d" % i, [128, 128]) for i in range(2)]
        MBT = [sb(st, nc, "cMBT%d" % i, [128, 128]) for i in range(2)]
        MK = [[sb(st, nc, "cMK%d_%d" % (i, q), [128, 128]) for q in range(2)] for i in range(2)]
        WB = [[sb(st, nc, "cWB%d_%d" % (i, q), [128, 64]) for q in range(2)] for i in range(2)]
        WK = [[sb(st, nc, "cWK%d_%d" % (i, q), [128, 64]) for q in range(2)] for i in range(2)]
        Btb = [[sb(st, nc, "cBtb%d_%d" % (i, q), [128, 128]) for q in range(2)] for i in range(2)]
        Ktb = [[sb(st, nc, "cKtb%d_%d" % (i, q), [128, 128]) for q in range(2)] for i in range(2)]
        VTb = [[sb(st, nc, "cVTb%d_%d" % (i, q), [128, 128]) for q in range(2)] for i in range(2)]
        Ma = [[sb(st, nc, "cMa%d_%d" % (c_, i), [128, 128]) for i in range(2)] for c_ in range(2)]
        MTa = [[sb(st, nc, "cMTa%d_%d" % (c_, i), [128, 128]) for i in range(2)] for c_ in range(2)]
        Nn = [[sb(st, nc, "cNn%d_%d" % (i, q), [128, 128]) for q in range(2)] for i in range(2)]
        VTs = [[sb(st, nc, "cVTs%d_%d" % (i, q), [128, 64]) for q in range(2)] for i in range(2)]
        GTs = [sb(st, nc, "cGTs%d" % i, [128, 64]) for i in range(2)]
        ZTs = [sb(st, nc, "cZTs%d" % i, [128, 64]) for i in range(2)]
        mbd = sb(st, nc, "cmbd", [128, 2, 128])
        I2 = sb(st, nc, "cI2", [128, 64])
        B1 = [ps(st, nc, "cB1_%d" % i, [128, 512]) for i in range(2)]
        B2 = [ps(st, nc, "cB2_%d" % i, [128, 512]) for i in range(2)]
        B3 = [ps(st, nc, "cB3_%d" % i, [128, 512]) for i in range(2)]
        B4 = [ps(st, nc, "cB4_%d" % i, [128, 512]) for i in range(2)]
        PM = [B1[0]]
        PI0, PI12 = B1[0], B1[1]
        P.dma("sync", mbd[:], g.rw_mbd)
        P.dma("sync", I2[:], g.rw_i2)
        for tl in (BTb, KTb, Vb, ZTb, STb):
            for i in range(2):
                P.memset(tl[i][:], 0.0, eng="gpsimd")
        for i in range(2):
            for q in range(2):
                P.memset(ATb[i][q][:], 0.0, eng="gpsimd")
        P.dma("sync", w2[:], g.rw_w2[l])
        P.dma("sync", a2[:], g.rw_a2[l])
        P.dma("sync", g2[:], g.rw_g2[l])
        P.dma("sync", pc[:], g.rw_pc[l])
        P.dma("sync", mk[:], g.rw_masks)
        P.memset(ONB[:], 1.0)
        for d in range(2):
            if d == 0:
                msk = [3, 1, 3, 0, 0]
            else:
                msk = [1, 3, 1, 2, 2]
            for ct in range(2):
                P.memset(ST[ct][:], 0.0)
                P.memset(STb[ct][:], 0.0)
            blocks = (BLOCKS[0:5] if l == DEPTH - 1 else BLOCKS) if d == 0 else [BLOCKS[0]] + BLOCKS[:0:-1]
            for (t0, n) in blocks:
                nck = n // 64
                P.dma("sync", wdt[:, :n], g.RWS[768:832, t0:t0 + n], rk=("RWS", 6))
                P.dma("sync", adt[:, :n], g.RWS[832:896, t0:t0 + n], rk=("RWS", 6))
                P.act(tw[:, :n], wdt[:, :n], AF.Tanh)
                if d == 0:
                    P.dma("sync", gd[:, :n], g.RWS[896:960, t0:t0 + n], rk=("RWS", 7))
                    P.act(sg[:, :n], gd[:, :n], AF.Sigmoid)
                for ct in range(2):
                    r, k, v, kk = R[ct], Kt[ct], V[ct], KK[ct]
                    P.dma("sync", r[:, :n], g.RWS[ct * 128:(ct + 1) * 128, t0:t0 + n], rk=("RWS", ct))
                    P.dma("sync", k[:, :n], g.RWS[256 + ct * 128:256 + (ct + 1) * 128, t0:t0 + n], rk=("RWS", 2 + ct))
                    P.dma("sync", v[:, :n], g.RWS[512 + ct * 128:512 + (ct + 1) * 128, t0:t0 + n], rk=("RWS", 4 + ct))
                    if d == 0:
                        P.mm(PM[0][:, :n], g2[:, ct * 128:(ct + 1) * 128], sg[:, :n])
                        P.copy(og[:, :n], PM[0][:, :n], eng="scalar")
                        P.dma("sync", g.RWG[256 + ct * 128:256 + (ct + 1) * 128, t0:t0 + n], og[:, :n], wk=("RWG", 2 + ct))
                        P.stt(t1[:, :n], r[:, :n], pc[:, ct, 6:7], k[:, :n], ALU.mult, ALU.mult)
                        P.mm(PM[0][:, :n], g.BLK[:], t1[:, :n])
                        P.tt(og[:, :n], PM[0][:, :n], v[:, :n], ALU.mult)
                        P.dma("sync", g.RWG[ct * 128:(ct + 1) * 128, t0:t0 + n], og[:, :n], wk=("RWG", ct))
                    P.ts(kk[:, :n], k[:, :n], pc[:, ct, 4:5], None, op0=ALU.mult)
                    P.tt(t1[:, :n], kk[:, :n], kk[:, :n], ALU.mult)
                    P.mm(PM[0][:, :n], g.BLK[:], t1[:, :n])
                    P.ts(t1[:, :n], PM[0][:, :n], 1e-24, None, op0=ALU.max)
                    P.act(t1[:, :n], t1[:, :n], AF.Sqrt)
                    P.recip(t1[:, :n], t1[:, :n])
                    P.tt(kk[:, :n], kk[:, :n], t1[:, :n], ALU.mult)
                    P.mm(PM[0][:, :n], w2[32 * d:32 * d + 32, ct * 128:(ct + 1) * 128], tw[32 * d:32 * d + 32, :n])
                    P.act(t1[:, :n], PM[0][:, :n], AF.Sigmoid, bias=pc[:, ct, d:d + 1])
                    P.ts(LW[ct][:, :n], t1[:, :n], EH, None, op0=ALU.mult)
                    P.op("vector", lambda e, o=CUM[ct][:, :n], on=ONB[:, :n], w=LW[ct][:, :n]: e.tensor_tensor_scan(out=o, data0=on, data1=w, initial=0.0, op0=ALU.mult, op1=ALU.add),
                         reads=[ONB, LW[ct]], writes=[CUM[ct]])
                    P.mm(PM[0][:, :n], a2[32 * d:32 * d + 32, ct * 128:(ct + 1) * 128], adt[32 * d:32 * d + 32, :n])
                    P.act(A[:, :n], PM[0][:, :n], AF.Sigmoid, bias=pc[:, ct, 2 + d:3 + d])
                    P.ts(t2[:, :n], A[:, :n], -1.0, pc[:, ct, 5:6], op0=ALU.add, op1=ALU.mult)
                    P.stt(KD[ct][:, :n], t2[:, :n], 1.0, k[:, :n], ALU.add, ALU.mult)
                    P.stt(BN[ct][:, :n], kk[:, :n], -1.0, A[:, :n], ALU.mult, ALU.mult)
                chunks = list(range(nck)) if d == 0 else list(range(nck - 1, -1, -1))
                if RWDBG[0] == 1:
                    chunks = []
                H = (slice(0, 64), slice(64, 128))
                i_s = 0 if d == 0 else 1
                last = 63 if d == 0 else 0

                def indep_gen(c, ct, par):
                    sl = slice(c * 64, (c + 1) * 64)
                    cm, E0, E1, E2 = cum[ct], e0[ct], e1[ct], e2[ct]
                    PSA = B1[ct][:, 0:384].rearrange("p (m k) -> p m k", m=3)
                    PSB = B2[ct][:, 0:128].rearrange("p (m k) -> p m k", m=2)
                    PSY = B2[ct][:, 128:192]
                    PSS = B2[ct][:, 192:256]
                    PI1 = B2[ct][:, 256:384]
                    PI2 = B2[ct][:, 384:512]
                    PST = B3[ct]
                    PSG = B3[ct][:, 448:512]
                    PI0 = B4[ct][:, 0:128]
                    PSZ = B4[ct][:, 128:192]
                    if c == 0:
                        P.copy(cm[:], CUM[ct][:, sl])
                    else:
                        P.ts(cm[:], CUM[ct][:, sl], CUM[ct][:, c * 64 - 1:c * 64], None, op0=ALU.subtract)
                    if d == 1:
                        P.copy(ptot[ct][par][:, 1:2], cm[:, 63:64])
                        P.stt(cm[:], cm[:], -1.0, LW[ct][:, sl], ALU.mult, ALU.add)
                        P.ts(cm[:], cm[:], ptot[ct][par][:, 1:2], None, op0=ALU.add)
                    P.tt(E0[:], cm[:], LW[ct][:, sl], ALU.subtract)
                    yield
                    P.act(E1[:], cm[:], AF.Exp)
                    P.act(E2[:], cm[:], AF.Exp, scale=-1.0)
                    P.act(E0[:], E0[:], AF.Exp)
                    yield
                    P.copy(ptot[ct][par][:, 0:1], E1[:, last:last + 1])
                    for h2 in range(2):
                        hs = H[h2]
                        P.tt(ATb[ct][par][hs, hs], KK[ct][hs, sl], E0[hs, :], ALU.mult)
                        P.tt(BTb[ct][hs, hs], BN[ct][hs, sl], E2[hs, :], ALU.mult)
                        P.tt(KTb[ct][hs, hs], KD[ct][hs, sl], E2[hs, :], ALU.mult)
                        P.copy(Vb[ct][hs, hs], V[ct][hs, sl], eng="gpsimd")
                    P.tt(RT[ct][par][:], R[ct][:, sl], E1[:], ALU.mult, eng="gpsimd")
                    yield
                    P.mm(PSA[:, 0, :], BTb[ct][:], ATb[ct][par][:], r=True)
                    P.mm(PSA[:, 1, :], ATb[ct][par][:], BTb[ct][:], r=True)
                    P.mm(PSA[:, 2, :], KTb[ct][:], ATb[ct][par][:], r=True)
                    P.mm(PSB[:, 0, :], BTb[ct][:], RT[ct][par][:], r=True)
                    P.mm(PSB[:, 1, :], KTb[ct][:], RT[ct][par][:], r=True)
                    P.tr(PST[:, 0:128], BTb[ct][:], g.IDENT[:])
                    P.tr(PST[:, 128:256], KTb[ct][:], g.IDENT[:])
                    P.tr(PST[:, 256:384], Vb[ct][:], g.IDENT[:])
                    P.mm(PST[:, 384:448], Vb[ct][:], I2[:])
                    yield
                    P.tt(MB[ct][:], PSA[:, 0, :], mbd[:, i_s, :], ALU.mult)
                    P.tt(MBT[ct][:], PSA[:, 1, :], mbd[:, 1 - i_s, :], ALU.mult)
                    P.tt(Nn[ct][par][:], MB[ct][:], g.IDENT[:], ALU.add)
                    P.tt(MK[ct][par][:], PSA[:, 2, :], mbd[:, i_s, :], ALU.mult)
                    P.tt(WB[ct][par][:], PSB[:, 0, :], mk[:, msk[3], :], ALU.mult)
                    P.tt(WK[ct][par][:], PSB[:, 1, :], mk[:, msk[4], :], ALU.mult)
                    P.copy(Btb[ct][par][:], PST[:, 0:128], eng="scalar")
                    P.copy(Ktb[ct][par][:], PST[:, 128:256], eng="scalar")
                    P.copy(VTb[ct][par][:], PST[:, 256:384], eng="scalar")
                    P.copy(VTs[ct][par][:], PST[:, 384:448], eng="scalar")
                    yield
                    Mc, MTc = MB[ct], MBT[ct]
                    for j in range(1, 6):
                        if j < 5:
                            P.mm(PI0, MTc[:], Mc[:], r=True)
                        P.mm(PI1, Mc[:], MTc[:], r=True)
                        yield
                        Mn, MTn = Ma[ct][j % 2], MTa[ct][j % 2]
                        if j < 5:
                            P.copy(Mn[:], PI0, eng="scalar")
                        P.copy(MTn[:], PI1, eng="vector")
                        yield
                        P.mm(PI2, MTn[:], Nn[ct][par][:], r=True)
                        yield
                        P.tt(Nn[ct][par][:], Nn[ct][par][:], PI2, ALU.add)
                        yield
                        Mc, MTc = Mn, MTn

                def dep_gen(c, ct, par):
                    PSY = B2[ct][:, 128:192]
                    PSS = B2[ct][:, 192:256]
                    PSG = B3[ct][:, 448:512]
                    PSZ = B4[ct][:, 128:192]
                    P.mm(PSG, ATb[ct][par][:], ST[ct][:], start=True, stop=False)
                    P.mm(PSG, MK[ct][par][:], VTs[ct][par][:], start=False, stop=True)
                    yield
                    P.copy(GTs[ct][:], PSG, eng="scalar")
                    yield
                    P.mm(PSZ, Nn[ct][par][:], GTs[ct][:], r=True)
                    yield
                    P.copy(ZTs[ct][:], PSZ, eng="scalar")
                    for h2 in range(2):
                        P.copy(ZTb[ct][H[h2], H[h2]], B4[ct][H[h2], 128:192], eng="scalar")
                    yield
                    P.mm(PSY, STb[ct][:], RT[ct][par][:], start=True, stop=False)
                    P.mm(PSY, ZTb[ct][:], WB[ct][par][:], start=False, stop=False)
                    P.mm(PSY, VTb[ct][par][:], WK[ct][par][:], start=False, stop=True)
                    P.mm(PSS, Btb[ct][par][:], ZTs[ct][:], start=True, stop=False)
                    P.mm(PSS, Ktb[ct][par][:], VTs[ct][par][:], start=False, stop=True)
                    yield
                    P.copy(Y[ct][:, t0 + c * 64:t0 + (c + 1) * 64], PSY, eng="vector")
                    P.ts(ST[ct][:], ST[ct][:], ptot[ct][par][:, 0:1], None, op0=ALU.mult)
                    P.stt(ST[ct][:], PSS, ptot[ct][par][:, 0:1], ST[ct][:], ALU.mult, ALU.add)
                    yield
                    for h2 in range(2):
                        P.copy(STb[ct][H[h2], H[h2]], ST[ct][H[h2], :], eng="scalar")

                def drive(gens):
                    while gens:
                        for gen in list(gens):
                            try:
                                next(gen)
                            except StopIteration:
                                gens.remove(gen)

                drive([indep_gen(chunks[0], 0, 0), indep_gen(chunks[0], 1, 0)])
                for ii, c in enumerate(chunks):
                    for _ in range(6):
                        if pending:
                            o_, i_, k_ = pending.pop(0)
                            P.dma("gpsimd", o_, i_, wk=k_)
                    gens = [dep_gen(c, 0, ii % 2), dep_gen(c, 1, ii % 2)]
                    if ii + 1 < len(chunks):
                        gens += [indep_gen(chunks[ii + 1], 0, (ii + 1) % 2), indep_gen(chunks[ii + 1], 1, (ii + 1) % 2)]
                    drive(gens)
            if d == 0:
                for (o_, i_, k_) in pending:
                    P.dma("gpsimd", o_, i_, wk=k_)
                pending = []
                for ct in range(2):
                    P.dma("sync", g.RWY[ct * 128:(ct + 1) * 128, :], Y[ct][:], wk=("RWY", ct))
        P.flush()
        with contextlib.ExitStack() as st2:
            lnp = sb(st2, nc, "cln", [128, 2, 2])
            epsg = sb(st2, nc, "cepsg", [128, 1])
            y0 = t2
            P.dma("sync", lnp[:], g.rw_ln[l])
            P.memset(epsg[:], 64e-5)
            for ct in range(2):
                for (t0, n) in (LAST_BLOCKS if l == DEPTH - 1 else BLOCKS):
                    bon, gg, yc, sq = og, A, t1, CUM[0]
                    P.dma("sync", bon[:, :n], g.RWG[ct * 128:(ct + 1) * 128, t0:t0 + n], rk=("RWG", ct))
                    P.dma("sync", gg[:, :n], g.RWG[256 + ct * 128:256 + (ct + 1) * 128, t0:t0 + n], rk=("RWG", 2 + ct))
                    P.dma("sync", y0[:, :n], g.RWY[ct * 128:(ct + 1) * 128, t0:t0 + n], rk=("RWY", ct))
                    P.tt(yc[:, :n], y0[:, :n], Y[ct][:, t0:t0 + n], ALU.add)
                    P.mm(PI12[:, :n], g.BLK[:], yc[:, :n])
                    P.stt(yc[:, :n], PI12[:, :n], -1.0 / 64, yc[:, :n], ALU.mult, ALU.add)
                    P.tt(sq[:, :n], yc[:, :n], yc[:, :n], ALU.mult)
                    P.mm(PI0[:, :n], g.BLK[:], sq[:, :n])
                    P.act(sq[:, :n], PI0[:, :n], AF.Sqrt, bias=epsg[:, 0:1], scale=1.0 / 64)
                    P.recip(sq[:, :n], sq[:, :n])
                    P.tt(yc[:, :n], yc[:, :n], sq[:, :n], ALU.mult)
                    P.ts(yc[:, :n], yc[:, :n], lnp[:, ct, 0:1], lnp[:, ct, 1:2], op0=ALU.mult, op1=ALU.add)
                    P.tt(yc[:, :n], yc[:, :n], bon[:, :n], ALU.add)
                    P.tt(yc[:, :n], yc[:, :n], gg[:, :n], ALU.mult)
                    P.dma("sync", g.YS[512 + ct * 128:512 + (ct + 1) * 128, t0:t0 + n], yc[:, :n], wk=("YS", 2, ct, t0))
            P.flush()


M0 = 1984
NCK = T // 64


def stage_ssd(P, nc, g, l):
    with contextlib.ExitStack() as st:
        cw = sb(st, nc, "mcw", [128, 6, 3])
        cb = sb(st, nc, "mcb", [128, 6])
        xi = [sb(st, nc, "mxi%d" % i, [128, T]) for i in range(2)]
        y = [sb(st, nc, "my%d" % i, [128, T]) for i in range(2)]
        tms = [sb(st, nc, "mtms%d" % i, [128, T // 128, 128]) for i in range(2)]
        pt = [ps(st, nc, "mpt%d" % i, [128, 128]) for i in range(2)]
        P.dma("sync", cw[:], g.m2_conv_w[l])
        P.dma("sync", cb[:], g.m2_conv_b[l])
        sxv = g.SXT.rearrange("(tt p) c -> p tt c", p=128)
        srcs = [(M0 + 256, 128, 0, 0, None), (M0 + 384, 128, 1, 128, None),
                (M0 + 512, 128, 2, 256, 0), (M0 + 640, 128, 3, 384, 128),
                (M0 + 768, 128, 4, None, 256), (M0 + 896, 128, 5, None, 384),
                (M0 + 0, 128, None, 512, None), (M0 + 128, 128, None, 640, None),
                (M0 + 1024, 8, "dt", 768, None)]
        for si, (r0, m, ci, col, brow) in enumerate(srcs):
            a, b = xi[si % 2], y[si % 2]
            tm = tms[si % 2]
            P.dma("sync", a[:m, :], g.PX[r0:r0 + m, :], rk=("PXm", si))
            if ci == "dt":
                b = a
            elif ci is None:
                P.act(b[:m, :], a[:m, :], AF.Silu)
            else:
                conv3(P, b, a, cw[:, ci, :], cb[:, ci:ci + 1])
                P.act(b[:, :], b[:, :], AF.Silu)
            if brow is not None:
                P.dma("sync", g.SBC[brow:brow + 128, :], b[:, :], wk=("SBC", brow))
            if col is not None:
                for tt_ in range(T // 128):
                    p_ = pt[tt_ % 2]
                    P.tr(p_[:, :m], b[:m, tt_ * 128:(tt_ + 1) * 128], g.IDENT[:m, :m])
                    P.copy(tm[:, tt_, :m], p_[:, :m], eng=("scalar" if tt_ % 2 else "vector"))
                P.dma("sync", sxv[:, :, col:col + m], tm[:, :, :m], wk=("SXT", col))
        P.flush()
    with contextlib.ExitStack() as st:
        mk = sb(st, nc, "mmk", [64, 4, 64])
        MFB = [sb(st, nc, "mMF%d" % d, [64, 4, 64]) for d in range(2)]
        bias8 = sb(st, nc, "mb8", [64, 8])
        a8 = sb(st, nc, "ma8", [64, 8])
        STd = [sb(st, nc, "mST%d" % d, [128, 4, 64]) for d in range(2)]
        tmc_ = [[sb(st, nc, "mtmc%d_%d" % (d, i), [64, 776]) for i in range(2)] for d in range(2)]
        bc_ = [[sb(st, nc, "mbc%d_%d" % (d, i), [128, 4, 64]) for i in range(2)] for d in range(2)]
        dtd_ = [sb(st, nc, "mdtd%d" % d, [64, 8]) for d in range(2)]
        adt_ = [sb(st, nc, "madt%d" % d, [64, 8]) for d in range(2)]
        rh_ = [sb(st, nc, "mrh%d" % d, [64, 4, 64]) for d in range(2)]
        E_ = [sb(st, nc, "mE%d" % d, [64, 4, 64]) for d in range(2)]
        SdT_ = [sb(st, nc, "mSdT%d" % d, [64, 4, 64]) for d in range(2)]
        xdt_ = [sb(st, nc, "mxdt%d" % d, [64, 4, 64]) for d in range(2)]
        xdw_ = [sb(st, nc, "mxdw%d" % d, [64, 4, 64]) for d in range(2)]
        sm_ = [sb(st, nc, "msm%d" % d, [128, 12]) for d in range(2)]
        Yd_ = [[sb(st, nc, "mYd%d_%d" % (d, i), [64, 256]) for i in range(2)] for d in range(2)]
        bkA = [ps(st, nc, "mbkA%d" % d, [128, 512]) for d in range(2)]
        bkB = [ps(st, nc, "mbkB%d" % d, [128, 512]) for d in range(2)]
        bkC = [ps(st, nc, "mbkC%d" % d, [128, 512]) for d in range(2)]
        P.dma("sync", mk[:], g.m2_masks)
        for h in range(4):
            P.copy(MFB[0][:, h, :], mk[:, 0, :])
            P.copy(MFB[1][:, h, :], mk[:, 2, :])
        P.dma("sync", bias8[:], g.m2_dt_bias[l:l + 1, :].partition_broadcast(64))
        P.dma("sync", a8[:], g.m2_a_log[l:l + 1, :].partition_broadcast(64))
        P.act(a8[:], a8[:], AF.Exp)
        P.ts(a8[:], a8[:], -1.0, None, op0=ALU.mult)
        for d in range(2):
            P.memset(STd[d][:], 0.0)
        orders = [list(range(NCK)), [3, 2, 1, 0] + list(range(NCK - 1, 3, -1))]
        for ci in range(NCK):
            for d in range(2):
                if l == DEPTH - 1 and d == 0 and ci >= (CTX + LOUT) // 64:
                    continue
                c = orders[d][ci]
                A_lhs = mk[:, 1, :] if d == 0 else mk[:, 3, :]
                A_rhs = mk[:, 0, :] if d == 0 else mk[:, 2, :]
                MM = MFB[d]
                t0 = c * 64
                tmc = tmc_[d][ci % 2]
                bc = bc_[d][ci % 2]
                dtd, adt, rh, E, SdT, xdt, xdw, sm = dtd_[d], adt_[d], rh_[d], E_[d], SdT_[d], xdt_[d], xdw_[d], sm_[d]
                Yd = Yd_[d][ci % 2]
                p_seg = bkA[d][0:64, 0:256].rearrange("p (h k) -> p h k", h=4)
                p_smA = bkA[d][:, 256:268]
                p_sc = bkB[d][0:64, 0:128].rearrange("p (h k) -> p h k", h=2)
                p_yo = bkB[d][0:64, 128:384].rearrange("p (h k) -> p h k", h=4)
                p_st = bkC[d][:, 0:256].rearrange("p (h k) -> p h k", h=4)
                p_yd = bkC[d][0:64, 256:512].rearrange("p (h k) -> p h k", h=4)
                P.dma("sync", tmc[:], g.SXT[t0:t0 + 64, :], rk=("SXTall",))
                P.dma("sync", bc[:], g.SBC.rearrange("(a n) t -> n a t", n=128)[:, :, t0:t0 + 64], rk=("SBCall",))
                P.tt(dtd[:], tmc[:, 768:776], bias8[:], ALU.add)
                P.act(dtd[:], dtd[:], AF.Exp)
                P.ts(dtd[:], dtd[:], 1.0, None, op0=ALU.add)
                P.act(dtd[:], dtd[:], AF.Ln)
                P.tt(adt[:], dtd[:], a8[:], ALU.mult)
                o4 = 4 * d
                bc4 = lambda ap: ap.unsqueeze(2).broadcast_to([ap.shape[0], 4, 64])
                light = (l == DEPTH - 1 and d == 1 and t0 >= CTX + LOUT)
                if light:
                    P.tt(xdt[:], tmc[:, 0:256].rearrange("p (h k) -> p h k", h=4), bc4(dtd[:, o4:o4 + 4]), ALU.mult)
                    P.mm(p_smA[0:64, 4:8], A_lhs, adt[:, o4:o4 + 4])
                    P.mm(p_smA[:, 8:12], g.ONES[0:64, :], adt[:, o4:o4 + 4])
                    P.act(sm[0:64, 4:8], p_smA[0:64, 4:8], AF.Exp)
                    P.act(sm[:, 8:12], p_smA[:, 8:12], AF.Exp)
                    P.tt(xdw[:], xdt[:], bc4(sm[0:64, 4:8]), ALU.mult)
                    for h in range(4):
                        P.mm(p_st[:, h, :], tmc[:, 256 + (h // 2) * 128:256 + (h // 2 + 1) * 128], xdw[:, h, :])
                    P.tt(STd[d][:], STd[d][:], bc4(sm[:, 8:12]), ALU.mult)
                    P.tt(STd[d][:], STd[d][:], p_st, ALU.add)
                    continue
                P.tt(rh[:], A_rhs.unsqueeze(1).broadcast_to([64, 4, 64]), bc4(adt[:, o4:o4 + 4]), ALU.mult)
                for h in range(4):
                    P.mm(p_seg[:, h, :], A_lhs, rh[:, h, :])
                P.act(E[:], p_seg, AF.Exp)
                P.tt(E[:], E[:], MM[:], ALU.mult)
                for gi in range(2):
                    P.mm(p_sc[:, gi, :], bc[:, gi, :], bc[:, 2 + gi, :])
                P.tt(SdT[:].rearrange("p (g h) k -> p g h k", g=2), E[:].rearrange("p (g h) k -> p g h k", g=2),
                     p_sc.unsqueeze(2).broadcast_to([64, 2, 2, 64]), ALU.mult)
                P.tt(xdt[:], tmc[:, 0:256].rearrange("p (h k) -> p h k", h=4), bc4(dtd[:, o4:o4 + 4]), ALU.mult)
                for h in range(4):
                    P.mm(p_yd[:, h, :], SdT[:, h, :], xdt[:, h, :])
                P.mm(p_smA[0:64, 0:4], A_rhs, adt[:, o4:o4 + 4])
                P.mm(p_smA[0:64, 4:8], A_lhs, adt[:, o4:o4 + 4])
                P.mm(p_smA[:, 8:12], g.ONES[0:64, :], adt[:, o4:o4 + 4])
                P.act(sm[0:64, 0:8], p_smA[0:64, 0:8], AF.Exp)
                P.act(sm[:, 8:12], p_smA[:, 8:12], AF.Exp)
                for h in range(4):
                    P.mm(p_yo[:, h, :], bc[:, 2 + h // 2, :], STd[d][:, h, :])
                Yv = Yd[:].rearrange("p (h k) -> p h k", h=4)
                P.tt(rh[:], p_yo, bc4(sm[0:64, 0:4]), ALU.mult)
                P.tt(Yv, p_yd, rh[:], ALU.add)
                P.tt(xdw[:], xdt[:], bc4(sm[0:64, 4:8]), ALU.mult)
                for h in range(4):
                    P.mm(p_st[:, h, :], tmc[:, 256 + (h // 2) * 128:256 + (h // 2 + 1) * 128], xdw[:, h, :])
                P.tt(STd[d][:], STd[d][:], bc4(sm[:, 8:12]), ALU.mult)
                P.tt(STd[d][:], STd[d][:], p_st, ALU.add)
                P.dma("sync", g.SYF[d, t0:t0 + 64, :], Yd[:], wk=("SYF", d, c))
        P.flush()
    with contextlib.ExitStack() as st:
        dsk = sb(st, nc, "mdsk", [128, 4])
        nw = sb(st, nc, "mnw", [128, 2])
        epsm = sb(st, nc, "mepsm", [128, 1])
        tm_ = [sb(st, nc, "m3tm%d" % i, [128, 776]) for i in range(2)]
        ya_ = [sb(st, nc, "m3ya%d" % i, [128, 256]) for i in range(2)]
        yb_ = [sb(st, nc, "m3yb%d" % i, [128, 256]) for i in range(2)]
        Yo = sb(st, nc, "m3Yo", [128, 256])
        ss = sb(st, nc, "m3ss", [128, 2])
        ot_ = [sb(st, nc, "m3ot%d" % i, [128, 128]) for i in range(2)]
        p_tr = [ps(st, nc, "m3ptr%d" % i, [128, 512]) for i in range(2)]
        P.dma("sync", dsk[:], g.m2_d[l:l + 1, :].partition_broadcast(128))
        P.dma("sync", nw[:], g.m2_norm_w[l])
        P.memset(epsm[:], EPS)
        for tt_ in (range(CTX // 128, (CTX + LOUT) // 128) if l == DEPTH - 1 else range(T // 128)):
            t0 = tt_ * 128
            tm, ya, yb = tm_[tt_ % 2], ya_[tt_ % 2], yb_[tt_ % 2]
            P.dma("sync", tm[:], g.SXT[t0:t0 + 128, :], rk=("SXTall",))
            P.dma("sync", ya[:], g.SYF[0, t0:t0 + 128, :], rk=("SYFall",))
            P.dma("sync", yb[:], g.SYF[1, t0:t0 + 128, :], rk=("SYFall",))
            P.tt(ya[:], ya[:], yb[:], ALU.add)
            for h in range(4):
                P.stt(ya[:, h * 64:(h + 1) * 64], tm[:, h * 64:(h + 1) * 64], dsk[:, h:h + 1], ya[:, h * 64:(h + 1) * 64], ALU.mult, ALU.add)
            P.tt(ya[:], ya[:], tm[:, 512:768], ALU.mult)
            P.act(Yo[:], ya[:], AF.Square, accum_out=ss[:, 0:1])
            P.act(ss[:, 1:2], ss[:, 0:1], AF.Sqrt, bias=epsm[:, 0:1], scale=1.0 / 256)
            P.recip(ss[:, 1:2], ss[:, 1:2])
            P.ts(ya[:], ya[:], ss[:, 1:2], None, op0=ALU.mult)
            for ct in range(2):
                p_ = p_tr[ct]
                o_ = ot_[ct]
                P.tr(p_[:, 0:128], ya[:, ct * 128:(ct + 1) * 128], g.IDENT[:])
                P.act(o_[:], p_[:, 0:128], AF.Identity, scale=nw[:, ct:ct + 1])
                P.dma("sync", g.YS[768 + ct * 128:768 + (ct + 1) * 128, t0:t0 + 128], o_[:], wk=("YS", 3, ct, tt_))
        P.flush()


ONLY = [None]


def stage_mixers(P, nc, g, l):
    if ONLY[0] in (None, "hy"):
        stage_hyena(P, nc, g, l)
    if ONLY[0] in (None, "s5"):
        stage_s5(P, nc, g, l)
    if ONLY[0] in (None, "rw"):
        stage_rwkv_chunked(P, nc, g, l)
    if ONLY[0] in (None, "m2"):
        stage_ssd(P, nc, g, l)


def stage_merge(P, nc, g, l, xsrc, xdst):
    issue_bg(P, g)
    P.flush()
    with contextlib.ExitStack() as st:
        wbr = sb(st, nc, "wbr", [128, 8, D], BF16)
        wo = sb(st, nc, "wo", [128, 8, D], BF16)
        wgt = [sb(st, nc, "wgt%d" % i, [128, 8, 512], BF16) for i in range(2)]
        ysf = sb(st, nc, "ysf", [128, 8, 512])
        xmb = sb(st, nc, "xmb", [128, 8, 512], BF16)
        xmv = g.XM16.rearrange("(fc p) t -> p fc t", p=128)
        ysb = sb(st, nc, "ysb", [128, 8, 512], BF16)
        mg = sb(st, nc, "mg", [128, 8, 512], BF16)
        acc = sb(st, nc, "acc", [128, 512])
        sg = sb(st, nc, "sg", [128, 512])
        xb = sb(st, nc, "xbm", [128, 8, 512])
        xo = sb(st, nc, "xom", [128, 8, 512])
        pb = [ps(st, nc, "pb%d" % i, [128, 512]) for i in range(2)]
        pg = [ps(st, nc, "pg%d" % i, [128, 512]) for i in range(2)]
        po = [ps(st, nc, "po%d" % i, [128, 512]) for i in range(2)]
        P.dma("sync", wbr[:], g.wbr16.rearrange("(c p) n -> p c n", p=128), rk=("wbr16",))
        P.dma("sync", wo[:], g.wout16.rearrange("(c p) n -> p c n", p=128), rk=("wout16",))
        wv = g.win16.rearrange("(fc p) n -> p fc n", p=128)
        xv = xsrc.rearrange("(fc p) t -> p fc t", p=128)
        xdv = xdst.rearrange("(fc p) t -> p fc t", p=128)
        ysv = g.YS.rearrange("(c p) t -> p c t", p=128)
        k = 0
        wi = 0
        for (t0, n) in (LAST_BLOCKS if l == DEPTH - 1 else BLOCKS):
            s = 1 if t0 < CTX else 0
            P.dma("sync", ysf[:, :, :n], ysv[:, :, t0:t0 + n], rk=("YS",))
            P.copy(ysb[:, :, :n], ysf[:, :, :n], eng="gpsimd")
            P.dma("sync", xb[:, :, :n], xv[:, :, t0:t0 + n])
            P.dma("sync", xmb[:, :, :n], xmv[:, :, t0:t0 + n], rk=("XM16",))
            for ft in range(8):
                w = wgt[wi % 2]
                wi += 1
                for i in range(4):
                    c0 = NG_COLS + i * D + ft * 128
                    P.dma("sync", w[:, :, i * 128:(i + 1) * 128], wv[:, :, c0:c0 + 128], rk=("win16",))
                for i in range(4):
                    b_ = pb[k % 2]
                    g_ = pg[k % 2]
                    k += 1
                    for kc in range(2):
                        P.mm(b_[:, :n], wbr[:, i * 2 + kc, ft * 128:(ft + 1) * 128], ysb[:, i * 2 + kc, :n], start=(kc == 0), stop=(kc == 1))
                    for fc in range(8):
                        P.mm(g_[:, :n], w[:, fc, i * 128:(i + 1) * 128], xmb[:, fc, :n], start=(fc == 0), stop=(fc == 7))
                    P.act(sg[:, :n], g_[:, :n], AF.Sigmoid)
                    if i == 0:
                        P.tt(acc[:, :n], sg[:, :n], b_[:, :n], ALU.mult)
                    else:
                        P.tt(sg[:, :n], sg[:, :n], b_[:, :n], ALU.mult)
                        P.tt(acc[:, :n], acc[:, :n], sg[:, :n], ALU.add)
                P.copy(mg[:, ft, :n], acc[:, :n], eng="gpsimd")
            for fo in range(8):
                o_ = po[fo % 2]
                for fc in range(8):
                    P.mm(o_[:, :n], wo[:, fc, fo * 128:(fo + 1) * 128], mg[:, fc, :n], start=(fc == 0), stop=(fc == 7))
                P.stt(xo[:, fo, :n], o_[:, :n], g.MODT[:, s, 16 + fo:17 + fo], xb[:, fo, :n], ALU.mult, ALU.add)
            P.dma("sync", xdv[:, :, t0:t0 + n], xo[:, :, :n])
        P.flush()


def stage_moe(P, nc, g, l, xsrc, xdst):
    HALF = [LAST_BLOCKS] if l == DEPTH - 1 else [BLOCKS[0:5], BLOCKS[5:9]]
    for hb in HALF:
        h0 = hb[0][0]
        hn = sum(n for _, n in hb)
        with contextlib.ExitStack() as st:
            u2 = sb(st, nc, "u2", [128, 8, hn], BF16)
            combT = sb(st, nc, "combT", [16, hn])
            with contextlib.ExitStack() as st2:
                xb = [sb(st2, nc, "xb%d" % i, [128, 8, 512]) for i in range(2)]
                u2f = sb(st2, nc, "u2f", [128, 8, 512])
                sq = sb(st2, nc, "sq", [128, 8, 512], BF16)
                rs = sb(st2, nc, "rs", [128, 512])
                tmp = sb(st2, nc, "tmp", [128, 512])
                wr = sb(st2, nc, "wr", [128, 8, 20])
                lg = sb(st2, nc, "lg", [128, 20])
                sm = sb(st2, nc, "sm", [128, 16])
                sel = sb(st2, nc, "sel", [128, 4])
                sel2 = sb(st2, nc, "sel2", [128, 4])
                oh = sb(st2, nc, "oh", [128, 4])
                oh1 = sb(st2, nc, "oh1", [128, 4])
                oh2 = sb(st2, nc, "oh2", [128, 4])
                we = sb(st2, nc, "we", [128, 4])
                comb = sb(st2, nc, "comb", [128, 16])
                pss = ps(st2, nc, "pss", [128, 512])
                plg = ps(st2, nc, "plg", [128, 20])
                pct = ps(st2, nc, "pct", [16, 128])
                P.dma("sync", wr[:], g.w_router[l])
                xv = xsrc.rearrange("(fc p) t -> p fc t", p=128)
                for bi, (t0, n) in enumerate(hb):
                    s = 1 if t0 < CTX else 0
                    x = xb[bi % 2]
                    P.dma("sync", x[:, :, :n], xv[:, :, t0:t0 + n])
                    modulate_block(P, nc, g, x, t0 - h0, n, s, g.S2[:, s, :], g.MODT[:, s, 24:32], u2, sq, pss, rs, tmp, out_f32=u2f)
                    for tt_ in range(n // 128):
                        for fc in range(8):
                            P.mm(plg[:], u2f[:, fc, tt_ * 128:(tt_ + 1) * 128], wr[:, fc, :], start=(fc == 0), stop=(fc == 7))
                        P.copy(lg[:], plg[:])
                        A = sm
                        P.reduce(A[:, 0:1], lg[:, 0:4], ALU.max)
                        P.ts(oh[:], lg[:, 0:4], A[:, 0:1], None, op0=ALU.is_equal)
                        P.ts(A[:, 1:2], A[:, 0:1], -1.0, None, op0=ALU.mult)
                        P.act(A[:, 4:8], lg[:, 0:4], AF.Exp, bias=A[:, 1:2], scale=1.0)
                        P.reduce(A[:, 2:3], A[:, 4:8], ALU.add)
                        P.recip(A[:, 3:4], A[:, 2:3])
                        P.ts(sel[:], lg[:, 4:8], oh[:, 0:1], None, op0=ALU.mult)
                        for gi in range(1, 4):
                            P.stt(sel[:], lg[:, 4 + 4 * gi:8 + 4 * gi], oh[:, gi:gi + 1], sel[:], ALU.mult, ALU.add)
                        P.reduce(A[:, 8:9], sel[:], ALU.max)
                        P.ts(oh1[:], sel[:], A[:, 8:9], None, op0=ALU.is_equal)
                        P.stt(sel2[:], oh1[:], -1e30, sel[:], ALU.mult, ALU.add)
                        P.reduce(A[:, 9:10], sel2[:], ALU.max)
                        P.ts(oh2[:], sel2[:], A[:, 9:10], None, op0=ALU.is_equal)
                        P.tt(A[:, 10:11], A[:, 9:10], A[:, 8:9], ALU.subtract)
                        P.act(A[:, 11:12], A[:, 10:11], AF.Exp)
                        P.ts(A[:, 12:13], A[:, 11:12], 1.0, None, op0=ALU.add)
                        P.recip(A[:, 13:14], A[:, 12:13])
                        P.tt(A[:, 14:15], A[:, 11:12], A[:, 13:14], ALU.mult)
                        P.ts(we[:], oh1[:], A[:, 13:14], None, op0=ALU.mult)
                        P.stt(we[:], oh2[:], A[:, 14:15], we[:], ALU.mult, ALU.add)
                        P.ts(we[:], we[:], A[:, 3:4], None, op0=ALU.mult)
                        for gi in range(4):
                            P.ts(comb[:, gi * 4:gi * 4 + 4], we[:], oh[:, gi:gi + 1], None, op0=ALU.mult)
                        P.tr(pct[:], comb[:], g.IDENT[:])
                        c0 = t0 - h0 + tt_ * 128
                        P.copy(combT[:, c0:c0 + 128], pct[:], eng="scalar")
                P.dma("sync", g.COMBD[:, 0:hn], combT[:, :], wk=("COMBD",))
                P.flush()
            yacc = sb(st, nc, "yacc", [128, 8, hn])
            with contextlib.ExitStack() as st2:
                wg = [sb(st2, nc, "wg%d" % i, [128, 8, 512], BF16) for i in range(2)]
                wu = [sb(st2, nc, "wu%d" % i, [128, 8, 512], BF16) for i in range(2)]
                wd = [sb(st2, nc, "wd%d" % i, [128, 4, D], BF16) for i in range(1)]
                hT = [sb(st2, nc, "hT%d" % i, [128, 4, 512], BF16) for i in range(2)]
                cbs = [sb(st2, nc, "cb%d" % i, [128, 512]) for i in range(2)]
                KD = [0]
                t1 = [sb(st2, nc, "t1_%d" % i, [128, 512]) for i in range(2)]
                pgm = [ps(st2, nc, "pgm%d" % i, [128, 512]) for i in range(2)]
                pum = [ps(st2, nc, "pum%d" % i, [128, 512]) for i in range(2)]
                pdm = [ps(st2, nc, "pdm%d" % i, [128, 512]) for i in range(2)]
                pcm = ps(st2, nc, "pcm", [128, 512])
                k = 0
                kd = 0
                for e in range(16):
                    a, b_, c_ = wg[e % 2], wu[e % 2], wd[0]
                    P.dma("sync", a[:], g.wg16[e].rearrange("(fc p) n -> p fc n", p=128), rk=("wg16", e))
                    P.dma("sync", b_[:], g.wu16[e].rearrange("(fc p) n -> p fc n", p=128), rk=("wu16", e))
                    P.dma("sync", c_[:], g.wd16[e].rearrange("(fc p) n -> p fc n", p=128), rk=("wd16", e))
                    def emit_back(o0, n, h_, e=e, c_=c_):
                        nonlocal_kd = KD
                        for fo in range(8):
                            pd_ = pdm[nonlocal_kd[0] % 2]
                            nonlocal_kd[0] += 1
                            for ff in range(4):
                                P.mm(pd_[:, :n], c_[:, ff, fo * 128:(fo + 1) * 128], h_[:, ff, :n], start=(ff == 0), stop=(ff == 3))
                            if e == 0:
                                P.copy(yacc[:, fo, o0:o0 + n], pd_[:, :n])
                            else:
                                P.tt(yacc[:, fo, o0:o0 + n], yacc[:, fo, o0:o0 + n], pd_[:, :n], ALU.add)

                    prev = None
                    for bi, (t0, n) in enumerate(hb):
                        o0 = t0 - h0
                        h_ = hT[bi % 2]
                        cbt = cbs[bi % 2]
                        P.dma("sync", cbt[:, :n], g.COMBD[e:e + 1, o0:o0 + n].partition_broadcast(128), rk=("COMBD",))
                        for ff in range(4):
                            pg_, pu_ = pgm[k % 2], pum[k % 2]
                            tt1 = t1[k % 2]
                            k += 1
                            for fc in range(8):
                                P.mm(pg_[:, :n], a[:, fc, ff * 128:(ff + 1) * 128], u2[:, fc, o0:o0 + n], start=(fc == 0), stop=(fc == 7))
                            for fc in range(8):
                                P.mm(pu_[:, :n], b_[:, fc, ff * 128:(ff + 1) * 128], u2[:, fc, o0:o0 + n], start=(fc == 0), stop=(fc == 7))
                            P.act(tt1[:, :n], pg_[:, :n], AF.Silu)
                            P.tt(tt1[:, :n], tt1[:, :n], pu_[:, :n], ALU.mult)
                            P.tt(h_[:, ff, :n], tt1[:, :n], cbt[:, :n], ALU.mult, eng="gpsimd")
                        if prev is not None:
                            emit_back(*prev)
                        prev = (o0, n, h_)
                    emit_back(*prev)
                P.flush()
            with contextlib.ExitStack() as st2:
                xb = [sb(st2, nc, "xr%d" % i, [128, 8, 512]) for i in range(2)]
                xv = xsrc.rearrange("(fc p) t -> p fc t", p=128)
                xdv = xdst.rearrange("(fc p) t -> p fc t", p=128)
                for bi, (t0, n) in enumerate(hb):
                    s = 1 if t0 < CTX else 0
                    o0 = t0 - h0
                    x = xb[bi % 2]
                    P.dma("sync", x[:, :, :n], xv[:, :, t0:t0 + n])
                    for fo in range(8):
                        P.stt(x[:, fo, :n], yacc[:, fo, o0:o0 + n], g.MODT[:, s, 40 + fo:41 + fo], x[:, fo, :n], ALU.mult, ALU.add)
                    P.dma("sync", xdv[:, :, t0:t0 + n], x[:, :, :n])
                P.flush()


def stage_final(P, nc, g, xsrc):
    with contextlib.ExitStack() as st:
        xb = [sb(st, nc, "xf%d" % i, [128, 8, 512]) for i in range(2)]
        ob = [sb(st, nc, "of%d" % i, [128, 8, 512]) for i in range(2)]
        sq = sb(st, nc, "sqf", [128, 8, 512])
        rs = sb(st, nc, "rsf", [128, 512])
        nf = sb(st, nc, "nf", [128, 8])
        pss = ps(st, nc, "pssf", [128, 512])
        P.dma("sync", nf[:], g.norm_final)
        xv = xsrc.rearrange("(fc p) t -> p fc t", p=128)
        ov = g.out.rearrange("(fc p) t -> p fc t", p=128)
        for bi, (t0, n) in enumerate(LAST_BLOCKS):
            x, o = xb[bi % 2], ob[bi % 2]
            P.dma("sync", x[:], xv[:, :, t0:t0 + n])
            P.act(sq[:], x[:], AF.Square)
            for fc in range(8):
                P.mm(pss[:], g.ONES[:], sq[:, fc, :], start=(fc == 0), stop=(fc == 7))
            P.act(rs[:], pss[:], AF.Sqrt, bias=g.EPSC[:, 0:1], scale=1.0 / D)
            P.recip(rs[:], rs[:])
            for fc in range(8):
                P.stt(o[:, fc, :], x[:, fc, :], nf[:, fc:fc + 1], rs[:], ALU.mult, ALU.mult)
            P.dma("sync", ov[:, :, t0 - CTX:t0 - CTX + n], o[:])
        P.flush()


def build(nc, debug=None):
    g = Ctx()
    dt = nc.dram_tensor

    def inp(name, shape, dtype=F32):
        return dt(name, list(shape), dtype, kind="ExternalInput").ap()

    def scr(name, shape, dtype=F32):
        return dt(name, list(shape), dtype, kind="Internal").ap()

    g.xin = inp("xin", [D, T])
    g.c2 = inp("c2", [128, 8, 2])
    g.mod_w = inp("mod_w", [DEPTH, D, 6 * D])
    g.mod_b = inp("mod_b", [DEPTH, 128, 48])
    g.norm_mix = inp("norm_mix", [DEPTH, 128, 8])
    g.norm_ffn = inp("norm_ffn", [DEPTH, 128, 8])
    g.norm_final = inp("norm_final", [128, 8])
    g.w_in = inp("w_in", [DEPTH, D, IN_COLS])
    g.w_branch = inp("w_branch", [DEPTH, 4, 256, D])
    g.w_out = inp("w_out", [DEPTH, D, D])
    g.moe_w_gate = inp("moe_w_gate", [DEPTH, 16, D, 512])
    g.moe_w_up = inp("moe_w_up", [DEPTH, 16, D, 512])
    g.moe_w_down = inp("moe_w_down", [DEPTH, 16, 512, D])
    g.ident_in = inp("ident_in", [128, 128])
    g.sel_in = inp("sel_in", [16, 16 * 128])
    g.w_router = inp("w_router", [DEPTH, 128, 8, 20])
    g.out = dt("out", [D, LOUT], F32, kind="ExternalOutput").ap()
    g.hy_conv_w = inp("hy_conv_w", [DEPTH, 128, 6, 3])
    g.hy_conv_b = inp("hy_conv_b", [DEPTH, 128, 6])
    g.hy_f_w1 = inp("hy_f_w1", [DEPTH, 17, 64])
    g.hy_f_w2 = inp("hy_f_w2", [DEPTH, 64, 64])
    g.hy_f_w3 = inp("hy_f_w3", [DEPTH, 64, 512])
    g.hy_fvec = inp("hy_fvec", [DEPTH, 64, 3])
    g.hy_bias = inp("hy_bias", [DEPTH, 128, 2])
    g.hy_lag0 = inp("hy_lag0", [1, 2])
    g.hy_tabs = {}
    for L_ in (CTX, LAT):
        nt_ = L_ // 128
        nb_ = min(512, L_)
        kq_ = min(8, nt_)
        g.hy_tabs[L_] = dict(
            feats=inp("hyt_feats%d" % L_, [17, L_]),
            dec=inp("hyt_dec%d" % L_, [L_, 256]),
            cf=inp("hyt_cf%d" % L_, [nt_, 128, nt_, 128], BF16),
            sf=inp("hyt_sf%d" % L_, [nt_, 128, nt_, 128], BF16),
            ci=inp("hyt_ci%d" % L_, [L_ // nb_, nt_ // kq_, 128, kq_, nb_], BF16),
            si=inp("hyt_si%d" % L_, [L_ // nb_, nt_ // kq_, 128, kq_, nb_], BF16),
        )
    g.s5_d = inp("s5_d", [DEPTH, 128, 2])
    g.s5_lam = inp("s5_lam", [DEPTH, 2, 8, 128, 3])
    g.s5_bre = inp("s5_bre", [DEPTH, 2, 8, 128, 128])
    g.s5_bim = inp("s5_bim", [DEPTH, 2, 8, 128, 128])
    g.s5_cre = inp("s5_cre", [DEPTH, 2, 8, 128, 128])
    g.s5_cim = inp("s5_cim", [DEPTH, 2, 8, 128, 128])
    g.s5_w_glu = inp("s5_w_glu", [DEPTH, 256, 256])
    g.rw_mu = inp("rw_mu", [DEPTH, 128, 8])
    g.rw_w2 = inp("rw_w2", [DEPTH, 64, 256])
    g.rw_a2 = inp("rw_a2", [DEPTH, 64, 256])
    g.rw_g2 = inp("rw_g2", [DEPTH, 64, 256])
    g.rw_pc = inp("rw_pc", [DEPTH, 128, 2, 8])
    g.rw_ln = inp("rw_ln", [DEPTH, 128, 2, 2])
    g.blk_in = inp("blk_in", [128, 128])
    g.sel64_in = inp("sel64_in", [64, 32, 64], BF16)
    g.rw_masks = inp("rw_masks", [128, 4, 64])
    g.rw_mbd = inp("rw_mbd", [128, 2, 128])
    g.rw_i2 = inp("rw_i2", [128, 64])
    g.RWY = scr("RWY", [256, T])
    g.RWS = scr("RWS", [960, T])
    g.RWG = scr("RWG", [512, T])
    g.TOKD = scr("TOKD", [2, 2, NCH, 64, 640], BF16)
    g.m2_conv_w = inp("m2_conv_w", [DEPTH, 128, 6, 3])
    g.m2_conv_b = inp("m2_conv_b", [DEPTH, 128, 6])
    g.m2_masks = inp("m2_masks", [64, 4, 64])
    g.m2_dt_bias = inp("m2_dt_bias", [DEPTH, 8])
    g.m2_a_log = inp("m2_a_log", [DEPTH, 8])
    g.m2_d = inp("m2_d", [DEPTH, 4])
    g.m2_norm_w = inp("m2_norm_w", [DEPTH, 128, 2])
    g.SXT = scr("SXT", [T, 776])
    g.SBC = scr("SBC", [512, T])
    g.SYF = scr("SYF", [2, T, 256])
    g.HU = scr("HU", [256, T])
    g.HX0 = scr("HX0", [256, T])
    g.XM16 = scr("XM16", [D, T], BF16)
    g.COMBD = scr("COMBD", [16, 2304])
    if debug == "ys":
        g.ys_dbg = inp("ys_dbg", [DEPTH, D, T])
    if debug == "mix":
        g.dbg = dt("dbg", [D, T], F32, kind="ExternalOutput").ap()
    elif debug and debug != "ys":
        g.dbg = dt("dbg", list(debug), F32, kind="ExternalOutput").ap()

    g.win16_all = scr("win16", [DEPTH, D, IN_COLS], BF16)
    g.wout16_all = scr("wout16", [DEPTH, D, D], BF16)
    g.wbr16_all = scr("wbr16", [DEPTH, D, D], BF16)
    g.wg16_all = scr("wg16", [DEPTH, 16, D, 512], BF16)
    g.wu16_all = scr("wu16", [DEPTH, 16, D, 512], BF16)
    g.wd16_all = scr("wd16", [DEPTH, 16, 512, D], BF16)
    g.PX = scr("PX", [NG_COLS, T])
    g.YS = scr("YS", [D, T])
    g.XA = scr("XA", [D, T])
    g.XB = scr("XB", [D, T])

    with contextlib.ExitStack() as st:
        P = Prog(nc, st)
        g.P = P
        g.ONES = sb(st, nc, "ONES", [128, 128])
        g.EPSC = sb(st, nc, "EPSC", [128, 1])
        g.MODT = sb(st, nc, "MODT", [128, 2, 48])
        g.S1 = sb(st, nc, "S1", [128, 2, 8])
        g.S2 = sb(st, nc, "S2", [128, 2, 8])
        g.NMIX = sb(st, nc, "NMIX", [128, 8])
        g.NFFN = sb(st, nc, "NFFN", [128, 8])
        g.IDENT = sb(st, nc, "IDENT", [128, 128])
        g.SEL = sb(st, nc, "SEL", [16, 16 * 128])
        P.dma("sync", g.IDENT[:], g.ident_in)
        P.dma("sync", g.SEL[:], g.sel_in)
        g.BLK = sb(st, nc, "BLK", [128, 128])
        g.SEL64 = sb(st, nc, "SEL64", [64, 32, 64], BF16)
        P.dma("sync", g.BLK[:], g.blk_in)
        P.dma("sync", g.SEL64[:], g.sel64_in)
        P.memset(g.ONES[:], 1.0)
        g.ONES16 = sb(st, nc, "ONES16", [128, 128], BF16)
        P.copy(g.ONES16[:], g.ONES[:])
        P.memset(g.EPSC[:], EPS)
        for l in range(DEPTH):
            xsrc = g.xin if l == 0 else g.XB
            if l == 0:
                stage_precast(P, nc, g, 0)
            else:
                set_layer_weights(g, l)
            g.prefetch_next = (l + 1) if (l + 1 < DEPTH and not debug) else None
            stage_mod(P, nc, g, l)
            stage_proj(P, nc, g, l, xsrc)
            if debug == "ys":
                for r in range(8):
                    P.dma("sync", g.YS[r * 128:(r + 1) * 128, :], g.ys_dbg[l, r * 128:(r + 1) * 128, :], wk=("YS",))
                P.flush()
            elif debug == "mix":
                stage_mixers(P, nc, g, l)
                break
            elif debug:
                break
            else:
                stage_mixers(P, nc, g, l)
            stage_merge(P, nc, g, l, xsrc, g.XA)
            stage_moe(P, nc, g, l, g.XA, g.XB)
        if debug == "mix":
            P.flush()
            for r in range(8):
                P.dma("sync", g.dbg[r * 128:(r + 1) * 128, :], g.YS[r * 128:(r + 1) * 128, :], rk=("YSall",))
        elif debug and debug != "ys":
            P.dma("sync", g.dbg, g.PX[0:debug[0], :], rk=("PXall",))
        else:
            stage_final(P, nc, g, g.XB)
        P.flush()
    return nc


_TABS = {}


def hyena_tables():
    if _TABS:
        return _TABS
    out = {}
    for L_ in (CTX, LAT):
        n = 2 * L_
        nt = L_ // 128
        nb = min(512, L_)
        kq = min(8, nt)
        nq = nt // kq
        d = np.arange(L_, dtype=np.float64)
        ang = 2.0 * np.pi * np.outer(d, d + 0.5) / n
        for nm, fn in (("c", np.cos), ("s", np.sin)):
            M = fn(ang)
            fwd = M.reshape(nt, 128, nt, 128).transpose(2, 1, 0, 3)
            out["hyt_%sf%d" % (nm, L_)] = np.ascontiguousarray(fwd).astype(ml_dtypes.bfloat16)
            MT = M.T
            inv = MT.reshape(nq, kq, 128, L_ // nb, nb).transpose(3, 0, 2, 1, 4)
            out["hyt_%si%d" % (nm, L_)] = np.ascontiguousarray(inv).astype(ml_dtypes.bfloat16)
        t = np.linspace(0.0, 1.0, L_, dtype=np.float32)[:, None]
        w = np.float32(2.0 * math.pi) * np.arange(L_, dtype=np.float32)[:, None] / np.float32(L_)
        fq = np.linspace(1e-4, 7, 8, dtype=np.float32)[None, :]
        feats = np.concatenate([t, np.cos(fq * w), -np.sin(fq * w)], axis=-1).astype(np.float32)
        out["hyt_feats%d" % L_] = np.ascontiguousarray(feats.T)
        deltas = np.abs(np.linspace(math.log(1e-2) / 1.5, math.log(1e-2) / 0.3, 256, dtype=np.float32))
        out["hyt_dec%d" % L_] = np.exp(-t * deltas).astype(np.float32)
    _TABS.update(out)
    return _TABS


def hyena_host(inputs):
    f = lambda a: np.ascontiguousarray(np.asarray(a, dtype=np.float32))
    o = dict(hyena_tables())
    o["hy_conv_w"] = f(np.asarray(inputs["hy_conv_w"]).reshape(DEPTH, 3, 6, 128).transpose(0, 3, 2, 1))
    o["hy_conv_b"] = f(np.asarray(inputs["hy_conv_b"]).reshape(DEPTH, 6, 128).transpose(0, 2, 1))
    o["hy_f_w1"] = f(inputs["hy_f_w1"])
    o["hy_f_w2"] = f(inputs["hy_f_w2"])
    o["hy_f_w3"] = f(inputs["hy_f_w3"])
    o["hy_fvec"] = f(np.stack([np.asarray(inputs["hy_f_b1"]), np.asarray(inputs["hy_f_b2"]), np.asarray(inputs["hy_f_freq"])], axis=-1))
    o["hy_bias"] = f(np.asarray(inputs["hy_bias"]).reshape(DEPTH, 2, 128).transpose(0, 2, 1))
    return o


def s5_host(inputs):
    f = lambda a: np.ascontiguousarray(np.asarray(a, dtype=np.float32))
    o = {}
    o["s5_d"] = f(np.asarray(inputs["s5_d"]).reshape(DEPTH, 2, 128).transpose(0, 2, 1))
    o["s5_w_glu"] = f(inputs["s5_w_glu"])
    lr = np.asarray(inputs["s5_lam_re"]).reshape(DEPTH, 2, 8, 128)
    li = np.asarray(inputs["s5_lam_im"]).reshape(DEPTH, 2, 8, 128)
    ls = np.repeat(np.asarray(inputs["s5_log_step"])[..., None], 64, axis=-1).reshape(DEPTH, 2, 8, 128)
    o["s5_lam"] = f(np.stack([lr, li, ls], axis=-1))
    for nm, src, tr in (("s5_bre", "s5_b_re", False), ("s5_bim", "s5_b_im", False), ("s5_cre", "s5_c_re", True), ("s5_cim", "s5_c_im", True)):
        a = np.asarray(inputs[src])
        if tr:
            a = a.transpose(0, 1, 2, 4, 3)
        pad = np.zeros((DEPTH, 2, 8, 2, 64, 128), np.float32)
        for gg in range(16):
            st_, g2 = gg // 2, gg % 2
            c0 = 16 * (gg % 8)
            pad[:, :, st_, g2, :, c0:c0 + 16] = a[:, :, gg]
        o[nm] = f(pad.reshape(DEPTH, 2, 8, 128, 128))
    return o


def rw_host(inputs):
    f = lambda a: np.ascontiguousarray(np.asarray(a, dtype=np.float32))
    o = {}
    mu = np.zeros((DEPTH, 1024), np.float32)
    mu[:, :960] = np.asarray(inputs["rw_mu"])
    o["rw_mu"] = f(mu.reshape(DEPTH, 8, 128).transpose(0, 2, 1))
    o["rw_w2"] = f(np.asarray(inputs["rw_w2"]).reshape(DEPTH, 64, 256))
    o["rw_a2"] = f(np.asarray(inputs["rw_a2"]).reshape(DEPTH, 64, 256))
    o["rw_g2"] = f(inputs["rw_g2"])
    chan = lambda a: np.asarray(a).reshape(DEPTH, 2, 128).transpose(0, 2, 1)
    w0 = np.asarray(inputs["rw_w0"])
    a0 = np.asarray(inputs["rw_a0"])
    cols = [chan(w0[:, 0]), chan(w0[:, 1]), chan(a0[:, 0]), chan(a0[:, 1]), chan(inputs["rw_k_k"]), chan(inputs["rw_k_a"]),
            chan(np.asarray(inputs["rw_r_k"]).reshape(DEPTH, 256)), np.zeros((DEPTH, 128, 2), np.float32)]
    o["rw_pc"] = f(np.stack(cols, axis=-1))
    o["rw_ln"] = f(np.stack([chan(inputs["rw_ln_w"]), chan(inputs["rw_ln_b"])], axis=-1))
    blk = np.zeros((128, 128), np.float32)
    blk[:64, :64] = 1.0
    blk[64:, 64:] = 1.0
    o["blk_in"] = blk
    sel = np.zeros((64, 32, 64), np.float32)
    for r in range(64):
        sel[r, r % 32, :] = 1.0
    o["sel64_in"] = sel.astype(ml_dtypes.bfloat16)
    a_ = np.arange(64)
    ms = np.stack([(a_[:, None] <= a_[None, :]), (a_[:, None] > a_[None, :]), (a_[:, None] >= a_[None, :]), (a_[:, None] < a_[None, :])], axis=1).astype(np.float32)
    o["rw_masks"] = np.ascontiguousarray(np.concatenate([ms, ms], axis=0))
    mbd = np.zeros((128, 2, 128), np.float32)
    for h in range(2):
        mbd[h * 64:(h + 1) * 64, 0, h * 64:(h + 1) * 64] = ms[:, 3, :]
        mbd[h * 64:(h + 1) * 64, 1, h * 64:(h + 1) * 64] = ms[:, 1, :]
    o["rw_mbd"] = mbd
    o["rw_i2"] = np.ascontiguousarray(np.concatenate([np.eye(64, dtype=np.float32)] * 2, axis=0))
    return o


def m2_host(inputs):
    f = lambda a: np.ascontiguousarray(np.asarray(a, dtype=np.float32))
    o = {}
    o["m2_conv_w"] = f(np.asarray(inputs["m2_conv_w"]).reshape(DEPTH, 3, 6, 128).transpose(0, 3, 2, 1))
    o["m2_conv_b"] = f(np.asarray(inputs["m2_conv_b"]).reshape(DEPTH, 6, 128).transpose(0, 2, 1))
    a = np.arange(64)
    le = (a[:, None] <= a[None, :]).astype(np.float32)
    gt = (a[:, None] > a[None, :]).astype(np.float32)
    ge = (a[:, None] >= a[None, :]).astype(np.float32)
    lt = (a[:, None] < a[None, :]).astype(np.float32)
    o["m2_masks"] = f(np.stack([le, gt, ge, lt], axis=1))
    o["m2_dt_bias"] = f(np.asarray(inputs["m2_dt_bias"]).reshape(DEPTH, 8))
    o["m2_a_log"] = f(np.asarray(inputs["m2_a_log"]).reshape(DEPTH, 8))
    o["m2_d"] = f(inputs["m2_d"])
    o["m2_norm_w"] = f(np.asarray(inputs["m2_norm_w"]).reshape(DEPTH, 2, 128).transpose(0, 2, 1))
    return o


def host_inputs(inputs, lag0=None):
    f = lambda a: np.ascontiguousarray(np.asarray(a, dtype=np.float32))
    shared = {}
    shared["mod_w"] = f(inputs["mod_w"])
    shared["mod_b"] = f(np.asarray(inputs["mod_b"]).reshape(DEPTH, 48, 128).transpose(0, 2, 1))
    shared["norm_mix"] = f(np.asarray(inputs["norm_mix"]).reshape(DEPTH, 8, 128).transpose(0, 2, 1))
    shared["norm_ffn"] = f(np.asarray(inputs["norm_ffn"]).reshape(DEPTH, 8, 128).transpose(0, 2, 1))
    shared["norm_final"] = f(np.asarray(inputs["norm_final"]).reshape(8, 128).T)
    shared["ident_in"] = np.eye(128, dtype=np.float32)
    sel = np.zeros((16, 16, 128), np.float32)
    for e in range(16):
        sel[e, e, :] = 1.0
    shared["sel_in"] = sel.reshape(16, 16 * 128)
    wr = np.concatenate([np.asarray(inputs["moe_w_group"]),
                         np.asarray(inputs["moe_w_expert"]).transpose(0, 2, 1, 3).reshape(DEPTH, D, 16)], axis=-1)
    shared["w_router"] = f(wr.reshape(DEPTH, 8, 128, 20).transpose(0, 2, 1, 3))
    for k in ("w_in", "w_branch", "w_out", "moe_w_gate", "moe_w_up", "moe_w_down"):
        shared[k] = f(inputs[k])
    shared["hy_lag0"] = np.array([[1.0, 0.0]], np.float32) if lag0 is None else lag0
    shared.update(hyena_host(inputs))
    shared.update(s5_host(inputs))
    shared.update(rw_host(inputs))
    shared.update(m2_host(inputs))
    maps = []
    for b in range(4):
        m = dict(shared)
        xcat = np.concatenate([np.asarray(inputs["ctx"][b]), np.asarray(inputs["x"][b])], axis=0)
        m["xin"] = f(xcat.T)
        c2 = np.stack([np.asarray(inputs["c"][b]).reshape(8, 128).T, np.asarray(inputs["c_ctx"]).reshape(8, 128).T], axis=-1)
        m["c2"] = f(c2)
        maps.append(m)
    return maps


def make_reversed(inputs):
    r = {k: np.asarray(v) for k, v in inputs.items()}
    r["x"] = r["x"][:, ::-1]
    r["ctx"] = r["ctx"][:, ::-1]
    idx = np.arange(IN_COLS)
    def swap(a, b, n):
        t = idx[a:a + n].copy(); idx[a:a + n] = idx[b:b + n]; idx[b:b + n] = t
    swap(1792, 1824, 32)
    swap(1856, 1888, 32)
    swap(3008, 3012, 4)
    r["w_in"] = r["w_in"][:, :, idx]
    mi = np.arange(960)
    for a_, b_ in ((768, 800), (832, 864)):
        t_ = mi[a_:a_ + 32].copy(); mi[a_:a_ + 32] = mi[b_:b_ + 32]; mi[b_:b_ + 32] = t_
    r["rw_mu"] = r["rw_mu"][:, mi]
    r["hy_conv_w"] = r["hy_conv_w"][:, ::-1]
    r["m2_conv_w"] = r["m2_conv_w"][:, ::-1]
    r["hy_f_w3"] = np.concatenate([r["hy_f_w3"][:, :, 256:], r["hy_f_w3"][:, :, :256]], axis=-1)
    for k in ("s5_lam_re", "s5_lam_im", "s5_log_step", "s5_b_re", "s5_b_im", "s5_c_re", "s5_c_im",
              "rw_w0", "rw_w2", "rw_a0", "rw_a2", "m2_a_log", "m2_dt_bias"):
        r[k] = r[k][:, ::-1]
    return r


def kernel(**inputs):
    nc = bass.Bass("TRN2", target_bir_lowering=False)
    build(nc)
    maps = host_inputs(inputs, np.array([[1.0, 0.0]], np.float32))
    maps += host_inputs(make_reversed(inputs), np.array([[0.0, 1.0]], np.float32))
    res = run_bass_kernel_spmd(nc, maps, core_ids=list(range(8)))
    out = np.empty((4, LAT, D), np.float32)
    for b in range(4):
        out[b, :LOUT] = res.results[b]["out"].T
        out[b, LOUT:] = res.results[4 + b]["out"].T[::-1]
    return out
```
